# Optimizing a Trainium2 kernel written in Bass

```python
import math
import jax
import jax.numpy as jnp
from jax import lax
import numpy as np

D_MODEL = 1024
BATCH = 4
SEQ = 4096
DEPTH = 2

GRID_W = 64
CTX_LEN = 256

A_HEADS = 4
A_QK_DIM = 64
A_V_DIM = 2 * A_QK_DIM
A_WIDTH = A_HEADS * A_V_DIM
A_QK_COLS = A_HEADS * 2 * A_QK_DIM
ROPE_THETA = 10000.0
Q_BLOCK = 128
B_CHUNK = 128
B_GROUPS = 4
B_WIDTH = 512
B_GROUP_DIM = B_WIDTH // B_GROUPS
C_WIDTH = 512
C_BLOCKS = 8
C_BLOCK_DIM = C_WIDTH // C_BLOCKS
C_CONV = 4
CONV_PAD = (C_CONV // 2, (C_CONV - 1) // 2)
C_POW = 8.0
N_BRANCH = 3
BRANCH_WIDTH = 512
IN_SIZES = (A_QK_COLS, A_QK_COLS, A_WIDTH, B_WIDTH, B_WIDTH, C_WIDTH, C_WIDTH, N_BRANCH * D_MODEL)
IN_COLS = sum(IN_SIZES)
IN_SPLITS = tuple(int(v) for v in np.cumsum(IN_SIZES)[:-1])
N_EXPERTS = 64
TOP_K = 8
N_GROUPS = 8
TOPK_GROUPS = 4
D_EXPERT = 256
D_SHARED = 256
ROUTED_SCALE = 2.5
MOE_BLOCK = 128
DN_ALPHA = (2 * DEPTH) ** 0.25
DN_BETA = (8 * DEPTH) ** -0.25
LN_EPS = 1e-6
RMS_EPS = 1e-5
F32 = jnp.float32

kernel_name = "hybrid_diffusion_block"


def layer_norm(x, gain=None, bias=None):
    xf = x.astype(F32)
    mu = jnp.mean(xf, -1, keepdims=True)
    var = jnp.mean(jnp.square(xf - mu), -1, keepdims=True)
    y = (xf - mu) * lax.rsqrt(var + LN_EPS)
    if gain is not None:
        y = y * gain.astype(F32) + bias.astype(F32)
    return y.astype(x.dtype)


def rms_norm(x, gain):
    xf = x.astype(F32)
    y = xf * lax.rsqrt(jnp.mean(jnp.square(xf), -1, keepdims=True) + RMS_EPS)
    return (y * gain.astype(F32)).astype(x.dtype)


def modulate(h, shift, scale):
    return h * (1.0 + scale) + shift


def axial_rope_tables(n):
    rows = n // GRID_W
    pos_row = jnp.repeat(jnp.arange(rows), GRID_W).astype(F32)
    pos_col = jnp.tile(jnp.arange(GRID_W), rows).astype(F32)
    quarter = A_QK_DIM // 4
    inv = ROPE_THETA ** (-jnp.arange(quarter, dtype=F32) / quarter)
    ang_r = pos_row[:, None] * inv
    ang_c = pos_col[:, None] * inv
    ang = jnp.concatenate([ang_r, ang_r, ang_c, ang_c], axis=-1)
    return jnp.cos(ang)[:, None, None, :], jnp.sin(ang)[:, None, None, :]


def apply_rope(t, cos, sin):
    tr = t.reshape(*t.shape[:-1], 2, 2, A_QK_DIM // 4)
    rot = jnp.stack([-tr[..., 1, :], tr[..., 0, :]], axis=-2).reshape(t.shape)
    return t * cos + rot * sin


def _qk_heads(t):
    return t.reshape(*t.shape[:-1], A_HEADS, 2, A_QK_DIM)


def _v_heads(t):
    return t.reshape(*t.shape[:-1], A_HEADS, A_V_DIM)


def diff_attn_core(q, k, v, lam):
    s = jnp.einsum("bqhcd,bkhcd->bhcqk", q, k, preferred_element_type=F32) * (A_QK_DIM ** -0.5)
    p = jax.nn.softmax(s, axis=-1)
    w = p[:, :, 0] - lam * p[:, :, 1]
    return jnp.einsum("bhqk,bkhe->bqhe", w.astype(v.dtype), v)


def diff_attn_latent(q, k, v, k_ctx, v_ctx, lam):
    b, n = q.shape[:2]
    k_all = jnp.concatenate([k_ctx, k.astype(k_ctx.dtype)], axis=1)
    v_all = jnp.concatenate([v_ctx, v], axis=1)
    qb = jnp.moveaxis(q.reshape(b, n // Q_BLOCK, Q_BLOCK, *q.shape[2:]), 1, 0)
    out = lax.map(lambda qi: diff_attn_core(qi, k_all, v_all, lam), qb)
    return jnp.moveaxis(out, 0, 1).reshape(b, n, A_HEADS, A_V_DIM)


def _diff_post(o, gain, lam_init):
    return (rms_norm(o, gain) * (1.0 - lam_init)).reshape(*o.shape[:2], A_WIDTH)


def spatial_gating(u, v, ln_g, ln_b, w_s, b_s):
    b, n, _ = v.shape
    vg = v.reshape(b, n // B_CHUNK, B_CHUNK, B_GROUPS, B_GROUP_DIM)
    vg = layer_norm(vg, ln_g.reshape(B_GROUPS, B_GROUP_DIM), ln_b.reshape(B_GROUPS, B_GROUP_DIM))
    mixed = jnp.einsum("gpq,bnqgc->bnpgc", w_s, vg) + b_s.T[:, :, None]
    return u * mixed.reshape(b, n, B_WIDTH)


def depthwise_conv(x, w, bias):
    y = lax.conv_general_dilated(
        x, w.astype(x.dtype)[:, None, :], window_strides=(1,), padding=[CONV_PAD],
        dimension_numbers=("NWC", "WIO", "NWC"), feature_group_count=x.shape[-1])
    return y + bias


def block_diag(x, w, bias):
    xb = x.reshape(*x.shape[:-1], C_BLOCKS, C_BLOCK_DIM)
    return jnp.einsum("bnhi,hij->bnhj", xb, w).reshape(x.shape) + bias


def rglru_coeffs(x, w_a, b_a, w_x, b_x, lam):
    xf = x.astype(F32)
    r = jax.nn.sigmoid(block_diag(xf, w_a, b_a).astype(F32))
    i = jax.nn.sigmoid(block_diag(xf, w_x, b_x).astype(F32))
    log_a = -C_POW * r * jax.nn.softplus(-lam.astype(F32))
    a = jnp.exp(log_a)
    return a, jnp.sqrt(-jnp.expm1(2.0 * log_a)) * (i * xf)


def _lin_combine(left, right):
    a_l, h_l = left
    a_r, h_r = right
    return a_l * a_r, a_r * h_l + h_r


def rglru_direction(x_ctx, x_lat, w_a, b_a, w_x, b_x, lam, reverse):
    if reverse:
        x_ctx, x_lat = x_ctx[:, ::-1], x_lat[:, ::-1]
    a_c, u_c = rglru_coeffs(x_ctx, w_a, b_a, w_x, b_x, lam)
    _, h_c = lax.associative_scan(_lin_combine, (a_c, u_c), axis=1)
    a_l, u_l = rglru_coeffs(x_lat, w_a, b_a, w_x, b_x, lam)
    a_cum, h_l = lax.associative_scan(_lin_combine, (a_l, u_l), axis=1)
    h_l = h_l + a_cum * h_c[:, -1:, :]
    if reverse:
        h_c, h_l = h_c[:, ::-1], h_l[:, ::-1]
    return h_c.astype(x_ctx.dtype), h_l.astype(x_lat.dtype)


def merge_branches(o_a, o_b, o_c, g, w_branch, w_out):
    o = jnp.stack([o_a, o_b, o_c], axis=-2)
    proj = jnp.einsum("bnrc,rcd->bnrd", o, w_branch)
    gates = jax.nn.sigmoid(g.reshape(*g.shape[:-1], N_BRANCH, D_MODEL).astype(F32)).astype(proj.dtype)
    return jnp.sum(gates * proj, axis=-2) @ w_out


def token_mixer(h_lat, h_ctx, cos, sin, layer_idx, need_ctx_out,
                w_in, b_in, lam_q1, lam_k1, lam_q2, lam_k2, attn_norm_g,
                sg_ln_g, sg_ln_b, sg_w, sg_b,
                conv_w, conv_b, lru_wa, lru_ba, lru_wx, lru_bx, lru_lam,
                w_branch, w_out):
    q_l, k_l, v_l, u_l, s_l, x_l, y_l, g_l = jnp.split(h_lat @ w_in + b_in, IN_SPLITS, axis=-1)
    q_c, k_c, v_c, u_c, s_c, x_c, y_c, g_c = jnp.split(h_ctx @ w_in + b_in, IN_SPLITS, axis=-1)

    lam_init = 0.8 - 0.6 * math.exp(-0.3 * layer_idx)
    lam = (jnp.exp(jnp.sum(lam_q1.astype(F32) * lam_k1.astype(F32)))
           - jnp.exp(jnp.sum(lam_q2.astype(F32) * lam_k2.astype(F32))) + lam_init)
    kc = _qk_heads(k_c)
    vc = _v_heads(v_c)
    attn_l = diff_attn_latent(apply_rope(_qk_heads(q_l), cos, sin), apply_rope(_qk_heads(k_l), cos, sin),
                              _v_heads(v_l), kc, vc, lam)
    o_a_l = _diff_post(attn_l, attn_norm_g, lam_init)

    o_b_l = spatial_gating(jax.nn.gelu(u_l), jax.nn.gelu(s_l), sg_ln_g, sg_ln_b, sg_w, sg_b)

    xs_l = depthwise_conv(x_l, conv_w, conv_b)
    xs_c = depthwise_conv(x_c, conv_w, conv_b)
    hc_f, hl_f = rglru_direction(xs_c, xs_l, lru_wa[0], lru_ba[0], lru_wx[0], lru_bx[0], lru_lam[0], False)
    hc_b, hl_b = rglru_direction(xs_c, xs_l, lru_wa[1], lru_ba[1], lru_wx[1], lru_bx[1], lru_lam[1], True)
    o_c_l = jax.nn.gelu(y_l) * (hl_f + hl_b)

    mix_lat = merge_branches(o_a_l, o_b_l, o_c_l, g_l, w_branch, w_out)
    if not need_ctx_out:
        return mix_lat, None
    o_a_c = _diff_post(diff_attn_core(_qk_heads(q_c), kc, vc, lam), attn_norm_g, lam_init)
    o_b_c = spatial_gating(jax.nn.gelu(u_c), jax.nn.gelu(s_c), sg_ln_g, sg_ln_b, sg_w, sg_b)
    o_c_c = jax.nn.gelu(y_c) * (hc_f + hc_b)
    mix_ctx = merge_branches(o_a_c, o_b_c, o_c_c, g_c, w_branch, w_out)
    return mix_lat, mix_ctx


def moe_ffn(h, w_router, router_bias, w_gate, w_up, w_down, sh_gate, sh_up, sh_down):
    n_tok = h.shape[0]
    per_group = N_EXPERTS // N_GROUPS
    scores = jax.nn.sigmoid(jnp.matmul(h, w_router, preferred_element_type=F32))
    biased = scores + router_bias.astype(F32)
    group_score = jnp.sum(lax.top_k(biased.reshape(n_tok, N_GROUPS, per_group), 2)[0], axis=-1)
    _, top_groups = lax.top_k(group_score, TOPK_GROUPS)
    tok_rows = jnp.arange(n_tok)[:, None]
    group_ok = jnp.zeros((n_tok, N_GROUPS), bool).at[tok_rows, top_groups].set(True)
    expert_ok = jnp.repeat(group_ok, per_group, axis=1)
    _, top_e = lax.top_k(jnp.where(expert_ok, biased, -jnp.inf), TOP_K)
    s_sel = jnp.take_along_axis(scores, top_e, axis=1)
    weights = s_sel / jnp.sum(s_sel, -1, keepdims=True) * ROUTED_SCALE

    n_assign = n_tok * TOP_K
    flat_e = top_e.reshape(n_assign)
    flat_tok = jnp.repeat(jnp.arange(n_tok, dtype=jnp.int32), TOP_K)
    flat_w = weights.reshape(n_assign)
    order = jnp.argsort(flat_e)
    e_sorted = flat_e[order]
    counts = jnp.bincount(flat_e, length=N_EXPERTS)
    padded = (counts + MOE_BLOCK - 1) // MOE_BLOCK * MOE_BLOCK
    pad_end = jnp.cumsum(padded)
    pad_start = pad_end - padded
    start = jnp.cumsum(counts) - counts
    dest = pad_start[e_sorted] + (jnp.arange(n_assign) - start[e_sorted])
    n_slots = n_assign + N_EXPERTS * MOE_BLOCK
    n_blocks = n_slots // MOE_BLOCK
    slot_tok = jnp.zeros((n_slots,), jnp.int32).at[dest].set(flat_tok[order])
    slot_w = jnp.zeros((n_slots,), h.dtype).at[dest].set(flat_w[order].astype(h.dtype))
    block_e = jnp.minimum(jnp.searchsorted(pad_end, jnp.arange(n_blocks) * MOE_BLOCK, side="right"),
                          N_EXPERTS - 1)

    def expert_block(args):
        tok, wts, e = args
        xb = h[tok]
        act = jax.nn.silu(xb @ w_gate[e]) * (xb @ w_up[e])
        return (act @ w_down[e]) * wts[:, None]

    routed_slots = lax.map(expert_block, (slot_tok.reshape(n_blocks, MOE_BLOCK),
                                          slot_w.reshape(n_blocks, MOE_BLOCK), block_e))
    routed = jnp.zeros_like(h).at[slot_tok].add(routed_slots.reshape(n_slots, D_MODEL).astype(h.dtype))
    shared = (jax.nn.silu(h @ sh_gate) * (h @ sh_up)) @ sh_down
    return routed + shared


def setup_inputs(seed: int = 0) -> dict:
    key = jax.random.key(seed)
    ks = iter(jax.random.split(key, 48))

    def nrm(shape, scale):
        return jax.random.normal(next(ks), shape, F32) * scale

    def gain(shape):
        return 1.0 + nrm(shape, 0.02)

    L = DEPTH
    u_lam = jax.random.uniform(next(ks), (L, 2, C_WIDTH), F32, minval=0.9, maxval=0.999)
    s_lam = u_lam ** (1.0 / C_POW)
    return {
        "x": nrm((BATCH, SEQ, D_MODEL), 1.0),
        "c": nrm((BATCH, D_MODEL), 1.0),
        "ctx": nrm((BATCH, CTX_LEN, D_MODEL), 1.0),
        "c_ctx": nrm((D_MODEL,), 1.0),
        "w_mod": nrm((L, D_MODEL, 6 * D_MODEL), 0.5 * D_MODEL ** -0.5),
        "b_mod": nrm((L, 6 * D_MODEL), 0.02),
        "w_in": nrm((L, D_MODEL, IN_COLS), D_MODEL ** -0.5),
        "b_in": nrm((L, IN_COLS), 0.02),
        "lam_q1": nrm((L, A_QK_DIM), 0.1),
        "lam_k1": nrm((L, A_QK_DIM), 0.1),
        "lam_q2": nrm((L, A_QK_DIM), 0.1),
        "lam_k2": nrm((L, A_QK_DIM), 0.1),
        "attn_norm_g": gain((L, A_HEADS, A_V_DIM)),
        "sg_ln_g": gain((L, B_WIDTH)),
        "sg_ln_b": nrm((L, B_WIDTH), 0.02),
        "sg_w": nrm((L, B_GROUPS, B_CHUNK, B_CHUNK), B_CHUNK ** -0.5),
        "sg_b": 1.0 + nrm((L, B_GROUPS, B_CHUNK), 0.01),
        "conv_w": nrm((L, C_CONV, C_WIDTH), C_CONV ** -0.5),
        "conv_b": nrm((L, C_WIDTH), 0.02),
        "lru_wa": nrm((L, 2, C_BLOCKS, C_BLOCK_DIM, C_BLOCK_DIM), C_BLOCK_DIM ** -0.5),
        "lru_ba": nrm((L, 2, C_WIDTH), 0.02),
        "lru_wx": nrm((L, 2, C_BLOCKS, C_BLOCK_DIM, C_BLOCK_DIM), C_BLOCK_DIM ** -0.5),
        "lru_bx": nrm((L, 2, C_WIDTH), 0.02),
        "lru_lam": jnp.log(s_lam) - jnp.log1p(-s_lam),
        "w_branch": nrm((L, N_BRANCH, BRANCH_WIDTH, D_MODEL), BRANCH_WIDTH ** -0.5),
        "w_out": nrm((L, D_MODEL, D_MODEL), DN_BETA * D_MODEL ** -0.5),
        "ln1_g": gain((L, D_MODEL)),
        "ln1_b": nrm((L, D_MODEL), 0.02),
        "w_router": nrm((L, D_MODEL, N_EXPERTS), D_MODEL ** -0.5),
        "router_bias": nrm((L, N_EXPERTS), 0.01),
        "moe_w_gate": nrm((L, N_EXPERTS, D_MODEL, D_EXPERT), D_MODEL ** -0.5),
        "moe_w_up": nrm((L, N_EXPERTS, D_MODEL, D_EXPERT), D_MODEL ** -0.5),
        "moe_w_down": nrm((L, N_EXPERTS, D_EXPERT, D_MODEL), DN_BETA * D_EXPERT ** -0.5),
        "sh_w_gate": nrm((L, D_MODEL, D_SHARED), D_MODEL ** -0.5),
        "sh_w_up": nrm((L, D_MODEL, D_SHARED), D_MODEL ** -0.5),
        "sh_w_down": nrm((L, D_SHARED, D_MODEL), DN_BETA * D_SHARED ** -0.5),
        "ln2_g": gain((L, D_MODEL)),
        "ln2_b": nrm((L, D_MODEL), 0.02),
    }


def reference(x, c, ctx, c_ctx, w_mod, b_mod, w_in, b_in, lam_q1, lam_k1, lam_q2, lam_k2, attn_norm_g,
              sg_ln_g, sg_ln_b, sg_w, sg_b, conv_w, conv_b, lru_wa, lru_ba, lru_wx, lru_bx, lru_lam,
              w_branch, w_out, ln1_g, ln1_b, w_router, router_bias, moe_w_gate, moe_w_up, moe_w_down,
              sh_w_gate, sh_w_up, sh_w_down, ln2_g, ln2_b):
    b, n, _ = x.shape
    n_ctx = ctx.shape[1]
    cos, sin = axial_rope_tables(n)
    for i in range(DEPTH):
        last = i == DEPTH - 1
        mod = jax.nn.silu(c) @ w_mod[i] + b_mod[i]
        mod_c = jax.nn.silu(c_ctx) @ w_mod[i] + b_mod[i]
        sh1, sc1, g1, sh2, sc2, g2 = jnp.split(mod[:, None, :], 6, axis=-1)
        csh1, csc1, cg1, csh2, csc2, cg2 = jnp.split(mod_c, 6, axis=-1)

        h_lat = modulate(layer_norm(x), sh1, sc1)
        h_ctx = modulate(layer_norm(ctx), csh1, csc1)
        mix_lat, mix_ctx = token_mixer(
            h_lat, h_ctx, cos, sin, i, not last,
            w_in[i], b_in[i], lam_q1[i], lam_k1[i], lam_q2[i], lam_k2[i], attn_norm_g[i],
            sg_ln_g[i], sg_ln_b[i], sg_w[i], sg_b[i],
            conv_w[i], conv_b[i], lru_wa[i], lru_ba[i], lru_wx[i], lru_bx[i], lru_lam[i],
            w_branch[i], w_out[i])
        x = layer_norm(DN_ALPHA * x + g1 * mix_lat, ln1_g[i], ln1_b[i])
        h2 = modulate(layer_norm(x), sh2, sc2).reshape(b * n, D_MODEL)
        moe_args = (w_router[i], router_bias[i], moe_w_gate[i], moe_w_up[i], moe_w_down[i],
                    sh_w_gate[i], sh_w_up[i], sh_w_down[i])
        if last:
            y = moe_ffn(h2, *moe_args)
            x = layer_norm(DN_ALPHA * x + g2 * y.reshape(b, n, D_MODEL), ln2_g[i], ln2_b[i])
        else:
            ctx = layer_norm(DN_ALPHA * ctx + cg1 * mix_ctx, ln1_g[i], ln1_b[i])
            h2c = modulate(layer_norm(ctx), csh2, csc2).reshape(b * n_ctx, D_MODEL)
            y = moe_ffn(jnp.concatenate([h2, h2c], axis=0), *moe_args)
            x = layer_norm(DN_ALPHA * x + g2 * y[: b * n].reshape(b, n, D_MODEL), ln2_g[i], ln2_b[i])
            ctx = layer_norm(DN_ALPHA * ctx + cg2 * y[b * n:].reshape(b, n_ctx, D_MODEL), ln2_g[i], ln2_b[i])
    return x
```

```python
import math
import numpy as np
import concourse.bass as bass
import concourse.mybir as mybir
from concourse.bass_utils import run_bass_kernel_spmd
from contextlib import ExitStack

F32 = mybir.dt.float32
BF16 = mybir.dt.bfloat16
I32 = mybir.dt.int32
AF = mybir.ActivationFunctionType
ALU = mybir.AluOpType
AX = mybir.AxisListType

D = 1024
NCTX = 256
NLAT = 4096
T = NCTX + NLAT
NT = T // 128
DEPTH = 2
INC = 6656
NE = 64
CAP = 2048
NSLOT = NE * CAP
DN_ALPHA = (2 * DEPTH) ** 0.25
LN_EPS = 1e-6
RMS_EPS = 1e-5
BIG = 1.0e6
OQ, OK_, OV, OU, OS, OX, OY, OG = 0, 512, 1024, 1536, 2048, 2560, 3072, 3584

W_NAMES = ["w_mod", "b_mod", "w_in", "b_in", "lam_q1", "lam_k1", "lam_q2", "lam_k2", "attn_norm_g",
           "sg_ln_g", "sg_ln_b", "sg_w", "sg_b", "conv_w", "conv_b", "lru_wa", "lru_ba", "lru_wx", "lru_bx",
           "lru_lam", "w_branch", "w_out", "ln1_g", "ln1_b", "w_router", "router_bias", "moe_w_gate",
           "moe_w_up", "moe_w_down", "sh_w_gate", "sh_w_up", "sh_w_down", "ln2_g", "ln2_b"]
W_SHAPES = {
    "w_mod": [2, 1024, 6144], "b_mod": [2, 6144], "w_in": [2, 1024, 6656], "b_in": [2, 6656],
    "lam_q1": [2, 64], "lam_k1": [2, 64], "lam_q2": [2, 64], "lam_k2": [2, 64], "attn_norm_g": [2, 4, 128],
    "sg_ln_g": [2, 512], "sg_ln_b": [2, 512], "sg_w": [2, 4, 128, 128], "sg_b": [2, 4, 128],
    "conv_w": [2, 4, 512], "conv_b": [2, 512], "lru_wa": [2, 2, 8, 64, 64], "lru_ba": [2, 2, 512],
    "lru_wx": [2, 2, 8, 64, 64], "lru_bx": [2, 2, 512], "lru_lam": [2, 2, 512],
    "w_branch": [2, 3, 512, 1024], "w_out": [2, 1024, 1024], "ln1_g": [2, 1024], "ln1_b": [2, 1024],
    "w_router": [2, 1024, 64], "router_bias": [2, 64], "moe_w_gate": [2, 64, 1024, 256],
    "moe_w_up": [2, 64, 1024, 256], "moe_w_down": [2, 64, 256, 1024], "sh_w_gate": [2, 1024, 256],
    "sh_w_up": [2, 1024, 256], "sh_w_down": [2, 256, 1024], "ln2_g": [2, 1024], "ln2_b": [2, 1024],
}

SEM_EPOCH = 30000
NS_DMA = 8


class Builder:
    def __init__(self, nc):
        self.nc = nc
        self.E = {"pe": nc.tensor, "act": nc.scalar, "dve": nc.vector, "pool": nc.gpsimd, "sp": nc.sync}
        self.cur = {}
        self.known = {e: {} for e in self.E}
        self.lastw = {}
        self.rd = {}
        self.nsem = 0
        self.own = {e: set() for e in self.E}
        self.dq = {q: {"sems": [None] * NS_DMA, "val": [0] * NS_DMA, "i": 0} for q in ("sp", "pool", "act")}
        self.ninst = 0

    def newsem(self):
        self.nsem += 1
        return self.nc.semaphore(f"sm{self.nsem}").__enter__()

    def _wait(self, e, sem, val):
        if self.known[e].get(sem, 0) >= val:
            return
        self.E[e].wait_ge(sem, val)
        self.known[e][sem] = val

    def _deps(self, e, R, W):
        toks = {}
        for r in R:
            t = self.lastw.get(r)
            if t is not None and toks.get(t[0], 0) < t[1]:
                toks[t[0]] = t[1]
        for w in W:
            t = self.lastw.get(w)
            if t is not None and toks.get(t[0], 0) < t[1]:
                toks[t[0]] = t[1]
            for sm, v in self.rd.get(w, {}).items():
                if toks.get(sm, 0) < v:
                    toks[sm] = v
        for sm, v in toks.items():
            if e == "pe" and sm in self.own["pe"]:
                continue
            self._wait(e, sm, v)

    def _commit(self, tok, R, W):
        for w in W:
            self.lastw[w] = tok
            self.rd[w] = {}
        for r in R:
            d = self.rd.setdefault(r, {})
            if d.get(tok[0], 0) < tok[1]:
                d[tok[0]] = tok[1]

    def op(self, e, fn, R=(), W=()):
        self._deps(e, R, W)
        st = self.cur.get(e)
        if st is None or st[1] >= SEM_EPOCH:
            st = self.cur[e] = [self.newsem(), 0]
            self.own[e].add(st[0])
        ins = fn(self.E[e])
        st[1] += 1
        ins.then_inc(st[0], 1)
        self.ninst += 1
        self._commit((st[0], st[1]), R, W)

    def dma(self, q, fn, R=(), W=()):
        self._deps(q, R, W)
        d = self.dq[q]
        slot = d["i"] % NS_DMA
        d["i"] += 1
        if d["sems"][slot] is None or d["val"][slot] + 16 > SEM_EPOCH:
            if d["sems"][slot] is not None:
                self._wait(q, d["sems"][slot], d["val"][slot])
            d["sems"][slot] = self.newsem()
            d["val"][slot] = 0
        sem, prev = d["sems"][slot], d["val"][slot]
        if prev > 0:
            self._wait(q, sem, prev)
        ins = fn(self.E[q])
        ins.then_inc(sem, 16)
        d["val"][slot] = prev + 16
        self.ninst += 1
        self._commit((sem, prev + 16), R, W)

    def barrier(self, engines=None):
        toks = []
        for e, st in self.cur.items():
            if st[1] > 0:
                toks.append((st[0], st[1]))
        for q, d in self.dq.items():
            for sm, v in zip(d["sems"], d["val"]):
                if sm is not None and v > 0:
                    toks.append((sm, v))
        for e in (engines or list(self.E)):
            for sm, v in toks:
                self._wait(e, sm, v)
        if engines is None:
            self.lastw.clear()
            self.rd.clear()


def build(n_layers=DEPTH, debug=()):
    nc = bass.Bass("TRN2", target_bir_lowering=False)
    Bd = Builder(nc)
    op, dma = Bd.op, Bd.dma
    bc_reg = nc.gpsimd.to_reg(NSLOT - 1)

    def din(name, shape, dt=F32):
        return nc.dram_tensor(name, list(shape), dt, kind="ExternalInput").ap()

    def dscr(name, shape, dt=F32):
        kind = "ExternalOutput" if name in debug else "Internal"
        return nc.dram_tensor(name, list(shape), dt, kind=kind).ap()

    x_in = din("x", [NLAT, D])
    ctx_in = din("ctx", [NCTX, D])
    c_in = din("c", [2, D])
    Wd = {n: din(n, W_SHAPES[n]) for n in W_NAMES}
    out_d = nc.dram_tensor("out", [NLAT, D], F32, kind="ExternalOutput").ap()

    xA = dscr("xA", [T, D])
    x1s = dscr("x1s", [T, D])
    modv = dscr("modv", [2, 6144])
    qT_d = dscr("qT_d", [4, 128, T], BF16)
    kT_d = dscr("kT_d", [4, 128, T], BF16)
    v_d = dscr("v_d", [T, 512], BF16)
    xT_d = dscr("xT_d", [512, T], F32)
    yT_d = dscr("yT_d", [512, T], BF16)
    gT_d = dscr("gT_d", [3072, T], BF16)
    oaT_d = dscr("oaT_d", [512, T], BF16)
    obT_d = dscr("obT_d", [512, T], BF16)
    ocT_d = dscr("ocT_d", [512, T], BF16)
    cos_d = dscr("cos_d", [128, NLAT], F32)
    sin_d = dscr("sin_d", [128, NLAT], F32)
    Xg = dscr("Xg", [NSLOT, D], BF16)
    Yg = dscr("Yg", [NSLOT, D], BF16)
    ysh_d = dscr("ysh_d", [T, D], F32)
    dbg_d = dscr("dbg_d", [T, 64], F32)

    uid = [0]

    def sb(stack, name, shape, dt=F32):
        uid[0] += 1
        return stack.enter_context(nc.sbuf_tensor(f"{name}_u{uid[0]}", list(shape), dt))

    top = ExitStack()
    psA = [top.enter_context(nc.psum_tensor(f"psA{i}", [128, 512], F32)) for i in range(4)]
    psS = [top.enter_context(nc.psum_tensor(f"psS{i}", [128, 512], F32)) for i in range(2)]
    psT = [top.enter_context(nc.psum_tensor(f"psT{i}", [128, 1024], BF16)) for i in range(2)]

    ident = sb(top, "ident", [128, 128], BF16)
    ones_bf = sb(top, "ones_bf", [128, 128], BF16)
    Ustr = sb(top, "Ustr", [128, 128], BF16)
    Pm = sb(top, "Pm", [128, 128], BF16)
    rowi = sb(top, "rowi", [128, 1], F32)
    coli = sb(top, "coli", [128, 128], F32)
    ctmp = sb(top, "ctmp", [128, 128], F32)
    ctmp2 = sb(top, "ctmp2", [128, 128], F32)
    eidx = sb(top, "eidx", [128, 64], F32)
    op("pool", lambda e: e.iota(rowi[:], pattern=[[0, 1]], base=0, channel_multiplier=1,
                                allow_small_or_imprecise_dtypes=True), W=["rowi"])
    op("pool", lambda e: e.iota(coli[:], pattern=[[1, 128]], base=0, channel_multiplier=0,
                                allow_small_or_imprecise_dtypes=True), W=["coli"])
    op("pool", lambda e: e.iota(eidx[:], pattern=[[CAP, 64]], base=0, channel_multiplier=0,
                                allow_small_or_imprecise_dtypes=True), W=["eidx"])
    op("dve", lambda e: e.tensor_scalar(out=ident[:], in0=coli[:], scalar1=rowi[:, 0:1], scalar2=None,
                                        op0=ALU.is_equal), R=["coli", "rowi"], W=["ident"])
    op("dve", lambda e: e.tensor_scalar(out=Ustr[:], in0=coli[:], scalar1=rowi[:, 0:1], scalar2=None,
                                        op0=ALU.is_gt), R=["coli", "rowi"], W=["Ustr"])
    op("dve", lambda e: e.memset(ones_bf[:], 1.0), W=["ones_bf"])
    identF = sb(top, "identF", [128, 128], F32)
    destI = sb(top, "destI", [128, NT, 8], I32)
    wk = sb(top, "wk", [128, NT, 8], F32)
    eps_t = sb(top, "eps_t", [128, 2], F32)
    op("dve", lambda e: e.memset(eps_t[:, 0:1], LN_EPS), W=["eps_t"])
    op("dve", lambda e: e.memset(eps_t[:, 1:2], RMS_EPS), R=["eps_t"], W=["eps_t"])
    op("dve", lambda e: e.tensor_scalar(out=identF[:], in0=coli[:], scalar1=rowi[:, 0:1], scalar2=None,
                                        op0=ALU.is_equal), R=["coli", "rowi"], W=["identF"])
    op("pool", lambda e: e.iota(ctmp[:], pattern=[[32, 4], [-16, 2], [1, 16]], base=16, channel_multiplier=0,
                                allow_small_or_imprecise_dtypes=True), W=["ctmp"])
    op("dve", lambda e: e.tensor_scalar(out=Pm[:], in0=ctmp[:], scalar1=rowi[:, 0:1], scalar2=None,
                                        op0=ALU.is_equal), R=["ctmp", "rowi"], W=["Pm"])

    op("dve", lambda e: e.memset(destI[:], 0), W=["destI"])
    dzero = sb(top, "dzero", [128, 64], BF16)
    op("dve", lambda e: e.memset(dzero[:], 0.0), W=["dzero"])
    dma("pool", lambda e: e.indirect_dma_start(out=Xg[:, 0:64], out_offset=bass.IndirectOffsetOnAxis(ap=destI[:, 0, 0:1], axis=0),
                                               in_=dzero[:, :], in_offset=None, bounds_check=bc_reg, oob_is_err=False), R=["destI", "dzero"], W=["Xg"])

    with ExitStack() as ph:
        tok = sb(ph, "r_tok", [128, NLAT], F32)
        colp = sb(ph, "r_col", [128, NLAT], F32)
        ang = sb(ph, "r_ang", [128, NLAT], F32)
        tb = sb(ph, "r_tb", [128, NLAT], F32)
        ti = sb(ph, "r_ti", [128, NLAT], I32)
        pv = sb(ph, "r_pv", [128, 8], F32)
        pat = sb(ph, "r_pat", [128, 128], F32)

        def per_part(pattern, base, col):
            op("pool", lambda e: e.iota(pat[:], pattern=pattern, base=base, channel_multiplier=0,
                                        allow_small_or_imprecise_dtypes=True), W=["r_pat"])
            op("dve", lambda e: e.tensor_tensor(out=pat[:], in0=pat[:], in1=identF[:], op=ALU.mult), R=["r_pat", "identF"], W=["r_pat"])
            op("dve", lambda e: e.reduce_sum(out=pv[:, col:col + 1], in_=pat[:], axis=AX.X), R=["r_pat", "r_pv"], W=["r_pv"])

        per_part([[0, 8], [1, 16]], 0, 0)
        per_part([[0, 2], [1, 2], [0, 32]], 0, 2)
        per_part([[0, 4], [2, 2], [0, 16]], -1, 3)
        op("act", lambda e: e.activation(out=pv[:, 1:2], in_=pv[:, 0:1], func=AF.Exp, scale=-math.log(10000.0) / 16.0), R=["r_pv"], W=["r_pv"])
        op("pool", lambda e: e.iota(tok[:], pattern=[[1, 64], [0, 64]], base=0, channel_multiplier=0,
                                    allow_small_or_imprecise_dtypes=True), W=["r_tok"])
        op("pool", lambda e: e.iota(colp[:], pattern=[[0, 64], [1, 64]], base=0, channel_multiplier=0,
                                    allow_small_or_imprecise_dtypes=True), W=["r_col"])
        op("dve", lambda e: e.tensor_tensor(out=colp[:], in0=colp[:], in1=tok[:], op=ALU.subtract), R=["r_tok", "r_col"], W=["r_col"])
        op("dve", lambda e: e.scalar_tensor_tensor(out=ang[:], in0=colp[:], scalar=pv[:, 2:3], in1=tok[:], op0=ALU.mult, op1=ALU.add),
           R=["r_col", "r_tok", "r_pv"], W=["r_ang"])
        op("dve", lambda e: e.tensor_scalar(out=ang[:], in0=ang[:], scalar1=pv[:, 1:2], scalar2=None, op0=ALU.mult), R=["r_ang", "r_pv"], W=["r_ang"])

        def sin_of(src, key_src, dst, key_dst, shift):
            if shift != 0.0:
                op("dve", lambda e: e.tensor_scalar(out=dst, in0=src, scalar1=shift, scalar2=None, op0=ALU.add), R=[key_src], W=[key_dst])
                src, key_src = dst, key_dst
            op("dve", lambda e: e.tensor_scalar(out=tb[:], in0=src, scalar1=1.0 / (2 * math.pi), scalar2=None, op0=ALU.mult), R=[key_src], W=["r_tb"])
            op("dve", lambda e: e.tensor_copy(out=ti[:], in_=tb[:]), R=["r_tb"], W=["r_ti"])
            op("dve", lambda e: e.tensor_copy(out=tb[:], in_=ti[:]), R=["r_ti"], W=["r_tb"])
            op("dve", lambda e: e.scalar_tensor_tensor(out=dst, in0=tb[:], scalar=-2 * math.pi, in1=src, op0=ALU.mult, op1=ALU.add), R=["r_tb", key_src], W=[key_dst])
            op("act", lambda e: e.activation(out=dst, in_=dst, func=AF.Sin), R=[key_dst], W=[key_dst])

        sin_of(ang[:], "r_ang", colp[:], "r_col", 0.0)
        op("dve", lambda e: e.tensor_scalar(out=colp[:], in0=colp[:], scalar1=pv[:, 3:4], scalar2=None, op0=ALU.mult), R=["r_col", "r_pv"], W=["r_col"])
        dma("sp", lambda e: e.dma_start(out=sin_d, in_=colp[:]), R=["r_col"], W=["sin_d"])
        sin_of(ang[:], "r_ang", tok[:], "r_tok", 0.5 * math.pi)
        dma("sp", lambda e: e.dma_start(out=cos_d, in_=tok[:]), R=["r_tok"], W=["cos_d"])
        Bd.barrier()

    def bcast_load(q, dst, src_row, n):
        return lambda e: e.dma_start(out=dst, in_=src_row.partition_broadcast(128))

    def layer_norm_tile(xt, key_x, out_ap, key_out, scr, key_scr, st, key_st, n=D, mul_b=None, add_b=None, keys_b=()):
        op("dve", lambda e: e.reduce_sum(out=st[:, 0:1], in_=xt, axis=AX.X), R=[key_x], W=[key_st])
        op("act", lambda e: e.activation(out=scr, in_=xt, func=AF.Square), R=[key_x], W=[key_scr])
        op("dve", lambda e: e.reduce_sum(out=st[:, 1:2], in_=scr, axis=AX.X), R=[key_scr, key_st], W=[key_st])
        op("dve", lambda e: e.tensor_scalar(out=st[:, 2:3], in0=st[:, 0:1], scalar1=1.0 / n, scalar2=None, op0=ALU.mult), R=[key_st], W=[key_st])
        op("dve", lambda e: e.tensor_tensor(out=st[:, 3:4], in0=st[:, 2:3], in1=st[:, 2:3], op=ALU.mult), R=[key_st], W=[key_st])
        op("dve", lambda e: e.scalar_tensor_tensor(out=st[:, 4:5], in0=st[:, 1:2], scalar=1.0 / n, in1=st[:, 3:4], op0=ALU.mult, op1=ALU.subtract), R=[key_st], W=[key_st])
        op("act", lambda e: e.activation(out=st[:, 5:6], in_=st[:, 4:5], func=AF.Sqrt, bias=eps_t[:, 0:1], scale=1.0), R=[key_st, "eps_t"], W=[key_st])
        op("dve", lambda e: e.reciprocal(out=st[:, 5:6], in_=st[:, 5:6]), R=[key_st], W=[key_st])
        if mul_b is None:
            op("dve", lambda e: e.tensor_scalar(out=out_ap, in0=xt, scalar1=st[:, 2:3], scalar2=st[:, 5:6], op0=ALU.subtract, op1=ALU.mult),
               R=[key_x, key_st], W=[key_out])
        else:
            op("dve", lambda e: e.tensor_scalar(out=scr, in0=xt, scalar1=st[:, 2:3], scalar2=st[:, 5:6], op0=ALU.subtract, op1=ALU.mult),
               R=[key_x, key_st], W=[key_scr])
            op("dve", lambda e: e.tensor_tensor(out=scr, in0=scr, in1=mul_b, op=ALU.mult), R=[key_scr] + list(keys_b), W=[key_scr])
            op("dve", lambda e: e.tensor_tensor(out=out_ap, in0=scr, in1=add_b, op=ALU.add), R=[key_scr] + list(keys_b), W=[key_out])

    def transpose_to(src_bf, key_src, nchunk, dstT, key_dst, tcol, pbank):
        pt = psT[pbank]
        for k in range(nchunk):
            op("pe", lambda e, k=k: e.transpose(out=pt[:, k * 128:(k + 1) * 128], in_=src_bf[:, k * 128:(k + 1) * 128], identity=ident[:]),
               R=[key_src, "ident"], W=[f"psT{pbank}"])
        op("act", lambda e: e.copy(out=dstT[:, 0:nchunk, tcol:tcol + 128],
                                   in_=pt[:, 0:nchunk * 128].rearrange("p (k t) -> p k t", k=nchunk)),
           R=[f"psT{pbank}"], W=[key_dst])

    blocks = [(0, 256)] + [(256 + 512 * i, 512) for i in range(8)]

    for L in range(n_layers):
        last = L == DEPTH - 1
        lam_init = 0.8 - 0.6 * math.exp(-0.3 * L)
        src_lat = x_in if L == 0 else xA[NCTX:T, :]
        src_ctx = ctx_in if L == 0 else xA[0:NCTX, :]

        def src_rows(t0, n):
            return src_ctx[t0:t0 + n, :] if t0 < NCTX else src_lat[t0 - NCTX:t0 - NCTX + n, :]

        with ExitStack() as ph:
            cT = sb(ph, "m_cT", [128, 2, 8], F32)
            crep = sb(ph, "m_crep", [128, 2, 8, 128], BF16)
            wm = [sb(ph, f"m_wm{i}", [128, 8, 512], BF16) for i in range(2)]
            bm = sb(ph, "m_bm", [1, 6144], F32)
            row = sb(ph, "m_row", [1, 2, 6144], F32)
            with nc.allow_non_contiguous_dma(reason="tiny transposed load of c"):
                dma("sp", lambda e: e.dma_start(out=cT[:], in_=c_in.rearrange("r (k p) -> p r k", p=128)), W=["m_cT"])
            dma("sp", lambda e: e.dma_start(out=bm[:], in_=Wd["b_mod"][L:L + 1, :]), W=["m_bm"])
            op("act", lambda e: e.activation(out=cT[:], in_=cT[:], func=AF.Silu), R=["m_cT"], W=["m_cT"])
            for r in range(2):
                op("dve", lambda e, r=r: e.tensor_copy(out=crep[:, r, :, :], in_=cT[:, r, :].unsqueeze(2).to_broadcast([128, 8, 128])),
                   R=["m_cT"], W=["m_crep"])
            for ch in range(12):
                w = wm[ch % 2]
                dma("pool", lambda e, w=w, ch=ch: e.dma_start(out=w[:], in_=Wd["w_mod"][L, :, ch * 512:(ch + 1) * 512].rearrange("(k p) n -> p k n", p=128)),
                    W=[f"m_wm{ch % 2}"])
                for r in range(2):
                    ps = psA[(ch * 2 + r) % 4]
                    for k in range(8):
                        op("pe", lambda e, ps=ps, k=k, r=r, w=w: e.matmul(ps[:], lhsT=crep[:, r, k, :], rhs=w[:, k, :], start=(k == 0), stop=(k == 7)),
                           R=["m_crep", f"m_wm{ch % 2}"], W=[f"psA{(ch * 2 + r) % 4}"])
                    op("dve", lambda e, ps=ps, r=r, ch=ch: e.tensor_tensor(out=row[0:1, r, ch * 512:(ch + 1) * 512], in0=ps[0:1, :], in1=bm[0:1, ch * 512:(ch + 1) * 512], op=ALU.add),
                       R=[f"psA{(ch * 2 + r) % 4}", "m_bm"], W=["m_row"])
            for seg in (1, 4):
                op("dve", lambda e, seg=seg: e.tensor_scalar(out=row[0:1, :, seg * 1024:(seg + 1) * 1024], in0=row[0:1, :, seg * 1024:(seg + 1) * 1024],
                                                             scalar1=1.0, scalar2=None, op0=ALU.add), R=["m_row"], W=["m_row"])
            dma("sp", lambda e: e.dma_start(out=modv.rearrange("(o r) n -> o r n", o=1), in_=row[:]), R=["m_row"], W=["modv"])
            Bd.barrier()

        def mod_b(r, seg):
            return modv[r, seg * 1024:(seg + 1) * 1024]

        with ExitStack() as ph:
            win = sb(ph, "a_win", [128, 8, INC], BF16)
            for k in range(8):
                dma("pool", lambda e, k=k: e.dma_start(out=win[:, k, :], in_=Wd["w_in"][L, k * 128:(k + 1) * 128, :]), W=["a_win"])
            binT = sb(ph, "a_binT", [128, 52], F32)
            with nc.allow_non_contiguous_dma(reason="tiny transposed bias load"):
                dma("sp", lambda e: e.dma_start(out=binT[:], in_=Wd["b_in"][L, :].rearrange("(j p) -> p j", p=128)), W=["a_binT"])
            bvus = sb(ph, "a_bvus", [128, 1536], F32)
            dma("sp", bcast_load("sp", bvus[:], Wd["b_in"][L, OV:OV + 1536], 1536), W=["a_bvus"])
            modb = sb(ph, "a_modb", [128, 4, D], F32)
            for r in range(2):
                for j, seg in enumerate((0, 1)):
                    dma("sp", bcast_load("sp", modb[:, r * 2 + j, :], mod_b(r, seg), D), R=["modv"], W=["a_modb"])
            lng = sb(ph, "a_lng", [128, 2, 512], F32)
            dma("sp", bcast_load("sp", lng[:, 0, :], Wd["sg_ln_g"][L, :], 512), W=["a_lng"])
            dma("sp", bcast_load("sp", lng[:, 1, :], Wd["sg_ln_b"][L, :], 512), W=["a_lng"])
            wsf = sb(ph, "a_wsf", [128, 4, 128], F32)
            wsb = sb(ph, "a_wsb", [128, 4, 128], BF16)
            wsT = sb(ph, "a_wsT", [128, 4, 128], BF16)
            bsT = sb(ph, "a_bsT", [128, 4], F32)
            dma("sp", lambda e: e.dma_start(out=wsf[:], in_=Wd["sg_w"][L].rearrange("g p q -> p g q")), W=["a_wsf"])
            with nc.allow_non_contiguous_dma(reason="tiny transposed bias load"):
                dma("sp", lambda e: e.dma_start(out=bsT[:], in_=Wd["sg_b"][L].rearrange("g p -> p g")), W=["a_bsT"])
            op("dve", lambda e: e.tensor_copy(out=wsb[:], in_=wsf[:]), R=["a_wsf"], W=["a_wsb"])
            for g in range(4):
                op("pe", lambda e, g=g: e.transpose(out=psT[0][:, g * 128:(g + 1) * 128], in_=wsb[:, g, :], identity=ident[:]), R=["a_wsb", "ident"], W=["psT0"])
            op("act", lambda e: e.copy(out=wsT[:], in_=psT[0][:, 0:512].rearrange("p (g t) -> p g t", g=4)), R=["psT0"], W=["a_wsT"])

            xt = [sb(ph, f"a_xt{i}", [128, D], F32) for i in range(2)]
            scr = sb(ph, "a_scr", [128, D], F32)
            st = sb(ph, "a_st", [128, 8], F32)
            hb = sb(ph, "a_hb", [128, D], BF16)
            hT = sb(ph, "a_hT", [128, 8, 512], BF16)
            fo = [sb(ph, f"a_fo{i}", [128, 512], F32) for i in range(2)]
            fob = [sb(ph, f"a_fob{i}", [128, 512], BF16) for i in range(2)]
            rp = sb(ph, "a_rp", [128, 512], F32)
            cs = sb(ph, "a_cs", [128, 2, 512], F32)
            tmv = sb(ph, "a_tmv", [128, 512], F32)
            tmb = sb(ph, "a_tmb", [128, 512], BF16)
            gu = sb(ph, "a_gu", [128, 512], F32)
            gs_ = sb(ph, "a_gs", [128, 512], F32)
            gsq = sb(ph, "a_gsq", [128, 512], F32)
            gst = sb(ph, "a_gst", [128, 4, 4], F32)
            vgb = sb(ph, "a_vgb", [128, 512], BF16)
            ob = sb(ph, "a_ob", [128, 512], BF16)
            obT = sb(ph, "a_obT", [128, 4, 512], BF16)

            fcnt = [0]
            for (t0, nb) in blocks:
                is_ctx = t0 < NCTX
                mr = 1 if is_ctx else 0
                nsub = nb // 128
                for s_ in range(nsub):
                    xx = xt[s_ % 2]
                    kx = f"a_xt{s_ % 2}"
                    dma("sp", lambda e, xx=xx, s_=s_: e.dma_start(out=xx[:], in_=src_rows(t0 + s_ * 128, 128)), W=[kx])
                    layer_norm_tile(xx[:], kx, hb[:], "a_hb", scr[:], "a_scr", st, "a_st",
                                    mul_b=modb[:, mr * 2 + 1, :], add_b=modb[:, mr * 2, :], keys_b=["a_modb"])
                    transpose_to(hb, "a_hb", 8, hT, "a_hT", s_ * 128, s_ % 2)
                if not is_ctx:
                    l0 = t0 - NCTX
                    dma("sp", lambda e: e.dma_start(out=cs[:, 0, :], in_=cos_d[:, l0:l0 + 512]), R=["cos_d"], W=["a_cs"])
                    dma("sp", lambda e: e.dma_start(out=cs[:, 1, :], in_=sin_d[:, l0:l0 + 512]), R=["sin_d"], W=["a_cs"])
                fm_tiles = [("q", j) for j in range(4)] + [("k", j) for j in range(4)] + [("x", j) for j in range(4)] + \
                           [("y", j) for j in range(4)] + [("g", j) for j in range(24)]
                for kind, j in fm_tiles:
                    col0 = {"q": OQ, "k": OK_, "x": OX, "y": OY, "g": OG}[kind] + j * 128
                    jt = col0 // 128
                    pi = fcnt[0] % 4
                    fi = fcnt[0] % 2
                    fcnt[0] += 1
                    ps = psA[pi]
                    for k in range(8):
                        op("pe", lambda e, ps=ps, k=k, col0=col0: e.matmul(ps[:, 0:nb], lhsT=win[:, k, col0:col0 + 128], rhs=hT[:, k, 0:nb], start=(k == 0), stop=(k == 7)),
                           R=["a_win", "a_hT"], W=[f"psA{pi}"])
                    if kind in ("q", "k"):
                        dst = qT_d if kind == "q" else kT_d
                        if is_ctx:
                            op("act", lambda e, ps=ps, fi=fi, jt=jt: e.activation(out=fob[fi][:, 0:nb], in_=ps[:, 0:nb], func=AF.Identity, bias=binT[:, jt:jt + 1], scale=1.0),
                               R=[f"psA{pi}", "a_binT"], W=[f"a_fob{fi}"])
                            dma("sp", lambda e, fi=fi, dst=dst, j=j: e.dma_start(out=dst[j, :, t0:t0 + nb], in_=fob[fi][:, 0:nb]), R=[f"a_fob{fi}"], W=[kind + "T_d"])
                        else:
                            op("act", lambda e, ps=ps, fi=fi, jt=jt: e.activation(out=fo[fi][:, 0:nb], in_=ps[:, 0:nb], func=AF.Identity, bias=binT[:, jt:jt + 1], scale=1.0),
                               R=[f"psA{pi}", "a_binT"], W=[f"a_fo{fi}"])
                            op("dve", lambda e, fi=fi: e.tensor_copy(out=fob[fi][:, 0:nb], in_=fo[fi][:, 0:nb]), R=[f"a_fo{fi}"], W=[f"a_fob{fi}"])
                            op("pe", lambda e, fi=fi: e.matmul(psS[0][:, 0:nb], lhsT=Pm[:], rhs=fob[fi][:, 0:nb], start=True, stop=True),
                               R=["Pm", f"a_fob{fi}"], W=["psS0"])
                            op("dve", lambda e: e.tensor_tensor(out=rp[:, 0:nb], in0=psS[0][:, 0:nb], in1=cs[:, 1, 0:nb], op=ALU.mult), R=["psS0", "a_cs"], W=["a_rp"])
                            op("dve", lambda e, fi=fi: e.tensor_tensor(out=fo[fi][:, 0:nb], in0=fo[fi][:, 0:nb], in1=cs[:, 0, 0:nb], op=ALU.mult), R=[f"a_fo{fi}", "a_cs"], W=[f"a_fo{fi}"])
                            op("dve", lambda e, fi=fi: e.tensor_tensor(out=fob[fi][:, 0:nb], in0=fo[fi][:, 0:nb], in1=rp[:, 0:nb], op=ALU.add), R=[f"a_fo{fi}", "a_rp"], W=[f"a_fob{fi}"])
                            dma("sp", lambda e, fi=fi, dst=dst, j=j: e.dma_start(out=dst[j, :, t0:t0 + nb], in_=fob[fi][:, 0:nb]), R=[f"a_fob{fi}"], W=[kind + "T_d"])
                    elif kind == "x":
                        op("act", lambda e, ps=ps, fi=fi, jt=jt: e.activation(out=fo[fi][:, 0:nb], in_=ps[:, 0:nb], func=AF.Identity, bias=binT[:, jt:jt + 1], scale=1.0),
                           R=[f"psA{pi}", "a_binT"], W=[f"a_fo{fi}"])
                        dma("sp", lambda e, fi=fi, j=j: e.dma_start(out=xT_d[j * 128:(j + 1) * 128, t0:t0 + nb], in_=fo[fi][:, 0:nb]), R=[f"a_fo{fi}"], W=["xT_d"])
                    elif kind == "y":
                        op("act", lambda e, ps=ps, fi=fi, jt=jt: e.activation(out=fob[fi][:, 0:nb], in_=ps[:, 0:nb], func=AF.Gelu_apprx_tanh, bias=binT[:, jt:jt + 1], scale=1.0),
                           R=[f"psA{pi}", "a_binT"], W=[f"a_fob{fi}"])
                        dma("sp", lambda e, fi=fi, j=j: e.dma_start(out=yT_d[j * 128:(j + 1) * 128, t0:t0 + nb], in_=fob[fi][:, 0:nb]), R=[f"a_fob{fi}"], W=["yT_d"])
                    else:
                        op("act", lambda e, ps=ps, fi=fi, jt=jt: e.activation(out=fob[fi][:, 0:nb], in_=ps[:, 0:nb], func=AF.Sigmoid, bias=binT[:, jt:jt + 1], scale=1.0),
                           R=[f"psA{pi}", "a_binT"], W=[f"a_fob{fi}"])
                        dma("sp", lambda e, fi=fi, j=j: e.dma_start(out=gT_d[j * 128:(j + 1) * 128, t0:t0 + nb], in_=fob[fi][:, 0:nb]), R=[f"a_fob{fi}"], W=["gT_d"])
                for s_ in range(nsub):
                    tt = t0 + s_ * 128
                    for wi, (kind, col0) in enumerate((("v", OV), ("u", OU), ("s", OS))):
                        pi = fcnt[0] % 4
                        fcnt[0] += 1
                        ps = psA[pi]
                        for k in range(8):
                            op("pe", lambda e, ps=ps, k=k, col0=col0, s_=s_: e.matmul(ps[:], lhsT=hT[:, k, s_ * 128:(s_ + 1) * 128], rhs=win[:, k, col0:col0 + 512], start=(k == 0), stop=(k == 7)),
                               R=["a_win", "a_hT"], W=[f"psA{pi}"])
                        if kind == "v":
                            op("dve", lambda e, ps=ps: e.tensor_tensor(out=tmb[:], in0=ps[:], in1=bvus[:, 0:512], op=ALU.add), R=[f"psA{pi}", "a_bvus"], W=["a_tmb"])
                            dma("sp", lambda e, tt=tt: e.dma_start(out=v_d[tt:tt + 128, :], in_=tmb[:]), R=["a_tmb"], W=["v_d"])
                        elif kind == "u":
                            op("dve", lambda e, ps=ps: e.tensor_tensor(out=tmv[:], in0=ps[:], in1=bvus[:, 512:1024], op=ALU.add), R=[f"psA{pi}", "a_bvus"], W=["a_tmv"])
                            op("act", lambda e: e.activation(out=gu[:], in_=tmv[:], func=AF.Gelu_apprx_tanh), R=["a_tmv"], W=["a_gu"])
                        else:
                            op("dve", lambda e, ps=ps: e.tensor_tensor(out=tmv[:], in0=ps[:], in1=bvus[:, 1024:1536], op=ALU.add), R=[f"psA{pi}", "a_bvus"], W=["a_tmv"])
                            op("act", lambda e: e.activation(out=gs_[:], in_=tmv[:], func=AF.Gelu_apprx_tanh), R=["a_tmv"], W=["a_gs"])
                    g3 = gs_[:].rearrange("p (g c) -> p g c", g=4)
                    op("dve", lambda e: e.reduce_sum(out=gst[:, 0, :], in_=g3, axis=AX.X), R=["a_gs"], W=["a_gst"])
                    op("act", lambda e: e.activation(out=gsq[:], in_=gs_[:], func=AF.Square), R=["a_gs"], W=["a_gsq"])
                    op("dve", lambda e: e.reduce_sum(out=gst[:, 1, :], in_=gsq[:].rearrange("p (g c) -> p g c", g=4), axis=AX.X), R=["a_gsq", "a_gst"], W=["a_gst"])
                    op("dve", lambda e: e.tensor_scalar(out=gst[:, 0, :], in0=gst[:, 0, :], scalar1=1.0 / 128, scalar2=None, op0=ALU.mult), R=["a_gst"], W=["a_gst"])
                    op("dve", lambda e: e.tensor_tensor(out=gst[:, 2, :], in0=gst[:, 0, :], in1=gst[:, 0, :], op=ALU.mult), R=["a_gst"], W=["a_gst"])
                    op("dve", lambda e: e.scalar_tensor_tensor(out=gst[:, 1, :], in0=gst[:, 1, :], scalar=1.0 / 128, in1=gst[:, 2, :], op0=ALU.mult, op1=ALU.subtract), R=["a_gst"], W=["a_gst"])
                    op("act", lambda e: e.activation(out=gst[:, 3, :], in_=gst[:, 1, :], func=AF.Sqrt, bias=eps_t[:, 0:1], scale=1.0), R=["a_gst", "eps_t"], W=["a_gst"])
                    op("dve", lambda e: e.reciprocal(out=gst[:, 3, :], in_=gst[:, 3, :]), R=["a_gst"], W=["a_gst"])
                    op("dve", lambda e: e.tensor_tensor(out=gsq[:].rearrange("p (g c) -> p g c", g=4), in0=g3, in1=gst[:, 0, :].unsqueeze(2).to_broadcast([128, 4, 128]), op=ALU.subtract),
                       R=["a_gs", "a_gst"], W=["a_gsq"])
                    op("dve", lambda e: e.tensor_tensor(out=gsq[:].rearrange("p (g c) -> p g c", g=4), in0=gsq[:].rearrange("p (g c) -> p g c", g=4),
                                                        in1=gst[:, 3, :].unsqueeze(2).to_broadcast([128, 4, 128]), op=ALU.mult), R=["a_gsq", "a_gst"], W=["a_gsq"])
                    op("dve", lambda e: e.tensor_tensor(out=gsq[:], in0=gsq[:], in1=lng[:, 0, :], op=ALU.mult), R=["a_gsq", "a_lng"], W=["a_gsq"])
                    op("dve", lambda e: e.tensor_tensor(out=vgb[:], in0=gsq[:], in1=lng[:, 1, :], op=ALU.add), R=["a_gsq", "a_lng"], W=["a_vgb"])
                    pi = fcnt[0] % 4
                    fcnt[0] += 1
                    ps = psA[pi]
                    for g in range(4):
                        op("pe", lambda e, ps=ps, g=g: e.matmul(ps[:, g * 128:(g + 1) * 128], lhsT=wsT[:, g, :], rhs=vgb[:, g * 128:(g + 1) * 128], start=True, stop=True),
                           R=["a_wsT", "a_vgb"], W=[f"psA{pi}"])
                    for g in range(4):
                        op("dve", lambda e, ps=ps, g=g: e.scalar_tensor_tensor(out=ob[:, g * 128:(g + 1) * 128], in0=ps[:, g * 128:(g + 1) * 128], scalar=bsT[:, g:g + 1],
                                                                               in1=gu[:, g * 128:(g + 1) * 128], op0=ALU.add, op1=ALU.mult),
                           R=[f"psA{pi}", "a_bsT", "a_gu"], W=["a_ob"])
                    transpose_to(ob, "a_ob", 4, obT, "a_obT", s_ * 128, s_ % 2)
                for cc in range(4):
                    dma("sp", lambda e, cc=cc: e.dma_start(out=obT_d[cc * 128:(cc + 1) * 128, t0:t0 + nb], in_=obT[:, cc, 0:nb]), R=["a_obT"], W=["obT_d"])
            Bd.barrier()

        if "stopA" in debug:
            break

        with ExitStack() as ph:
            xp = sb(ph, "b_xp", [128, T + 8], F32)
            xs = sb(ph, "b_xs", [128, T], F32)
            xsb = sb(ph, "b_xsb", [128, T], BF16)
            r_ = sb(ph, "b_r", [128, T], F32)
            i_ = sb(ph, "b_i", [128, T], F32)
            a2 = sb(ph, "b_a2", [128, T], F32)
            hf = sb(ph, "b_hf", [128, T], F32)
            hb_ = sb(ph, "b_hb", [128, T], F32)
            yb = sb(ph, "b_yb", [128, T], BF16)
            oc = sb(ph, "b_oc", [128, T], BF16)
            cw = sb(ph, "b_cw", [128, 4, 4], F32)
            cb = sb(ph, "b_cb", [128, 4], F32)
            gb = sb(ph, "b_gb", [128, 3, 2, 4], F32)
            cn = sb(ph, "b_cn", [128, 2, 2, 4], F32)
            one_t = sb(ph, "b_one", [128, 1], F32)
            wst = sb(ph, "b_wst", [128, 16, 128], F32)
            wbd = sb(ph, "b_wbd", [128, 16, 128], BF16)
            op("dve", lambda e: e.memset(xp[:], 0.0), W=["b_xp"])
            op("dve", lambda e: e.memset(one_t[:], 1.0), W=["b_one"])
            op("pool", lambda e: e.memset(wst[:], 0.0), W=["b_wst"])
            with nc.allow_non_contiguous_dma(reason="tiny per-channel parameter loads"):
                for j in range(4):
                    dma("sp", lambda e, j=j: e.dma_start(out=cw[:, :, j], in_=Wd["conv_w"][L, j].rearrange("(ct p) -> p ct", p=128)), R=["b_cw"], W=["b_cw"])
                dma("sp", lambda e: e.dma_start(out=cb[:], in_=Wd["conv_b"][L].rearrange("(ct p) -> p ct", p=128)), W=["b_cb"])
                for wi, nm in enumerate(("lru_ba", "lru_bx", "lru_lam")):
                    for d in range(2):
                        dma("sp", lambda e, wi=wi, nm=nm, d=d: e.dma_start(out=gb[:, wi, d, :], in_=Wd[nm][L, d].rearrange("(ct p) -> p ct", p=128)), R=["b_gb"], W=["b_gb"])
            for d in range(2):
                for gi, nm in enumerate(("lru_wa", "lru_wx")):
                    for ct in range(4):
                        idx = (d * 2 + gi) * 4 + ct
                        for hh in range(2):
                            dma("sp", lambda e, idx=idx, hh=hh, nm=nm, d=d, ct=ct: e.dma_start(
                                out=wst[hh * 64:(hh + 1) * 64, idx, hh * 64:(hh + 1) * 64], in_=Wd[nm][L, d, 2 * ct + hh]), R=["b_wst"], W=["b_wst"])
            op("dve", lambda e: e.tensor_copy(out=wbd[:], in_=wst[:]), R=["b_wst"], W=["b_wbd"])
            op("act", lambda e: e.activation(out=cn[:, 0, :, :], in_=gb[:, 2, :, :], func=AF.Exp, scale=-1.0), R=["b_gb"], W=["b_cn"])
            op("act", lambda e: e.activation(out=cn[:, 0, :, :], in_=cn[:, 0, :, :], func=AF.Ln, bias=one_t[:, 0:1], scale=1.0), R=["b_cn", "b_one"], W=["b_cn"])
            op("dve", lambda e: e.tensor_scalar(out=cn[:, 1, :, :], in0=cn[:, 0, :, :], scalar1=-16.0, scalar2=None, op0=ALU.mult), R=["b_cn"], W=["b_cn"])
            op("dve", lambda e: e.tensor_scalar(out=cn[:, 0, :, :], in0=cn[:, 0, :, :], scalar1=-8.0, scalar2=None, op0=ALU.mult), R=["b_cn"], W=["b_cn"])
            chunks = [(i * 512, 512) for i in range(8)] + [(4096, 256)]
            pcn = [0]
            for ct in range(4):
                dma("sp", lambda e, ct=ct: e.dma_start(out=xp[:, 2:2 + NCTX], in_=xT_d[ct * 128:(ct + 1) * 128, 0:NCTX]), R=["xT_d"], W=["b_xp"])
                dma("sp", lambda e, ct=ct: e.dma_start(out=xp[:, 6 + NCTX:6 + T], in_=xT_d[ct * 128:(ct + 1) * 128, NCTX:T]), R=["xT_d", "b_xp"], W=["b_xp"])
                dma("sp", lambda e, ct=ct: e.dma_start(out=yb[:], in_=yT_d[ct * 128:(ct + 1) * 128, :]), R=["yT_d"], W=["b_yb"])
                for (base, n, o0) in ((0, NCTX, 0), (4 + NCTX, NLAT, NCTX)):
                    op("dve", lambda e, base=base, n=n, o0=o0, ct=ct: e.tensor_scalar(out=xs[:, o0:o0 + n], in0=xp[:, base:base + n], scalar1=cw[:, ct, 0:1], scalar2=cb[:, ct:ct + 1],
                                                                                  op0=ALU.mult, op1=ALU.add), R=["b_xp", "b_cw", "b_cb"], W=["b_xs"])
                    for j in range(1, 4):
                        op("dve", lambda e, base=base, n=n, o0=o0, ct=ct, j=j: e.scalar_tensor_tensor(out=xs[:, o0:o0 + n], in0=xp[:, base + j:base + j + n], scalar=cw[:, ct, j:j + 1],
                                                                                                   in1=xs[:, o0:o0 + n], op0=ALU.mult, op1=ALU.add), R=["b_xp", "b_cw", "b_xs"], W=["b_xs"])
                op("act", lambda e: e.copy(out=xsb[:], in_=xs[:]), R=["b_xs"], W=["b_xsb"])
                for d in range(2):
                    for gi, (gt, gk) in enumerate(((r_, "b_r"), (i_, "b_i"))):
                        idx = (d * 2 + gi) * 4 + ct
                        for (c0, cnb) in chunks:
                            pi = pcn[0] % 4
                            pcn[0] += 1
                            op("pe", lambda e, pi=pi, idx=idx, c0=c0, cnb=cnb: e.matmul(psA[pi][:, 0:cnb], lhsT=wbd[:, idx, :], rhs=xsb[:, c0:c0 + cnb], start=True, stop=True),
                               R=["b_wbd", "b_xsb"], W=[f"psA{pi}"])
                            op("act", lambda e, pi=pi, gt=gt, gi=gi, d=d, ct=ct, c0=c0, cnb=cnb: e.activation(out=gt[:, c0:c0 + cnb], in_=psA[pi][:, 0:cnb], func=AF.Sigmoid,
                                                                                                       bias=gb[:, gi, d, ct:ct + 1], scale=1.0), R=[f"psA{pi}", "b_gb"], W=[gk])
                    op("act", lambda e, d=d, ct=ct: e.activation(out=a2[:], in_=r_[:], func=AF.Exp, scale=cn[:, 1, d, ct:ct + 1]), R=["b_r", "b_cn"], W=["b_a2"])
                    op("act", lambda e, d=d, ct=ct: e.activation(out=r_[:], in_=r_[:], func=AF.Exp, scale=cn[:, 0, d, ct:ct + 1]), R=["b_r", "b_cn"], W=["b_r"])
                    op("act", lambda e: e.activation(out=a2[:], in_=a2[:], func=AF.Sqrt, bias=one_t[:, 0:1], scale=-1.0), R=["b_a2", "b_one"], W=["b_a2"])
                    op("dve", lambda e: e.tensor_tensor(out=i_[:], in0=i_[:], in1=xs[:], op=ALU.mult), R=["b_i", "b_xs"], W=["b_i"])
                    op("dve", lambda e: e.tensor_tensor(out=i_[:], in0=i_[:], in1=a2[:], op=ALU.mult), R=["b_i", "b_a2"], W=["b_i"])
                    if d == 0:
                        op("dve", lambda e: e.tensor_tensor_scan(out=hf[:], data0=r_[:], data1=i_[:], initial=0.0, op0=ALU.mult, op1=ALU.add), R=["b_r", "b_i"], W=["b_hf"])
                    else:
                        op("dve", lambda e: e.tensor_tensor_scan(out=hb_[:, 0:NCTX][:, ::-1], data0=r_[:, 0:NCTX][:, ::-1], data1=i_[:, 0:NCTX][:, ::-1], initial=0.0,
                                                                 op0=ALU.mult, op1=ALU.add), R=["b_r", "b_i"], W=["b_hb"])
                        op("dve", lambda e: e.tensor_tensor_scan(out=hb_[:, NCTX:T][:, ::-1], data0=r_[:, NCTX:T][:, ::-1], data1=i_[:, NCTX:T][:, ::-1], initial=hb_[:, 0:1],
                                                                 op0=ALU.mult, op1=ALU.add), R=["b_r", "b_i", "b_hb"], W=["b_hb"])
                op("dve", lambda e: e.tensor_tensor(out=hf[:], in0=hf[:], in1=hb_[:], op=ALU.add), R=["b_hf", "b_hb"], W=["b_hf"])
                op("dve", lambda e: e.tensor_tensor(out=oc[:], in0=hf[:], in1=yb[:], op=ALU.mult), R=["b_hf", "b_yb"], W=["b_oc"])
                dma("sp", lambda e, ct=ct: e.dma_start(out=ocT_d[ct * 128:(ct + 1) * 128, :], in_=oc[:]), R=["b_oc"], W=["ocT_d"])
            Bd.barrier()

        if "stopB" in debug:
            break

        with ExitStack() as ph:
            KT = sb(ph, "c_KT", [128, 4, T], BF16)
            VA = sb(ph, "c_VA", [128, NT, 4, 132], BF16)
            QT = [sb(ph, f"c_QT{i}", [128, 512], BF16) for i in range(2)]
            Pb = [sb(ph, f"c_P{i}", [128, 512], BF16) for i in range(3)]
            lq = sb(ph, "c_lq", [128, 4, 64], F32)
            lamt = sb(ph, "c_lamt", [128, 4], F32)
            gain = sb(ph, "c_gain", [128, 512], F32)
            sm = sb(ph, "c_sm", [128, 8], F32)
            o1 = sb(ph, "c_o1", [128, 128], F32)
            o2 = sb(ph, "c_o2", [128, 128], F32)
            oab = sb(ph, "c_oab", [128, 128], BF16)
            oaT = sb(ph, "c_oaT", [128, 4, 512], BF16)
            op("dve", lambda e: e.memset(VA[:], 1.0), W=["c_VA"])
            for h in range(4):
                dma("sp", lambda e, h=h: e.dma_start(out=KT[:, h, :], in_=kT_d[h]), R=["kT_d"], W=["c_KT"])
            for kt in range(NT):
                dma("sp", lambda e, kt=kt: e.dma_start(out=VA[:, kt, :, 0:128], in_=v_d[kt * 128:(kt + 1) * 128, :].rearrange("p (h e) -> p h e", h=4)), R=["v_d", "c_VA"], W=["c_VA"])
            for i, nm in enumerate(("lam_q1", "lam_k1", "lam_q2", "lam_k2")):
                dma("sp", bcast_load("sp", lq[:, i, :], Wd[nm][L, :], 64), R=["c_lq"], W=["c_lq"])
            dma("sp", bcast_load("sp", gain[:], Wd["attn_norm_g"][L].rearrange("h e -> (h e)"), 512), W=["c_gain"])
            op("dve", lambda e: e.tensor_scalar(out=gain[:], in0=gain[:], scalar1=(1.0 - lam_init), scalar2=None, op0=ALU.mult), R=["c_gain"], W=["c_gain"])
            for i in range(2):
                op("dve", lambda e, i=i: e.tensor_tensor(out=lq[:, 2 * i, :], in0=lq[:, 2 * i, :], in1=lq[:, 2 * i + 1, :], op=ALU.mult), R=["c_lq"], W=["c_lq"])
                op("dve", lambda e, i=i: e.reduce_sum(out=lamt[:, i:i + 1], in_=lq[:, 2 * i, :], axis=AX.X), R=["c_lq", "c_lamt"], W=["c_lamt"])
            op("act", lambda e: e.activation(out=lamt[:, 0:2], in_=lamt[:, 0:2], func=AF.Exp), R=["c_lamt"], W=["c_lamt"])
            op("dve", lambda e: e.tensor_tensor(out=lamt[:, 2:3], in0=lamt[:, 0:1], in1=lamt[:, 1:2], op=ALU.subtract), R=["c_lamt"], W=["c_lamt"])
            op("dve", lambda e: e.tensor_scalar(out=lamt[:, 2:3], in0=lamt[:, 2:3], scalar1=lam_init, scalar2=None, op0=ALU.add), R=["c_lamt"], W=["c_lamt"])
            if "lamt" in debug:
                dma("sp", lambda e: e.dma_start(out=dbg_d[0:128, 0:4], in_=lamt[:]), R=["c_lamt"], W=["dbg_d"])

            def acc(c, qs):
                return psA[c * 2 + qs // 2][:, (qs % 2) * 256:(qs % 2) * 256 + 129], f"psA{c * 2 + qs // 2}"

            sc_ = [0]
            qblocks = blocks if not last else blocks[1:]
            for (t0, nb) in qblocks:
                nsub = nb // 128
                key_tiles = list(range(2)) if t0 < NCTX else list(range(NT))
                nkt = len(key_tiles)
                for h in range(4):
                    qi = h % 2
                    dma("sp", lambda e, qi=qi, h=h: e.dma_start(out=QT[qi][:, 0:nb], in_=qT_d[h, :, t0:t0 + nb]), R=["qT_d"], W=[f"c_QT{qi}"])
                    seq = [(c, ki, kt) for c in range(2) for ki, kt in enumerate(key_tiles)]
                    base = sc_[0]
                    sc_[0] += len(seq)

                    def emit_qk(i, qi=qi, h=h, seq=seq, base=base):
                        c, ki, kt = seq[i]
                        si = (base + i) % 2
                        op("pe", lambda e: e.matmul(psS[si][:, 0:nb], lhsT=KT[c * 64:(c + 1) * 64, h, kt * 128:(kt + 1) * 128],
                                                    rhs=QT[qi][c * 64:(c + 1) * 64, 0:nb], start=True, stop=True),
                           R=["c_KT", f"c_QT{qi}"], W=[f"psS{si}"])

                    emit_qk(0)
                    for i in range(len(seq)):
                        c, ki, kt = seq[i]
                        si = (base + i) % 2
                        pj = (base + i) % 3
                        if i + 1 < len(seq):
                            emit_qk(i + 1)
                        op("act", lambda e, si=si, pj=pj: e.activation(out=Pb[pj][:, 0:nb], in_=psS[si][:, 0:nb], func=AF.Exp, scale=0.125), R=[f"psS{si}"], W=[f"c_P{pj}"])
                        for qs in range(nsub):
                            a_ap, a_key = acc(c, qs)
                            op("pe", lambda e, a_ap=a_ap, pj=pj, qs=qs, kt=kt, h=h, ki=ki: e.matmul(a_ap, lhsT=Pb[pj][:, qs * 128:(qs + 1) * 128], rhs=VA[:, kt, h, 0:129],
                                                                                             start=(ki == 0 and qs % 2 == 0), stop=(ki == nkt - 1)),
                               R=[f"c_P{pj}", "c_VA"], W=[a_key])
                    for qs in range(nsub):
                        a0, k0 = acc(0, qs)
                        a1, k1 = acc(1, qs)
                        op("dve", lambda e, a0=a0: e.reciprocal(out=sm[:, 0:1], in_=a0[:, 128:129]), R=[k0], W=["c_sm"])
                        op("dve", lambda e, a1=a1: e.reciprocal(out=sm[:, 1:2], in_=a1[:, 128:129]), R=[k1, "c_sm"], W=["c_sm"])
                        op("dve", lambda e: e.tensor_tensor(out=sm[:, 1:2], in0=sm[:, 1:2], in1=lamt[:, 2:3], op=ALU.mult), R=["c_sm", "c_lamt"], W=["c_sm"])
                        op("dve", lambda e, a1=a1: e.tensor_scalar(out=o2[:], in0=a1[:, 0:128], scalar1=sm[:, 1:2], scalar2=None, op0=ALU.mult), R=[k1, "c_sm"], W=["c_o2"])
                        op("dve", lambda e, a0=a0: e.scalar_tensor_tensor(out=o1[:], in0=a0[:, 0:128], scalar=sm[:, 0:1], in1=o2[:], op0=ALU.mult, op1=ALU.subtract),
                           R=[k0, "c_sm", "c_o2"], W=["c_o1"])
                        op("act", lambda e: e.activation(out=o2[:], in_=o1[:], func=AF.Square), R=["c_o1"], W=["c_o2"])
                        op("dve", lambda e: e.reduce_sum(out=sm[:, 2:3], in_=o2[:], axis=AX.X), R=["c_o2", "c_sm"], W=["c_sm"])
                        op("act", lambda e: e.activation(out=sm[:, 3:4], in_=sm[:, 2:3], func=AF.Sqrt, bias=eps_t[:, 1:2], scale=1.0 / 128), R=["c_sm", "eps_t"], W=["c_sm"])
                        op("dve", lambda e: e.reciprocal(out=sm[:, 3:4], in_=sm[:, 3:4]), R=["c_sm"], W=["c_sm"])
                        op("dve", lambda e, h=h: e.scalar_tensor_tensor(out=oab[:], in0=o1[:], scalar=sm[:, 3:4], in1=gain[:, h * 128:(h + 1) * 128], op0=ALU.mult, op1=ALU.mult),
                           R=["c_o1", "c_sm", "c_gain"], W=["c_oab"])
                        op("pe", lambda e: e.transpose(out=psT[0][:, 0:128], in_=oab[:], identity=ident[:]), R=["c_oab", "ident"], W=["psT0"])
                        op("act", lambda e, h=h, qs=qs: e.copy(out=oaT[:, h, qs * 128:(qs + 1) * 128], in_=psT[0][:, 0:128]), R=["psT0"], W=["c_oaT"])
                for h in range(4):
                    dma("sp", lambda e, h=h: e.dma_start(out=oaT_d[h * 128:(h + 1) * 128, t0:t0 + nb], in_=oaT[:, h, 0:nb]), R=["c_oaT"], W=["oaT_d"])
            Bd.barrier()

        if "stopC" in debug:
            break

        with ExitStack() as ph:
            wb = sb(ph, "d_wb", [128, 12, D], BF16)
            wo = sb(ph, "d_wo", [128, 8, D], BF16)
            wr = sb(ph, "d_wr", [128, 8, 64], BF16)
            wsg = sb(ph, "d_wsg", [128, 8, 256], BF16)
            wsu = sb(ph, "d_wsu", [128, 8, 256], BF16)
            wsd = sb(ph, "d_wsd", [128, 2, D], BF16)
            modb2 = sb(ph, "d_modb", [128, 6, D], F32)
            lnb = sb(ph, "d_lnb", [128, 2, D], F32)
            rbias = sb(ph, "d_rbias", [128, 64], F32)
            oT = sb(ph, "d_oT", [128, 3, 4, 512], BF16)
            gT = sb(ph, "d_gT", [128, 24, 512], BF16)
            mT = sb(ph, "d_mT", [128, 8, 512], BF16)
            tm = [sb(ph, f"d_tm{i}", [128, 512], F32) for i in range(2)]
            xt = [sb(ph, f"d_xt{i}", [128, D], F32) for i in range(2)]
            r1 = sb(ph, "d_r1", [128, D], F32)
            scr = sb(ph, "d_scr", [128, D], F32)
            st = sb(ph, "d_st", [128, 8], F32)
            x1t = sb(ph, "d_x1t", [128, D], F32)
            h2b = [sb(ph, f"d_h2b{i}", [128, D], BF16) for i in range(2)]
            h2T = sb(ph, "d_h2T", [128, 8, 512], BF16)
            sgt = sb(ph, "d_sgt", [128, 512], F32)
            actT = sb(ph, "d_actT", [128, 2, 512], BF16)
            ysh = sb(ph, "d_ysh", [128, D], F32)
            rs_sc = sb(ph, "d_rsc", [128, 64], F32)
            rs_bs = sb(ph, "d_rbs", [128, 64], F32)
            rs_t = sb(ph, "d_rt", [128, 64], F32)
            rs_sel = sb(ph, "d_rsel", [128, 64], F32)
            rs_selb = sb(ph, "d_rselb", [128, 64], BF16)
            rs_wd = sb(ph, "d_rwd", [128, 64], F32)
            rs_da = sb(ph, "d_rda", [128, 64], F32)
            rs_g = sb(ph, "d_rg", [128, 6, 8], F32)
            rs_m8 = sb(ph, "d_rm8", [128, 8], F32)
            rs_d8 = sb(ph, "d_rd8", [128, 8], F32)
            rs_s = sb(ph, "d_rs", [128, 4], F32)
            rs_m3 = sb(ph, "d_rm3", [128, 8, 64], F32)
            Rrun = sb(ph, "d_Rrun", [128, 64], F32)
            Rrunb = sb(ph, "d_Rrunb", [128, 64], BF16)
            op("dve", lambda e: e.memset(Rrun[:], 0.0), W=["d_Rrun"])
            for r in range(3):
                dma("pool", lambda e, r=r: e.dma_start(out=wb[:, r * 4:(r + 1) * 4, :], in_=Wd["w_branch"][L, r].rearrange("(cc p) n -> p cc n", p=128)), R=["d_wb"], W=["d_wb"])
            dma("pool", lambda e: e.dma_start(out=wo[:], in_=Wd["w_out"][L].rearrange("(k p) n -> p k n", p=128)), W=["d_wo"])
            dma("pool", lambda e: e.dma_start(out=wr[:], in_=Wd["w_router"][L].rearrange("(k p) n -> p k n", p=128)), W=["d_wr"])
            dma("pool", lambda e: e.dma_start(out=wsg[:], in_=Wd["sh_w_gate"][L].rearrange("(k p) n -> p k n", p=128)), W=["d_wsg"])
            dma("pool", lambda e: e.dma_start(out=wsu[:], in_=Wd["sh_w_up"][L].rearrange("(k p) n -> p k n", p=128)), W=["d_wsu"])
            dma("pool", lambda e: e.dma_start(out=wsd[:], in_=Wd["sh_w_down"][L].rearrange("(k p) n -> p k n", p=128)), W=["d_wsd"])
            for r in range(2):
                for j, seg in enumerate((2, 3, 4)):
                    dma("sp", bcast_load("sp", modb2[:, r * 3 + j, :], mod_b(r, seg), D), R=["modv", "d_modb"], W=["d_modb"])
            dma("sp", bcast_load("sp", lnb[:, 0, :], Wd["ln1_g"][L, :], D), R=["d_lnb"], W=["d_lnb"])
            dma("sp", bcast_load("sp", lnb[:, 1, :], Wd["ln1_b"][L, :], D), R=["d_lnb"], W=["d_lnb"])
            dma("sp", bcast_load("sp", rbias[:], Wd["router_bias"][L, :], 64), W=["d_rbias"])

            pc = [0]
            dblocks = blocks if not last else blocks[1:]
            for (t0, nb) in dblocks:
                mr = 1 if t0 < NCTX else 0
                nsub = nb // 128
                for r, srcT in enumerate((oaT_d, obT_d, ocT_d)):
                    dma("sp", lambda e, r=r, srcT=srcT: e.dma_start(out=oT[:, r, :, 0:nb], in_=srcT[:, t0:t0 + nb].rearrange("(cc p) t -> p cc t", p=128)),
                        R=[("oaT_d", "obT_d", "ocT_d")[r], "d_oT"], W=["d_oT"])
                dma("sp", lambda e: e.dma_start(out=gT[:, :, 0:nb], in_=gT_d[:, t0:t0 + nb].rearrange("(j p) t -> p j t", p=128)), R=["gT_d"], W=["d_gT"])
                for dt in range(8):
                    pss = []
                    for r in range(3):
                        pi = pc[0] % 4
                        pc[0] += 1
                        pss.append(pi)
                        for cc in range(4):
                            op("pe", lambda e, pi=pi, r=r, cc=cc, dt=dt: e.matmul(psA[pi][:, 0:nb], lhsT=wb[:, r * 4 + cc, dt * 128:(dt + 1) * 128], rhs=oT[:, r, cc, 0:nb],
                                                                                  start=(cc == 0), stop=(cc == 3)), R=["d_wb", "d_oT"], W=[f"psA{pi}"])
                    op("dve", lambda e, dt=dt, p0=pss[0]: e.tensor_tensor(out=tm[0][:, 0:nb], in0=psA[p0][:, 0:nb], in1=gT[:, dt, 0:nb], op=ALU.mult), R=[f"psA{pss[0]}", "d_gT"], W=["d_tm0"])
                    op("dve", lambda e, dt=dt, p1=pss[1]: e.tensor_tensor(out=tm[1][:, 0:nb], in0=psA[p1][:, 0:nb], in1=gT[:, 8 + dt, 0:nb], op=ALU.mult), R=[f"psA{pss[1]}", "d_gT"], W=["d_tm1"])
                    op("dve", lambda e: e.tensor_tensor(out=tm[0][:, 0:nb], in0=tm[0][:, 0:nb], in1=tm[1][:, 0:nb], op=ALU.add), R=["d_tm0", "d_tm1"], W=["d_tm0"])
                    op("dve", lambda e, dt=dt, p2=pss[2]: e.tensor_tensor(out=tm[1][:, 0:nb], in0=psA[p2][:, 0:nb], in1=gT[:, 16 + dt, 0:nb], op=ALU.mult), R=[f"psA{pss[2]}", "d_gT", "d_tm1"], W=["d_tm1"])
                    op("dve", lambda e, dt=dt: e.tensor_tensor(out=mT[:, dt, 0:nb], in0=tm[0][:, 0:nb], in1=tm[1][:, 0:nb], op=ALU.add), R=["d_tm0", "d_tm1"], W=["d_mT"])
                for s_ in range(nsub):
                    tt = t0 + s_ * 128
                    ti = tt // 128
                    xx = xt[s_ % 2]
                    kx = f"d_xt{s_ % 2}"
                    hh = h2b[s_ % 2]
                    kh = f"d_h2b{s_ % 2}"
                    dma("sp", lambda e, xx=xx, tt=tt: e.dma_start(out=xx[:], in_=src_rows(tt, 128)), W=[kx])
                    for half in range(2):
                        for k in range(8):
                            op("pe", lambda e, half=half, k=k, s_=s_: e.matmul(psS[half][:], lhsT=mT[:, k, s_ * 128:(s_ + 1) * 128], rhs=wo[:, k, half * 512:(half + 1) * 512],
                                                                             start=(k == 0), stop=(k == 7)), R=["d_mT", "d_wo"], W=[f"psS{half}"])
                        op("dve", lambda e, half=half: e.tensor_tensor(out=r1[:, half * 512:(half + 1) * 512], in0=psS[half][:], in1=modb2[:, mr * 3, half * 512:(half + 1) * 512], op=ALU.mult),
                           R=[f"psS{half}", "d_modb", "d_r1"], W=["d_r1"])
                    op("dve", lambda e, xx=xx: e.scalar_tensor_tensor(out=r1[:], in0=xx[:], scalar=DN_ALPHA, in1=r1[:], op0=ALU.mult, op1=ALU.add), R=[kx, "d_r1"], W=["d_r1"])
                    layer_norm_tile(r1[:], "d_r1", x1t[:], "d_x1t", scr[:], "d_scr", st, "d_st", mul_b=lnb[:, 0, :], add_b=lnb[:, 1, :], keys_b=["d_lnb"])
                    dma("sp", lambda e, tt=tt: e.dma_start(out=x1s[tt:tt + 128, :], in_=x1t[:]), R=["d_x1t"], W=["x1s"])
                    layer_norm_tile(x1t[:], "d_x1t", hh[:], kh, scr[:], "d_scr", st, "d_st", mul_b=modb2[:, mr * 3 + 2, :], add_b=modb2[:, mr * 3 + 1, :], keys_b=["d_modb"])
                    transpose_to(hh, kh, 8, h2T, "d_h2T", s_ * 128, s_ % 2)
                    pi = pc[0] % 4
                    pc[0] += 1
                    for k in range(8):
                        op("pe", lambda e, pi=pi, k=k, s_=s_: e.matmul(psA[pi][:, 0:64], lhsT=h2T[:, k, s_ * 128:(s_ + 1) * 128], rhs=wr[:, k, :], start=(k == 0), stop=(k == 7)),
                           R=["d_h2T", "d_wr"], W=[f"psA{pi}"])
                    op("act", lambda e, pi=pi: e.activation(out=rs_sc[:], in_=psA[pi][:, 0:64], func=AF.Sigmoid), R=[f"psA{pi}"], W=["d_rsc"])
                    op("dve", lambda e: e.tensor_tensor(out=rs_bs[:], in0=rs_sc[:], in1=rbias[:], op=ALU.add), R=["d_rsc", "d_rbias"], W=["d_rbs"])
                    bs3 = rs_bs[:].rearrange("p (g e) -> p g e", g=8)
                    t3 = rs_t[:].rearrange("p (g e) -> p g e", g=8)
                    op("dve", lambda e: e.tensor_reduce(out=rs_g[:, 0, :], in_=bs3, axis=AX.X, op=ALU.max), R=["d_rbs"], W=["d_rg"])
                    op("dve", lambda e: e.tensor_tensor(out=t3, in0=bs3, in1=rs_g[:, 0, :].unsqueeze(2).to_broadcast([128, 8, 8]), op=ALU.is_equal), R=["d_rbs", "d_rg"], W=["d_rt"])
                    op("dve", lambda e: e.scalar_tensor_tensor(out=rs_t[:], in0=rs_t[:], scalar=-BIG, in1=rs_bs[:], op0=ALU.mult, op1=ALU.add), R=["d_rt", "d_rbs"], W=["d_rt"])
                    op("dve", lambda e: e.tensor_reduce(out=rs_g[:, 1, :], in_=t3, axis=AX.X, op=ALU.max), R=["d_rt", "d_rg"], W=["d_rg"])
                    op("dve", lambda e: e.tensor_tensor(out=rs_g[:, 2, :], in0=rs_g[:, 0, :], in1=rs_g[:, 1, :], op=ALU.add), R=["d_rg"], W=["d_rg"])
                    op("dve", lambda e: e.max(out=rs_m8[:], in_=rs_g[:, 2, :]), R=["d_rg"], W=["d_rm8"])
                    op("dve", lambda e: e.tensor_scalar(out=rs_g[:, 3, :], in0=rs_g[:, 2, :], scalar1=rs_m8[:, 3:4], scalar2=None, op0=ALU.is_ge), R=["d_rg", "d_rm8"], W=["d_rg"])
                    op("dve", lambda e: e.tensor_scalar(out=rs_g[:, 3, :], in0=rs_g[:, 3, :], scalar1=BIG, scalar2=-BIG, op0=ALU.mult, op1=ALU.add), R=["d_rg"], W=["d_rg"])
                    op("dve", lambda e: e.tensor_tensor(out=t3, in0=bs3, in1=rs_g[:, 3, :].unsqueeze(2).to_broadcast([128, 8, 8]), op=ALU.add), R=["d_rbs", "d_rg"], W=["d_rt"])
                    op("dve", lambda e: e.max(out=rs_m8[:], in_=rs_t[:]), R=["d_rt", "d_rm8"], W=["d_rm8"])
                    op("dve", lambda e: e.tensor_scalar(out=rs_sel[:], in0=rs_t[:], scalar1=rs_m8[:, 7:8], scalar2=None, op0=ALU.is_ge), R=["d_rt", "d_rm8"], W=["d_rsel"])
                    op("dve", lambda e: e.tensor_tensor(out=rs_wd[:], in0=rs_sel[:], in1=rs_sc[:], op=ALU.mult), R=["d_rsel", "d_rsc"], W=["d_rwd"])
                    op("dve", lambda e: e.reduce_sum(out=rs_s[:, 0:1], in_=rs_wd[:], axis=AX.X), R=["d_rwd"], W=["d_rs"])
                    op("dve", lambda e: e.reciprocal(out=rs_s[:, 1:2], in_=rs_s[:, 0:1]), R=["d_rs"], W=["d_rs"])
                    op("dve", lambda e: e.tensor_scalar(out=rs_wd[:], in0=rs_wd[:], scalar1=rs_s[:, 1:2], scalar2=2.5, op0=ALU.mult, op1=ALU.mult), R=["d_rwd", "d_rs"], W=["d_rwd"])
                    op("act", lambda e: e.copy(out=rs_selb[:], in_=rs_sel[:]), R=["d_rsel"], W=["d_rselb"])
                    op("act", lambda e: e.copy(out=Rrunb[:], in_=Rrun[:]), R=["d_Rrun"], W=["d_Rrunb"])
                    pi = pc[0] % 4
                    pc[0] += 1
                    op("pe", lambda e, pi=pi: e.matmul(psA[pi][:, 0:64], lhsT=Ustr[:], rhs=rs_selb[:], start=True, stop=False), R=["Ustr", "d_rselb"], W=[f"psA{pi}"])
                    op("pe", lambda e, pi=pi: e.matmul(psA[pi][:, 0:64], lhsT=ones_bf[:], rhs=Rrunb[:], start=False, stop=True), R=["ones_bf", "d_Rrunb"], W=[f"psA{pi}"])
                    op("dve", lambda e: e.tensor_tensor(out=Rrun[:], in0=Rrun[:], in1=rs_sel[:], op=ALU.add), R=["d_Rrun", "d_rsel"], W=["d_Rrun"])
                    op("dve", lambda e, pi=pi: e.tensor_scalar(out=rs_t[:], in0=psA[pi][:, 0:64], scalar1=float(CAP), scalar2=None, op0=ALU.is_lt), R=[f"psA{pi}"], W=["d_rt"])
                    op("dve", lambda e: e.tensor_tensor(out=rs_t[:], in0=rs_t[:], in1=rs_sel[:], op=ALU.mult), R=["d_rt", "d_rsel"], W=["d_rt"])
                    op("dve", lambda e, pi=pi: e.tensor_tensor(out=rs_da[:], in0=psA[pi][:, 0:64], in1=eidx[:], op=ALU.add), R=[f"psA{pi}", "eidx"], W=["d_rda"])
                    op("dve", lambda e: e.tensor_scalar(out=rs_bs[:], in0=rs_da[:], scalar1=-1.0, scalar2=BIG, op0=ALU.mult, op1=ALU.add), R=["d_rda"], W=["d_rbs"])
                    op("dve", lambda e: e.tensor_tensor(out=rs_bs[:], in0=rs_bs[:], in1=rs_t[:], op=ALU.mult), R=["d_rbs", "d_rt"], W=["d_rbs"])
                    op("dve", lambda e: e.max(out=rs_m8[:], in_=rs_bs[:]), R=["d_rbs", "d_rm8"], W=["d_rm8"])
                    op("dve", lambda e: e.tensor_scalar(out=rs_d8[:], in0=rs_m8[:], scalar1=-1.0, scalar2=BIG, op0=ALU.mult, op1=ALU.add), R=["d_rm8"], W=["d_rd8"])
                    op("dve", lambda e, ti=ti: e.tensor_copy(out=destI[:, ti, :], in_=rs_d8[:]), R=["d_rd8", "destI"], W=["destI"])
                    op("dve", lambda e: e.tensor_tensor(out=rs_m3[:], in0=rs_da[:].unsqueeze(1).to_broadcast([128, 8, 64]), in1=rs_d8[:].unsqueeze(2).to_broadcast([128, 8, 64]), op=ALU.is_equal),
                       R=["d_rda", "d_rd8"], W=["d_rm3"])
                    op("dve", lambda e: e.tensor_tensor(out=rs_m3[:], in0=rs_m3[:], in1=rs_wd[:].unsqueeze(1).to_broadcast([128, 8, 64]), op=ALU.mult), R=["d_rm3", "d_rwd"], W=["d_rm3"])
                    op("dve", lambda e, ti=ti: e.reduce_sum(out=wk[:, ti, :], in_=rs_m3[:], axis=AX.X), R=["d_rm3", "wk"], W=["wk"])
                    if "route" in debug:
                        dma("sp", lambda e, tt=tt: e.dma_start(out=dbg_d[tt:tt + 128, 0:8], in_=rs_d8[:]), R=["d_rd8"], W=["dbg_d"])
                        dma("sp", lambda e, tt=tt, ti=ti: e.dma_start(out=dbg_d[tt:tt + 128, 8:16], in_=wk[:, ti, :]), R=["wk"], W=["dbg_d"])
                    for k in range(8):
                        dma("pool", lambda e, hh=hh, ti=ti, k=k: e.indirect_dma_start(out=Xg[:, :], out_offset=bass.IndirectOffsetOnAxis(ap=destI[:, ti, k:k + 1], axis=0),
                                                                                 in_=hh[:, :], in_offset=None, bounds_check=bc_reg, oob_is_err=False),
                            R=[kh, "destI"], W=["Xg"])
                for ft in range(2):
                    pg = pc[0] % 4
                    pu = (pc[0] + 1) % 4
                    pc[0] += 2
                    for k in range(8):
                        op("pe", lambda e, pg=pg, k=k, ft=ft: e.matmul(psA[pg][:, 0:nb], lhsT=wsg[:, k, ft * 128:(ft + 1) * 128], rhs=h2T[:, k, 0:nb], start=(k == 0), stop=(k == 7)),
                           R=["d_wsg", "d_h2T"], W=[f"psA{pg}"])
                    for k in range(8):
                        op("pe", lambda e, pu=pu, k=k, ft=ft: e.matmul(psA[pu][:, 0:nb], lhsT=wsu[:, k, ft * 128:(ft + 1) * 128], rhs=h2T[:, k, 0:nb], start=(k == 0), stop=(k == 7)),
                           R=["d_wsu", "d_h2T"], W=[f"psA{pu}"])
                    op("act", lambda e, pg=pg: e.activation(out=sgt[:, 0:nb], in_=psA[pg][:, 0:nb], func=AF.Silu), R=[f"psA{pg}"], W=["d_sgt"])
                    op("dve", lambda e, pu=pu, ft=ft: e.tensor_tensor(out=actT[:, ft, 0:nb], in0=sgt[:, 0:nb], in1=psA[pu][:, 0:nb], op=ALU.mult), R=["d_sgt", f"psA{pu}"], W=["d_actT"])
                for s_ in range(nsub):
                    tt = t0 + s_ * 128
                    for half in range(2):
                        for fc in range(2):
                            op("pe", lambda e, half=half, fc=fc, s_=s_: e.matmul(psS[half][:], lhsT=actT[:, fc, s_ * 128:(s_ + 1) * 128], rhs=wsd[:, fc, half * 512:(half + 1) * 512],
                                                                               start=(fc == 0), stop=(fc == 1)), R=["d_actT", "d_wsd"], W=[f"psS{half}"])
                        op("act", lambda e, half=half: e.copy(out=ysh[:, half * 512:(half + 1) * 512], in_=psS[half][:]), R=[f"psS{half}", "d_ysh"], W=["d_ysh"])
                    dma("sp", lambda e, tt=tt: e.dma_start(out=ysh_d[tt:tt + 128, :], in_=ysh[:]), R=["d_ysh"], W=["ysh_d"])
            Bd.barrier()

        if "stopD" in debug:
            break

        with ExitStack() as ph:
            wg = [sb(ph, f"f_wg{i}", [128, 8, 256], BF16) for i in range(2)]
            wu = [sb(ph, f"f_wu{i}", [128, 8, 256], BF16) for i in range(2)]
            wdn = [sb(ph, f"f_wd{i}", [128, 2, D], BF16) for i in range(2)]
            xg = [sb(ph, f"f_xg{i}", [128, 8, 512], BF16) for i in range(2)]
            sgt = sb(ph, "f_sgt", [128, 512], F32)
            aT = [sb(ph, f"f_aT{i}", [128, 2, 512], BF16) for i in range(2)]
            yo = [sb(ph, f"f_yo{i}", [128, D], BF16) for i in range(2)]
            pc = [0]
            yi = [0]
            NXG = 3
            xg3 = xg + [sb(ph, "f_xg2", [128, 8, 512], BF16)]
            aT3 = aT + [sb(ph, "f_aT2", [128, 2, 512], BF16)]
            chunks_f = [(ex, ch) for ex in range(NE) for ch in range(CAP // 512)]

            def load_w(ex):
                b = ex % 2
                dma("pool", lambda e: e.dma_start(out=wg[b][:], in_=Wd["moe_w_gate"][L, ex].rearrange("(k p) n -> p k n", p=128)), W=[f"f_wg{b}"])
                dma("pool", lambda e: e.dma_start(out=wu[b][:], in_=Wd["moe_w_up"][L, ex].rearrange("(k p) n -> p k n", p=128)), W=[f"f_wu{b}"])
                dma("pool", lambda e: e.dma_start(out=wdn[b][:], in_=Wd["moe_w_down"][L, ex].rearrange("(k p) n -> p k n", p=128)), W=[f"f_wd{b}"])

            def load_x(i):
                ex, ch = chunks_f[i]
                slot0 = ex * CAP + ch * 512
                xb = i % NXG
                for k in range(8):
                    dma("sp", lambda e, k=k: e.dma_start_transpose(out=xg3[xb][:, k, :], in_=Xg[slot0:slot0 + 512, k * 128:(k + 1) * 128]), R=["Xg", f"f_xg{xb}"], W=[f"f_xg{xb}"])

            load_w(0)
            load_w(1)
            load_x(0)
            load_x(1)
            bank6 = [(psA[0], "psA0"), (psA[1], "psA1"), (psA[2], "psA2"), (psA[3], "psA3"), (psS[0], "psS0"), (psS[1], "psS1")]

            def nxt():
                bk = bank6[pc[0] % 6]
                pc[0] += 1
                return bk

            def emit_gu(i):
                ex, ch = chunks_f[i]
                b = ex % 2
                xb = i % NXG
                if i + 2 < len(chunks_f):
                    load_x(i + 2)
                for ft in range(2):
                    (pg, kg), (pu, ku) = nxt(), nxt()
                    for k in range(8):
                        op("pe", lambda e, k=k: e.matmul(pg[:], lhsT=wg[b][:, k, ft * 128:(ft + 1) * 128], rhs=xg3[xb][:, k, :], start=(k == 0), stop=(k == 7)),
                           R=[f"f_wg{b}", f"f_xg{xb}"], W=[kg])
                    for k in range(8):
                        op("pe", lambda e, k=k: e.matmul(pu[:], lhsT=wu[b][:, k, ft * 128:(ft + 1) * 128], rhs=xg3[xb][:, k, :], start=(k == 0), stop=(k == 7)),
                           R=[f"f_wu{b}", f"f_xg{xb}"], W=[ku])
                    op("act", lambda e: e.activation(out=sgt2[ft][:], in_=pg[:], func=AF.Silu), R=[kg], W=[f"f_sgt{ft}"])
                    op("dve", lambda e: e.tensor_tensor(out=aT3[xb][:, ft, :], in0=sgt2[ft][:], in1=pu[:], op=ALU.mult), R=[f"f_sgt{ft}", ku], W=[f"f_aT{xb}"])

            def emit_down(i):
                ex, ch = chunks_f[i]
                b = ex % 2
                xb = i % NXG
                slot0 = ex * CAP + ch * 512
                for st_ in range(4):
                    yb_ = yi[0] % 3
                    yi[0] += 1
                    for half in range(2):
                        pd, kd = nxt()
                        for fc in range(2):
                            op("pe", lambda e, fc=fc: e.matmul(pd[:], lhsT=aT3[xb][:, fc, st_ * 128:(st_ + 1) * 128], rhs=wdn[b][:, fc, half * 512:(half + 1) * 512],
                                                               start=(fc == 0), stop=(fc == 1)), R=[f"f_aT{xb}", f"f_wd{b}"], W=[kd])
                        if half == 0:
                            op("act", lambda e: e.copy(out=yo3[yb_][:, 0:512], in_=pd[:]), R=[kd, f"f_yo{yb_}"], W=[f"f_yo{yb_}"])
                        else:
                            op("dve", lambda e: e.tensor_copy(out=yo3[yb_][:, 512:1024], in_=pd[:]), R=[kd, f"f_yo{yb_}"], W=[f"f_yo{yb_}"])
                    dma("sp", lambda e: e.dma_start(out=Yg[slot0 + st_ * 128:slot0 + (st_ + 1) * 128, :], in_=yo3[yb_][:]), R=[f"f_yo{yb_}"], W=["Yg"])

            sgt2 = [sgt, sb(ph, "f_sgtb", [128, 512], F32)]
            yo3 = yo + [sb(ph, "f_yo2", [128, D], BF16)]
            emit_gu(0)
            for i in range(len(chunks_f)):
                if i + 1 < len(chunks_f):
                    emit_gu(i + 1)
                emit_down(i)
                ex_i, ch_i = chunks_f[i]
                if ch_i == CAP // 512 - 1 and ex_i + 2 < NE:
                    load_w(ex_i + 2)
            Bd.barrier()

        with ExitStack() as ph:
            g2b = sb(ph, "g_g2b", [128, 2, D], F32)
            lnb2 = sb(ph, "g_lnb", [128, 2, D], F32)
            x1t = [sb(ph, f"g_x1t{i}", [128, D], F32) for i in range(2)]
            accb = [sb(ph, f"g_acc{i}", [128, D], F32) for i in range(2)]
            gath = [sb(ph, f"g_gath{i}", [128, D], BF16) for i in range(4)]
            scr = sb(ph, "g_scr", [128, D], F32)
            st = sb(ph, "g_st", [128, 8], F32)
            xo = [sb(ph, f"g_xo{i}", [128, D], F32) for i in range(2)]
            for r in range(2):
                dma("sp", bcast_load("sp", g2b[:, r, :], mod_b(r, 5), D), R=["modv", "g_g2b"], W=["g_g2b"])
            dma("sp", bcast_load("sp", lnb2[:, 0, :], Wd["ln2_g"][L, :], D), R=["g_lnb"], W=["g_lnb"])
            dma("sp", bcast_load("sp", lnb2[:, 1, :], Wd["ln2_b"][L, :], D), R=["g_lnb"], W=["g_lnb"])
            for i in range(4):
                op("dve", lambda e, i=i: e.memset(gath[i][:], 0.0), W=[f"g_gath{i}"])
            gi = [0]
            tiles = list(range(NT)) if not last else list(range(2, NT))
            for n_, ti in enumerate(tiles):
                tt = ti * 128
                mr = 1 if tt < NCTX else 0
                bi = n_ % 2
                dma("sp", lambda e, bi=bi, tt=tt: e.dma_start(out=x1t[bi][:], in_=x1s[tt:tt + 128, :]), R=["x1s"], W=[f"g_x1t{bi}"])
                dma("sp", lambda e, bi=bi, tt=tt: e.dma_start(out=accb[bi][:], in_=ysh_d[tt:tt + 128, :]), R=["ysh_d"], W=[f"g_acc{bi}"])
                for k in range(8):
                    gj = gi[0] % 4
                    gi[0] += 1
                    dma("pool", lambda e, gj=gj, ti=ti, k=k: e.indirect_dma_start(out=gath[gj][:, :], out_offset=None, in_=Yg[:, :],
                                                                              in_offset=bass.IndirectOffsetOnAxis(ap=destI[:, ti, k:k + 1], axis=0),
                                                                              bounds_check=bc_reg, oob_is_err=False), R=["Yg", "destI"], W=[f"g_gath{gj}"])
                    op("dve", lambda e, gj=gj, bi=bi, ti=ti, k=k: e.scalar_tensor_tensor(out=accb[bi][:], in0=gath[gj][:], scalar=wk[:, ti, k:k + 1], in1=accb[bi][:], op0=ALU.mult, op1=ALU.add),
                       R=[f"g_gath{gj}", "wk", f"g_acc{bi}"], W=[f"g_acc{bi}"])
                op("dve", lambda e, bi=bi, mr=mr: e.tensor_tensor(out=accb[bi][:], in0=accb[bi][:], in1=g2b[:, mr, :], op=ALU.mult), R=[f"g_acc{bi}", "g_g2b"], W=[f"g_acc{bi}"])
                op("dve", lambda e, bi=bi: e.scalar_tensor_tensor(out=accb[bi][:], in0=x1t[bi][:], scalar=DN_ALPHA, in1=accb[bi][:], op0=ALU.mult, op1=ALU.add), R=[f"g_x1t{bi}", f"g_acc{bi}"], W=[f"g_acc{bi}"])
                layer_norm_tile(accb[bi][:], f"g_acc{bi}", xo[bi][:], f"g_xo{bi}", scr[:], "g_scr", st, "g_st", mul_b=lnb2[:, 0, :], add_b=lnb2[:, 1, :], keys_b=["g_lnb"])
                if last:
                    dma("sp", lambda e, bi=bi, tt=tt: e.dma_start(out=out_d[tt - NCTX:tt - NCTX + 128, :], in_=xo[bi][:]), R=[f"g_xo{bi}"], W=["out"])
                else:
                    dma("sp", lambda e, bi=bi, tt=tt: e.dma_start(out=xA[tt:tt + 128, :], in_=xo[bi][:]), R=[f"g_xo{bi}"], W=["xA"])
            Bd.barrier()

    Bd.barrier()
    print("instructions:", Bd.ninst, "semaphores:", Bd.nsem)
    return nc


_NC_CACHE = {}


def kernel(**inputs):
    x = np.ascontiguousarray(inputs["x"], dtype=np.float32)
    ctx = np.ascontiguousarray(inputs["ctx"], dtype=np.float32)
    c = np.asarray(inputs["c"], dtype=np.float32)
    c_ctx = np.asarray(inputs["c_ctx"], dtype=np.float32)
    nb = x.shape[0]
    if "nc" not in _NC_CACHE:
        _NC_CACHE["nc"] = build()
    nc = _NC_CACHE["nc"]
    shared = {n: np.ascontiguousarray(inputs[n], dtype=np.float32) for n in W_NAMES}
    in_maps = []
    for b in range(nb):
        m = dict(shared)
        m["x"] = x[b]
        m["ctx"] = ctx[b]
        m["c"] = np.stack([c[b], c_ctx], axis=0)
        in_maps.append(m)
    res = run_bass_kernel_spmd(nc, in_maps, core_ids=list(range(nb)))
    return np.stack([np.asarray(r["out"], dtype=np.float32) for r in res.results], axis=0)
```

```python
import math
import numpy as np
import concourse.bass as bass
import concourse.mybir as mybir
from concourse.bass_utils import run_bass_kernel_spmd
from contextlib import ExitStack

F32 = mybir.dt.float32
BF16 = mybir.dt.bfloat16
I32 = mybir.dt.int32
AF = mybir.ActivationFunctionType
ALU = mybir.AluOpType
AX = mybir.AxisListType

D = 1024
NCTX = 256
NLAT = 4096
T = NCTX + NLAT
NT = T // 128
DEPTH = 2
INC = 6656
NE = 64
CAP = 2048
NSLOT = NE * CAP
DN_ALPHA = (2 * DEPTH) ** 0.25
LN_EPS = 1e-6
RMS_EPS = 1e-5
BIG = 1.0e6
OQ, OK_, OV, OU, OS, OX, OY, OG = 0, 512, 1024, 1536, 2048, 2560, 3072, 3584

W_NAMES = ["w_mod", "b_mod", "w_in", "b_in", "lam_q1", "lam_k1", "lam_q2", "lam_k2", "attn_norm_g",
           "sg_ln_g", "sg_ln_b", "sg_w", "sg_b", "conv_w", "conv_b", "lru_wa", "lru_ba", "lru_wx", "lru_bx",
           "lru_lam", "w_branch", "w_out", "ln1_g", "ln1_b", "w_router", "router_bias", "moe_w_gate",
           "moe_w_up", "moe_w_down", "sh_w_gate", "sh_w_up", "sh_w_down", "ln2_g", "ln2_b"]
W_SHAPES = {
    "w_mod": [2, 1024, 6144], "b_mod": [2, 6144], "w_in": [2, 1024, 6656], "b_in": [2, 6656],
    "lam_q1": [2, 64], "lam_k1": [2, 64], "lam_q2": [2, 64], "lam_k2": [2, 64], "attn_norm_g": [2, 4, 128],
    "sg_ln_g": [2, 512], "sg_ln_b": [2, 512], "sg_w": [2, 4, 128, 128], "sg_b": [2, 4, 128],
    "conv_w": [2, 4, 512], "conv_b": [2, 512], "lru_wa": [2, 2, 8, 64, 64], "lru_ba": [2, 2, 512],
    "lru_wx": [2, 2, 8, 64, 64], "lru_bx": [2, 2, 512], "lru_lam": [2, 2, 512],
    "w_branch": [2, 3, 512, 1024], "w_out": [2, 1024, 1024], "ln1_g": [2, 1024], "ln1_b": [2, 1024],
    "w_router": [2, 1024, 64], "router_bias": [2, 64], "moe_w_gate": [2, 64, 1024, 256],
    "moe_w_up": [2, 64, 1024, 256], "moe_w_down": [2, 64, 256, 1024], "sh_w_gate": [2, 1024, 256],
    "sh_w_up": [2, 1024, 256], "sh_w_down": [2, 256, 1024], "ln2_g": [2, 1024], "ln2_b": [2, 1024],
}

SEM_EPOCH = 30000
NS_DMA = 8


class Builder:
    def __init__(self, nc):
        self.nc = nc
        self.E = {"pe": nc.tensor, "act": nc.scalar, "dve": nc.vector, "pool": nc.gpsimd, "sp": nc.sync}
        self.cur = {}
        self.known = {e: {} for e in self.E}
        self.lastw = {}
        self.rd = {}
        self.nsem = 0
        self.own = {e: set() for e in self.E}
        self.dq = {q: {"sems": [None] * NS_DMA, "val": [0] * NS_DMA, "i": 0} for q in ("sp", "pool", "act")}
        self.ninst = 0

    def newsem(self):
        self.nsem += 1
        return self.nc.semaphore(f"sm{self.nsem}").__enter__()

    def _wait(self, e, sem, val):
        if self.known[e].get(sem, 0) >= val:
            return
        self.E[e].wait_ge(sem, val)
        self.known[e][sem] = val

    def _deps(self, e, R, W):
        toks = {}
        for r in R:
            t = self.lastw.get(r)
            if t is not None and toks.get(t[0], 0) < t[1]:
                toks[t[0]] = t[1]
        for w in W:
            t = self.lastw.get(w)
            if t is not None and toks.get(t[0], 0) < t[1]:
                toks[t[0]] = t[1]
            for sm, v in self.rd.get(w, {}).items():
                if toks.get(sm, 0) < v:
                    toks[sm] = v
        for sm, v in toks.items():
            if e == "pe" and sm in self.own["pe"]:
                continue
            self._wait(e, sm, v)

    def _commit(self, tok, R, W):
        for w in W:
            self.lastw[w] = tok
            self.rd[w] = {}
        for r in R:
            d = self.rd.setdefault(r, {})
            if d.get(tok[0], 0) < tok[1]:
                d[tok[0]] = tok[1]

    def op(self, e, fn, R=(), W=()):
        self._deps(e, R, W)
        st = self.cur.get(e)
        if st is None or st[1] >= SEM_EPOCH:
            st = self.cur[e] = [self.newsem(), 0]
            self.own[e].add(st[0])
        ins = fn(self.E[e])
        st[1] += 1
        ins.then_inc(st[0], 1)
        self.ninst += 1
        self._commit((st[0], st[1]), R, W)

    def dma(self, q, fn, R=(), W=()):
        self._deps(q, R, W)
        d = self.dq[q]
        slot = d["i"] % NS_DMA
        d["i"] += 1
        if d["sems"][slot] is None or d["val"][slot] + 16 > SEM_EPOCH:
            if d["sems"][slot] is not None:
                self._wait(q, d["sems"][slot], d["val"][slot])
            d["sems"][slot] = self.newsem()
            d["val"][slot] = 0
        sem, prev = d["sems"][slot], d["val"][slot]
        if prev > 0:
            self._wait(q, sem, prev)
        ins = fn(self.E[q])
        ins.then_inc(sem, 16)
        d["val"][slot] = prev + 16
        self.ninst += 1
        self._commit((sem, prev + 16), R, W)

    def barrier(self, engines=None):
        toks = []
        for e, st in self.cur.items():
            if st[1] > 0:
                toks.append((st[0], st[1]))
        for q, d in self.dq.items():
            for sm, v in zip(d["sems"], d["val"]):
                if sm is not None and v > 0:
                    toks.append((sm, v))
        for e in (engines or list(self.E)):
            for sm, v in toks:
                self._wait(e, sm, v)
        if engines is None:
            self.lastw.clear()
            self.rd.clear()


def build(n_layers=DEPTH, debug=()):
    nc = bass.Bass("TRN2", target_bir_lowering=False)
    Bd = Builder(nc)
    op, dma = Bd.op, Bd.dma
    bc_reg = nc.gpsimd.to_reg(NSLOT - 1)

    def din(name, shape, dt=F32):
        return nc.dram_tensor(name, list(shape), dt, kind="ExternalInput").ap()

    def dscr(name, shape, dt=F32):
        kind = "ExternalOutput" if name in debug else "Internal"
        return nc.dram_tensor(name, list(shape), dt, kind=kind).ap()

    x_in = din("x", [NLAT, D])
    ctx_in = din("ctx", [NCTX, D])
    c_in = din("c", [2, D])
    Wd = {n: din(n, W_SHAPES[n]) for n in W_NAMES}
    out_d = nc.dram_tensor("out", [NLAT, D], F32, kind="ExternalOutput").ap()

    xA = dscr("xA", [T, D])
    x1s = dscr("x1s", [T, D])
    modv = dscr("modv", [2, 6144])
    qT_d = dscr("qT_d", [4, 128, T], BF16)
    kT_d = dscr("kT_d", [4, 128, T], BF16)
    v_d = dscr("v_d", [T, 512], BF16)
    xT_d = dscr("xT_d", [512, T], F32)
    yT_d = dscr("yT_d", [512, T], BF16)
    gT_d = dscr("gT_d", [3072, T], BF16)
    oaT_d = dscr("oaT_d", [512, T], BF16)
    obT_d = dscr("obT_d", [512, T], BF16)
    ocT_d = dscr("ocT_d", [512, T], BF16)
    cos_d = dscr("cos_d", [128, NLAT], F32)
    sin_d = dscr("sin_d", [128, NLAT], F32)
    Xg = dscr("Xg", [NSLOT, D], BF16)
    Yg = dscr("Yg", [NSLOT, D], BF16)
    ysh_d = dscr("ysh_d", [T, D], F32)
    dbg_d = dscr("dbg_d", [T, 64], F32)

    uid = [0]

    def sb(stack, name, shape, dt=F32):
        uid[0] += 1
        return stack.enter_context(nc.sbuf_tensor(f"{name}_u{uid[0]}", list(shape), dt))

    top = ExitStack()
    psA = [top.enter_context(nc.psum_tensor(f"psA{i}", [128, 512], F32)) for i in range(4)]
    psS = [top.enter_context(nc.psum_tensor(f"psS{i}", [128, 512], F32)) for i in range(2)]
    psT = [top.enter_context(nc.psum_tensor(f"psT{i}", [128, 1024], BF16)) for i in range(2)]

    ident = sb(top, "ident", [128, 128], BF16)
    ones_bf = sb(top, "ones_bf", [128, 128], BF16)
    Ustr = sb(top, "Ustr", [128, 128], BF16)
    Pm = sb(top, "Pm", [128, 128], BF16)
    rowi = sb(top, "rowi", [128, 1], F32)
    coli = sb(top, "coli", [128, 128], F32)
    ctmp = sb(top, "ctmp", [128, 128], F32)
    ctmp2 = sb(top, "ctmp2", [128, 128], F32)
    eidx = sb(top, "eidx", [128, 64], F32)
    op("pool", lambda e: e.iota(rowi[:], pattern=[[0, 1]], base=0, channel_multiplier=1,
                                allow_small_or_imprecise_dtypes=True), W=["rowi"])
    op("pool", lambda e: e.iota(coli[:], pattern=[[1, 128]], base=0, channel_multiplier=0,
                                allow_small_or_imprecise_dtypes=True), W=["coli"])
    op("pool", lambda e: e.iota(eidx[:], pattern=[[CAP, 64]], base=0, channel_multiplier=0,
                                allow_small_or_imprecise_dtypes=True), W=["eidx"])
    op("dve", lambda e: e.tensor_scalar(out=ident[:], in0=coli[:], scalar1=rowi[:, 0:1], scalar2=None,
                                        op0=ALU.is_equal), R=["coli", "rowi"], W=["ident"])
    op("dve", lambda e: e.tensor_scalar(out=Ustr[:], in0=coli[:], scalar1=rowi[:, 0:1], scalar2=None,
                                        op0=ALU.is_gt), R=["coli", "rowi"], W=["Ustr"])
    op("dve", lambda e: e.memset(ones_bf[:], 1.0), W=["ones_bf"])
    identF = sb(top, "identF", [128, 128], F32)
    destI = sb(top, "destI", [128, NT, 8], I32)
    wk = sb(top, "wk", [128, NT, 8], F32)
    eps_t = sb(top, "eps_t", [128, 2], F32)
    op("dve", lambda e: e.memset(eps_t[:, 0:1], LN_EPS), W=["eps_t"])
    op("dve", lambda e: e.memset(eps_t[:, 1:2], RMS_EPS), R=["eps_t"], W=["eps_t"])
    op("dve", lambda e: e.tensor_scalar(out=identF[:], in0=coli[:], scalar1=rowi[:, 0:1], scalar2=None,
                                        op0=ALU.is_equal), R=["coli", "rowi"], W=["identF"])
    op("pool", lambda e: e.iota(ctmp[:], pattern=[[32, 4], [-16, 2], [1, 16]], base=16, channel_multiplier=0,
                                allow_small_or_imprecise_dtypes=True), W=["ctmp"])
    op("dve", lambda e: e.tensor_scalar(out=Pm[:], in0=ctmp[:], scalar1=rowi[:, 0:1], scalar2=None,
                                        op0=ALU.is_equal), R=["ctmp", "rowi"], W=["Pm"])

    op("dve", lambda e: e.memset(destI[:], 0), W=["destI"])
    dzero = sb(top, "dzero", [128, 64], BF16)
    op("dve", lambda e: e.memset(dzero[:], 0.0), W=["dzero"])
    dma("pool", lambda e: e.indirect_dma_start(out=Xg[:, 0:64], out_offset=bass.IndirectOffsetOnAxis(ap=destI[:, 0, 0:1], axis=0),
                                               in_=dzero[:, :], in_offset=None, bounds_check=bc_reg, oob_is_err=False), R=["destI", "dzero"], W=["Xg"])

    with ExitStack() as ph:
        tok = sb(ph, "r_tok", [128, NLAT], F32)
        colp = sb(ph, "r_col", [128, NLAT], F32)
        ang = sb(ph, "r_ang", [128, NLAT], F32)
        tb = sb(ph, "r_tb", [128, NLAT], F32)
        ti = sb(ph, "r_ti", [128, NLAT], I32)
        pv = sb(ph, "r_pv", [128, 8], F32)
        pat = sb(ph, "r_pat", [128, 128], F32)

        def per_part(pattern, base, col):
            op("pool", lambda e: e.iota(pat[:], pattern=pattern, base=base, channel_multiplier=0,
                                        allow_small_or_imprecise_dtypes=True), W=["r_pat"])
            op("dve", lambda e: e.tensor_tensor(out=pat[:], in0=pat[:], in1=identF[:], op=ALU.mult), R=["r_pat", "identF"], W=["r_pat"])
            op("dve", lambda e: e.reduce_sum(out=pv[:, col:col + 1], in_=pat[:], axis=AX.X), R=["r_pat", "r_pv"], W=["r_pv"])

        per_part([[0, 8], [1, 16]], 0, 0)
        per_part([[0, 2], [1, 2], [0, 32]], 0, 2)
        per_part([[0, 4], [2, 2], [0, 16]], -1, 3)
        op("act", lambda e: e.activation(out=pv[:, 1:2], in_=pv[:, 0:1], func=AF.Exp, scale=-math.log(10000.0) / 16.0), R=["r_pv"], W=["r_pv"])
        op("pool", lambda e: e.iota(tok[:], pattern=[[1, 64], [0, 64]], base=0, channel_multiplier=0,
                                    allow_small_or_imprecise_dtypes=True), W=["r_tok"])
        op("pool", lambda e: e.iota(colp[:], pattern=[[0, 64], [1, 64]], base=0, channel_multiplier=0,
                                    allow_small_or_imprecise_dtypes=True), W=["r_col"])
        op("dve", lambda e: e.tensor_tensor(out=colp[:], in0=colp[:], in1=tok[:], op=ALU.subtract), R=["r_tok", "r_col"], W=["r_col"])
        op("dve", lambda e: e.scalar_tensor_tensor(out=ang[:], in0=colp[:], scalar=pv[:, 2:3], in1=tok[:], op0=ALU.mult, op1=ALU.add),
           R=["r_col", "r_tok", "r_pv"], W=["r_ang"])
        op("dve", lambda e: e.tensor_scalar(out=ang[:], in0=ang[:], scalar1=pv[:, 1:2], scalar2=None, op0=ALU.mult), R=["r_ang", "r_pv"], W=["r_ang"])

        def sin_of(src, key_src, dst, key_dst, shift):
            if shift != 0.0:
                op("dve", lambda e: e.tensor_scalar(out=dst, in0=src, scalar1=shift, scalar2=None, op0=ALU.add), R=[key_src], W=[key_dst])
                src, key_src = dst, key_dst
            op("dve", lambda e: e.tensor_scalar(out=tb[:], in0=src, scalar1=1.0 / (2 * math.pi), scalar2=None, op0=ALU.mult), R=[key_src], W=["r_tb"])
            op("dve", lambda e: e.tensor_copy(out=ti[:], in_=tb[:]), R=["r_tb"], W=["r_ti"])
            op("dve", lambda e: e.tensor_copy(out=tb[:], in_=ti[:]), R=["r_ti"], W=["r_tb"])
            op("dve", lambda e: e.scalar_tensor_tensor(out=dst, in0=tb[:], scalar=-2 * math.pi, in1=src, op0=ALU.mult, op1=ALU.add), R=["r_tb", key_src], W=[key_dst])
            op("act", lambda e: e.activation(out=dst, in_=dst, func=AF.Sin), R=[key_dst], W=[key_dst])

        sin_of(ang[:], "r_ang", colp[:], "r_col", 0.0)
        op("dve", lambda e: e.tensor_scalar(out=colp[:], in0=colp[:], scalar1=pv[:, 3:4], scalar2=None, op0=ALU.mult), R=["r_col", "r_pv"], W=["r_col"])
        dma("sp", lambda e: e.dma_start(out=sin_d, in_=colp[:]), R=["r_col"], W=["sin_d"])
        sin_of(ang[:], "r_ang", tok[:], "r_tok", 0.5 * math.pi)
        dma("sp", lambda e: e.dma_start(out=cos_d, in_=tok[:]), R=["r_tok"], W=["cos_d"])
        Bd.barrier()

    def bcast_load(q, dst, src_row, n):
        return lambda e: e.dma_start(out=dst, in_=src_row.partition_broadcast(128))

    def layer_norm_tile(xt, key_x, out_ap, key_out, scr, key_scr, st, key_st, n=D, mul_b=None, add_b=None, keys_b=()):
        op("dve", lambda e: e.reduce_sum(out=st[:, 0:1], in_=xt, axis=AX.X), R=[key_x], W=[key_st])
        op("act", lambda e: e.activation(out=scr, in_=xt, func=AF.Square), R=[key_x], W=[key_scr])
        op("dve", lambda e: e.reduce_sum(out=st[:, 1:2], in_=scr, axis=AX.X), R=[key_scr, key_st], W=[key_st])
        op("dve", lambda e: e.tensor_scalar(out=st[:, 2:3], in0=st[:, 0:1], scalar1=1.0 / n, scalar2=None, op0=ALU.mult), R=[key_st], W=[key_st])
        op("dve", lambda e: e.tensor_tensor(out=st[:, 3:4], in0=st[:, 2:3], in1=st[:, 2:3], op=ALU.mult), R=[key_st], W=[key_st])
        op("dve", lambda e: e.scalar_tensor_tensor(out=st[:, 4:5], in0=st[:, 1:2], scalar=1.0 / n, in1=st[:, 3:4], op0=ALU.mult, op1=ALU.subtract), R=[key_st], W=[key_st])
        op("act", lambda e: e.activation(out=st[:, 5:6], in_=st[:, 4:5], func=AF.Sqrt, bias=eps_t[:, 0:1], scale=1.0), R=[key_st, "eps_t"], W=[key_st])
        op("dve", lambda e: e.reciprocal(out=st[:, 5:6], in_=st[:, 5:6]), R=[key_st], W=[key_st])
        if mul_b is None:
            op("dve", lambda e: e.tensor_scalar(out=out_ap, in0=xt, scalar1=st[:, 2:3], scalar2=st[:, 5:6], op0=ALU.subtract, op1=ALU.mult),
               R=[key_x, key_st], W=[key_out])
        else:
            op("dve", lambda e: e.tensor_scalar(out=scr, in0=xt, scalar1=st[:, 2:3], scalar2=st[:, 5:6], op0=ALU.subtract, op1=ALU.mult),
               R=[key_x, key_st], W=[key_scr])
            op("dve", lambda e: e.tensor_tensor(out=scr, in0=scr, in1=mul_b, op=ALU.mult), R=[key_scr] + list(keys_b), W=[key_scr])
            op("dve", lambda e: e.tensor_tensor(out=out_ap, in0=scr, in1=add_b, op=ALU.add), R=[key_scr] + list(keys_b), W=[key_out])

    def transpose_to(src_bf, key_src, nchunk, dstT, key_dst, tcol, pbank):
        pt = psT[pbank]
        for k in range(nchunk):
            op("pe", lambda e, k=k: e.transpose(out=pt[:, k * 128:(k + 1) * 128], in_=src_bf[:, k * 128:(k + 1) * 128], identity=ident[:]),
               R=[key_src, "ident"], W=[f"psT{pbank}"])
        op("act", lambda e: e.copy(out=dstT[:, 0:nchunk, tcol:tcol + 128],
                                   in_=pt[:, 0:nchunk * 128].rearrange("p (k t) -> p k t", k=nchunk)),
           R=[f"psT{pbank}"], W=[key_dst])

    blocks = [(0, 256)] + [(256 + 512 * i, 512) for i in range(8)]

    for L in range(n_layers):
        last = L == DEPTH - 1
        lam_init = 0.8 - 0.6 * math.exp(-0.3 * L)
        src_lat = x_in if L == 0 else xA[NCTX:T, :]
        src_ctx = ctx_in if L == 0 else xA[0:NCTX, :]

        def src_rows(t0, n):
            return src_ctx[t0:t0 + n, :] if t0 < NCTX else src_lat[t0 - NCTX:t0 - NCTX + n, :]

        with ExitStack() as ph:
            cT = sb(ph, "m_cT", [128, 2, 8], F32)
            crep = sb(ph, "m_crep", [128, 2, 8, 128], BF16)
            wm = [sb(ph, f"m_wm{i}", [128, 8, 512], BF16) for i in range(2)]
            bm = sb(ph, "m_bm", [1, 6144], F32)
            row = sb(ph, "m_row", [1, 2, 6144], F32)
            with nc.allow_non_contiguous_dma(reason="tiny transposed load of c"):
                dma("sp", lambda e: e.dma_start(out=cT[:], in_=c_in.rearrange("r (k p) -> p r k", p=128)), W=["m_cT"])
            dma("sp", lambda e: e.dma_start(out=bm[:], in_=Wd["b_mod"][L:L + 1, :]), W=["m_bm"])
            op("act", lambda e: e.activation(out=cT[:], in_=cT[:], func=AF.Silu), R=["m_cT"], W=["m_cT"])
            for r in range(2):
                op("dve", lambda e, r=r: e.tensor_copy(out=crep[:, r, :, :], in_=cT[:, r, :].unsqueeze(2).to_broadcast([128, 8, 128])),
                   R=["m_cT"], W=["m_crep"])
            for ch in range(12):
                w = wm[ch % 2]
                dma("pool", lambda e, w=w, ch=ch: e.dma_start(out=w[:], in_=Wd["w_mod"][L, :, ch * 512:(ch + 1) * 512].rearrange("(k p) n -> p k n", p=128)),
                    W=[f"m_wm{ch % 2}"])
                for r in range(2):
                    ps = psA[(ch * 2 + r) % 4]
                    for k in range(8):
                        op("pe", lambda e, ps=ps, k=k, r=r, w=w: e.matmul(ps[:], lhsT=crep[:, r, k, :], rhs=w[:, k, :], start=(k == 0), stop=(k == 7)),
                           R=["m_crep", f"m_wm{ch % 2}"], W=[f"psA{(ch * 2 + r) % 4}"])
                    op("dve", lambda e, ps=ps, r=r, ch=ch: e.tensor_tensor(out=row[0:1, r, ch * 512:(ch + 1) * 512], in0=ps[0:1, :], in1=bm[0:1, ch * 512:(ch + 1) * 512], op=ALU.add),
                       R=[f"psA{(ch * 2 + r) % 4}", "m_bm"], W=["m_row"])
            for seg in (1, 4):
                op("dve", lambda e, seg=seg: e.tensor_scalar(out=row[0:1, :, seg * 1024:(seg + 1) * 1024], in0=row[0:1, :, seg * 1024:(seg + 1) * 1024],
                                                             scalar1=1.0, scalar2=None, op0=ALU.add), R=["m_row"], W=["m_row"])
            dma("sp", lambda e: e.dma_start(out=modv.rearrange("(o r) n -> o r n", o=1), in_=row[:]), R=["m_row"], W=["modv"])
            Bd.barrier()

        def mod_b(r, seg):
            return modv[r, seg * 1024:(seg + 1) * 1024]

        with ExitStack() as ph:
            win = sb(ph, "a_win", [128, 8, INC], BF16)
            for k in range(8):
                dma("pool", lambda e, k=k: e.dma_start(out=win[:, k, :], in_=Wd["w_in"][L, k * 128:(k + 1) * 128, :]), W=["a_win"])
            binT = sb(ph, "a_binT", [128, 52], F32)
            with nc.allow_non_contiguous_dma(reason="tiny transposed bias load"):
                dma("sp", lambda e: e.dma_start(out=binT[:], in_=Wd["b_in"][L, :].rearrange("(j p) -> p j", p=128)), W=["a_binT"])
            bvus = sb(ph, "a_bvus", [128, 1536], F32)
            dma("sp", bcast_load("sp", bvus[:], Wd["b_in"][L, OV:OV + 1536], 1536), W=["a_bvus"])
            modb = sb(ph, "a_modb", [128, 4, D], F32)
            for r in range(2):
                for j, seg in enumerate((0, 1)):
                    dma("sp", bcast_load("sp", modb[:, r * 2 + j, :], mod_b(r, seg), D), R=["modv"], W=["a_modb"])
            lng = sb(ph, "a_lng", [128, 2, 512], F32)
            dma("sp", bcast_load("sp", lng[:, 0, :], Wd["sg_ln_g"][L, :], 512), W=["a_lng"])
            dma("sp", bcast_load("sp", lng[:, 1, :], Wd["sg_ln_b"][L, :], 512), W=["a_lng"])
            wsf = sb(ph, "a_wsf", [128, 4, 128], F32)
            wsb = sb(ph, "a_wsb", [128, 4, 128], BF16)
            wsT = sb(ph, "a_wsT", [128, 4, 128], BF16)
            bsT = sb(ph, "a_bsT", [128, 4], F32)
            dma("sp", lambda e: e.dma_start(out=wsf[:], in_=Wd["sg_w"][L].rearrange("g p q -> p g q")), W=["a_wsf"])
            with nc.allow_non_contiguous_dma(reason="tiny transposed bias load"):
                dma("sp", lambda e: e.dma_start(out=bsT[:], in_=Wd["sg_b"][L].rearrange("g p -> p g")), W=["a_bsT"])
            op("dve", lambda e: e.tensor_copy(out=wsb[:], in_=wsf[:]), R=["a_wsf"], W=["a_wsb"])
            for g in range(4):
                op("pe", lambda e, g=g: e.transpose(out=psT[0][:, g * 128:(g + 1) * 128], in_=wsb[:, g, :], identity=ident[:]), R=["a_wsb", "ident"], W=["psT0"])
            op("act", lambda e: e.copy(out=wsT[:], in_=psT[0][:, 0:512].rearrange("p (g t) -> p g t", g=4)), R=["psT0"], W=["a_wsT"])

            xt = [sb(ph, f"a_xt{i}", [128, D], F32) for i in range(2)]
            scr = sb(ph, "a_scr", [128, D], F32)
            st = sb(ph, "a_st", [128, 8], F32)
            hb = sb(ph, "a_hb", [128, D], BF16)
            hT = sb(ph, "a_hT", [128, 8, 512], BF16)
            fo = [sb(ph, f"a_fo{i}", [128, 512], F32) for i in range(2)]
            fob = [sb(ph, f"a_fob{i}", [128, 512], BF16) for i in range(2)]
            rp = sb(ph, "a_rp", [128, 512], F32)
            cs = sb(ph, "a_cs", [128, 2, 512], F32)
            tmv = sb(ph, "a_tmv", [128, 512], F32)
            tmb = sb(ph, "a_tmb", [128, 512], BF16)
            gu = sb(ph, "a_gu", [128, 512], F32)
            gs_ = sb(ph, "a_gs", [128, 512], F32)
            gsq = sb(ph, "a_gsq", [128, 512], F32)
            gst = sb(ph, "a_gst", [128, 4, 4], F32)
            vgb = sb(ph, "a_vgb", [128, 512], BF16)
            ob = sb(ph, "a_ob", [128, 512], BF16)
            obT = sb(ph, "a_obT", [128, 4, 512], BF16)

            fcnt = [0]
            for (t0, nb) in blocks:
                is_ctx = t0 < NCTX
                mr = 1 if is_ctx else 0
                nsub = nb // 128
                for s_ in range(nsub):
                    xx = xt[s_ % 2]
                    kx = f"a_xt{s_ % 2}"
                    dma("sp", lambda e, xx=xx, s_=s_: e.dma_start(out=xx[:], in_=src_rows(t0 + s_ * 128, 128)), W=[kx])
                    layer_norm_tile(xx[:], kx, hb[:], "a_hb", scr[:], "a_scr", st, "a_st",
                                    mul_b=modb[:, mr * 2 + 1, :], add_b=modb[:, mr * 2, :], keys_b=["a_modb"])
                    transpose_to(hb, "a_hb", 8, hT, "a_hT", s_ * 128, s_ % 2)
                if not is_ctx:
                    l0 = t0 - NCTX
                    dma("sp", lambda e: e.dma_start(out=cs[:, 0, :], in_=cos_d[:, l0:l0 + 512]), R=["cos_d"], W=["a_cs"])
                    dma("sp", lambda e: e.dma_start(out=cs[:, 1, :], in_=sin_d[:, l0:l0 + 512]), R=["sin_d"], W=["a_cs"])
                fm_tiles = [("q", j) for j in range(4)] + [("k", j) for j in range(4)] + [("x", j) for j in range(4)] + \
                           [("y", j) for j in range(4)] + [("g", j) for j in range(24)]
                for kind, j in fm_tiles:
                    col0 = {"q": OQ, "k": OK_, "x": OX, "y": OY, "g": OG}[kind] + j * 128
                    jt = col0 // 128
                    pi = fcnt[0] % 4
                    fi = fcnt[0] % 2
                    fcnt[0] += 1
                    ps = psA[pi]
                    for k in range(8):
                        op("pe", lambda e, ps=ps, k=k, col0=col0: e.matmul(ps[:, 0:nb], lhsT=win[:, k, col0:col0 + 128], rhs=hT[:, k, 0:nb], start=(k == 0), stop=(k == 7)),
                           R=["a_win", "a_hT"], W=[f"psA{pi}"])
                    if kind in ("q", "k"):
                        dst = qT_d if kind == "q" else kT_d
                        if is_ctx:
                            op("act", lambda e, ps=ps, fi=fi, jt=jt: e.activation(out=fob[fi][:, 0:nb], in_=ps[:, 0:nb], func=AF.Identity, bias=binT[:, jt:jt + 1], scale=1.0),
                               R=[f"psA{pi}", "a_binT"], W=[f"a_fob{fi}"])
                            dma("sp", lambda e, fi=fi, dst=dst, j=j: e.dma_start(out=dst[j, :, t0:t0 + nb], in_=fob[fi][:, 0:nb]), R=[f"a_fob{fi}"], W=[kind + "T_d"])
                        else:
                            op("act", lambda e, ps=ps, fi=fi, jt=jt: e.activation(out=fo[fi][:, 0:nb], in_=ps[:, 0:nb], func=AF.Identity, bias=binT[:, jt:jt + 1], scale=1.0),
                               R=[f"psA{pi}", "a_binT"], W=[f"a_fo{fi}"])
                            op("dve", lambda e, fi=fi: e.tensor_copy(out=fob[fi][:, 0:nb], in_=fo[fi][:, 0:nb]), R=[f"a_fo{fi}"], W=[f"a_fob{fi}"])
                            op("pe", lambda e, fi=fi: e.matmul(psS[0][:, 0:nb], lhsT=Pm[:], rhs=fob[fi][:, 0:nb], start=True, stop=True),
                               R=["Pm", f"a_fob{fi}"], W=["psS0"])
                            op("dve", lambda e: e.tensor_tensor(out=rp[:, 0:nb], in0=psS[0][:, 0:nb], in1=cs[:, 1, 0:nb], op=ALU.mult), R=["psS0", "a_cs"], W=["a_rp"])
                            op("dve", lambda e, fi=fi: e.tensor_tensor(out=fo[fi][:, 0:nb], in0=fo[fi][:, 0:nb], in1=cs[:, 0, 0:nb], op=ALU.mult), R=[f"a_fo{fi}", "a_cs"], W=[f"a_fo{fi}"])
                            op("dve", lambda e, fi=fi: e.tensor_tensor(out=fob[fi][:, 0:nb], in0=fo[fi][:, 0:nb], in1=rp[:, 0:nb], op=ALU.add), R=[f"a_fo{fi}", "a_rp"], W=[f"a_fob{fi}"])
                            dma("sp", lambda e, fi=fi, dst=dst, j=j: e.dma_start(out=dst[j, :, t0:t0 + nb], in_=fob[fi][:, 0:nb]), R=[f"a_fob{fi}"], W=[kind + "T_d"])
                    elif kind == "x":
                        op("act", lambda e, ps=ps, fi=fi, jt=jt: e.activation(out=fo[fi][:, 0:nb], in_=ps[:, 0:nb], func=AF.Identity, bias=binT[:, jt:jt + 1], scale=1.0),
                           R=[f"psA{pi}", "a_binT"], W=[f"a_fo{fi}"])
                        dma("sp", lambda e, fi=fi, j=j: e.dma_start(out=xT_d[j * 128:(j + 1) * 128, t0:t0 + nb], in_=fo[fi][:, 0:nb]), R=[f"a_fo{fi}"], W=["xT_d"])
                    elif kind == "y":
                        op("act", lambda e, ps=ps, fi=fi, jt=jt: e.activation(out=fob[fi][:, 0:nb], in_=ps[:, 0:nb], func=AF.Gelu_apprx_tanh, bias=binT[:, jt:jt + 1], scale=1.0),
                           R=[f"psA{pi}", "a_binT"], W=[f"a_fob{fi}"])
                        dma("sp", lambda e, fi=fi, j=j: e.dma_start(out=yT_d[j * 128:(j + 1) * 128, t0:t0 + nb], in_=fob[fi][:, 0:nb]), R=[f"a_fob{fi}"], W=["yT_d"])
                    else:
                        op("act", lambda e, ps=ps, fi=fi, jt=jt: e.activation(out=fob[fi][:, 0:nb], in_=ps[:, 0:nb], func=AF.Sigmoid, bias=binT[:, jt:jt + 1], scale=1.0),
                           R=[f"psA{pi}", "a_binT"], W=[f"a_fob{fi}"])
                        dma("sp", lambda e, fi=fi, j=j: e.dma_start(out=gT_d[j * 128:(j + 1) * 128, t0:t0 + nb], in_=fob[fi][:, 0:nb]), R=[f"a_fob{fi}"], W=["gT_d"])
                for s_ in range(nsub):
                    tt = t0 + s_ * 128
                    for wi, (kind, col0) in enumerate((("v", OV), ("u", OU), ("s", OS))):
                        pi = fcnt[0] % 4
                        fcnt[0] += 1
                        ps = psA[pi]
                        for k in range(8):
                            op("pe", lambda e, ps=ps, k=k, col0=col0, s_=s_: e.matmul(ps[:], lhsT=hT[:, k, s_ * 128:(s_ + 1) * 128], rhs=win[:, k, col0:col0 + 512], start=(k == 0), stop=(k == 7)),
                               R=["a_win", "a_hT"], W=[f"psA{pi}"])
                        if kind == "v":
                            op("dve", lambda e, ps=ps: e.tensor_tensor(out=tmb[:], in0=ps[:], in1=bvus[:, 0:512], op=ALU.add), R=[f"psA{pi}", "a_bvus"], W=["a_tmb"])
                            dma("sp", lambda e, tt=tt: e.dma_start(out=v_d[tt:tt + 128, :], in_=tmb[:]), R=["a_tmb"], W=["v_d"])
                        elif kind == "u":
                            op("dve", lambda e, ps=ps: e.tensor_tensor(out=tmv[:], in0=ps[:], in1=bvus[:, 512:1024], op=ALU.add), R=[f"psA{pi}", "a_bvus"], W=["a_tmv"])
                            op("act", lambda e: e.activation(out=gu[:], in_=tmv[:], func=AF.Gelu_apprx_tanh), R=["a_tmv"], W=["a_gu"])
                        else:
                            op("dve", lambda e, ps=ps: e.tensor_tensor(out=tmv[:], in0=ps[:], in1=bvus[:, 1024:1536], op=ALU.add), R=[f"psA{pi}", "a_bvus"], W=["a_tmv"])
                            op("act", lambda e: e.activation(out=gs_[:], in_=tmv[:], func=AF.Gelu_apprx_tanh), R=["a_tmv"], W=["a_gs"])
                    g3 = gs_[:].rearrange("p (g c) -> p g c", g=4)
                    op("dve", lambda e: e.reduce_sum(out=gst[:, 0, :], in_=g3, axis=AX.X), R=["a_gs"], W=["a_gst"])
                    op("act", lambda e: e.activation(out=gsq[:], in_=gs_[:], func=AF.Square), R=["a_gs"], W=["a_gsq"])
                    op("dve", lambda e: e.reduce_sum(out=gst[:, 1, :], in_=gsq[:].rearrange("p (g c) -> p g c", g=4), axis=AX.X), R=["a_gsq", "a_gst"], W=["a_gst"])
                    op("dve", lambda e: e.tensor_scalar(out=gst[:, 0, :], in0=gst[:, 0, :], scalar1=1.0 / 128, scalar2=None, op0=ALU.mult), R=["a_gst"], W=["a_gst"])
                    op("dve", lambda e: e.tensor_tensor(out=gst[:, 2, :], in0=gst[:, 0, :], in1=gst[:, 0, :], op=ALU.mult), R=["a_gst"], W=["a_gst"])
                    op("dve", lambda e: e.scalar_tensor_tensor(out=gst[:, 1, :], in0=gst[:, 1, :], scalar=1.0 / 128, in1=gst[:, 2, :], op0=ALU.mult, op1=ALU.subtract), R=["a_gst"], W=["a_gst"])
                    op("act", lambda e: e.activation(out=gst[:, 3, :], in_=gst[:, 1, :], func=AF.Sqrt, bias=eps_t[:, 0:1], scale=1.0), R=["a_gst", "eps_t"], W=["a_gst"])
                    op("dve", lambda e: e.reciprocal(out=gst[:, 3, :], in_=gst[:, 3, :]), R=["a_gst"], W=["a_gst"])
                    op("dve", lambda e: e.tensor_tensor(out=gsq[:].rearrange("p (g c) -> p g c", g=4), in0=g3, in1=gst[:, 0, :].unsqueeze(2).to_broadcast([128, 4, 128]), op=ALU.subtract),
                       R=["a_gs", "a_gst"], W=["a_gsq"])
                    op("dve", lambda e: e.tensor_tensor(out=gsq[:].rearrange("p (g c) -> p g c", g=4), in0=gsq[:].rearrange("p (g c) -> p g c", g=4),
                                                        in1=gst[:, 3, :].unsqueeze(2).to_broadcast([128, 4, 128]), op=ALU.mult), R=["a_gsq", "a_gst"], W=["a_gsq"])
                    op("dve", lambda e: e.tensor_tensor(out=gsq[:], in0=gsq[:], in1=lng[:, 0, :], op=ALU.mult), R=["a_gsq", "a_lng"], W=["a_gsq"])
                    op("dve", lambda e: e.tensor_tensor(out=vgb[:], in0=gsq[:], in1=lng[:, 1, :], op=ALU.add), R=["a_gsq", "a_lng"], W=["a_vgb"])
                    pi = fcnt[0] % 4
                    fcnt[0] += 1
                    ps = psA[pi]
                    for g in range(4):
                        op("pe", lambda e, ps=ps, g=g: e.matmul(ps[:, g * 128:(g + 1) * 128], lhsT=wsT[:, g, :], rhs=vgb[:, g * 128:(g + 1) * 128], start=True, stop=True),
                           R=["a_wsT", "a_vgb"], W=[f"psA{pi}"])
                    for g in range(4):
                        op("dve", lambda e, ps=ps, g=g: e.scalar_tensor_tensor(out=ob[:, g * 128:(g + 1) * 128], in0=ps[:, g * 128:(g + 1) * 128], scalar=bsT[:, g:g + 1],
                                                                               in1=gu[:, g * 128:(g + 1) * 128], op0=ALU.add, op1=ALU.mult),
                           R=[f"psA{pi}", "a_bsT", "a_gu"], W=["a_ob"])
                    transpose_to(ob, "a_ob", 4, obT, "a_obT", s_ * 128, s_ % 2)
                for cc in range(4):
                    dma("sp", lambda e, cc=cc: e.dma_start(out=obT_d[cc * 128:(cc + 1) * 128, t0:t0 + nb], in_=obT[:, cc, 0:nb]), R=["a_obT"], W=["obT_d"])
            Bd.barrier()

        if "stopA" in debug:
            break

        with ExitStack() as ph:
            xp = sb(ph, "b_xp", [128, T + 8], F32)
            xs = sb(ph, "b_xs", [128, T], F32)
            xsb = sb(ph, "b_xsb", [128, T], BF16)
            r_ = sb(ph, "b_r", [128, T], F32)
            i_ = sb(ph, "b_i", [128, T], F32)
            a2 = sb(ph, "b_a2", [128, T], F32)
            hf = sb(ph, "b_hf", [128, T], F32)
            hb_ = sb(ph, "b_hb", [128, T], F32)
            yb = sb(ph, "b_yb", [128, T], BF16)
            oc = sb(ph, "b_oc", [128, T], BF16)
            cw = sb(ph, "b_cw", [128, 4, 4], F32)
            cb = sb(ph, "b_cb", [128, 4], F32)
            gb = sb(ph, "b_gb", [128, 3, 2, 4], F32)
            cn = sb(ph, "b_cn", [128, 2, 2, 4], F32)
            one_t = sb(ph, "b_one", [128, 1], F32)
            wst = sb(ph, "b_wst", [128, 16, 128], F32)
            wbd = sb(ph, "b_wbd", [128, 16, 128], BF16)
            op("dve", lambda e: e.memset(xp[:], 0.0), W=["b_xp"])
            op("dve", lambda e: e.memset(one_t[:], 1.0), W=["b_one"])
            op("pool", lambda e: e.memset(wst[:], 0.0), W=["b_wst"])
            with nc.allow_non_contiguous_dma(reason="tiny per-channel parameter loads"):
                for j in range(4):
                    dma("sp", lambda e, j=j: e.dma_start(out=cw[:, :, j], in_=Wd["conv_w"][L, j].rearrange("(ct p) -> p ct", p=128)), R=["b_cw"], W=["b_cw"])
                dma("sp", lambda e: e.dma_start(out=cb[:], in_=Wd["conv_b"][L].rearrange("(ct p) -> p ct", p=128)), W=["b_cb"])
                for wi, nm in enumerate(("lru_ba", "lru_bx", "lru_lam")):
                    for d in range(2):
                        dma("sp", lambda e, wi=wi, nm=nm, d=d: e.dma_start(out=gb[:, wi, d, :], in_=Wd[nm][L, d].rearrange("(ct p) -> p ct", p=128)), R=["b_gb"], W=["b_gb"])
            for d in range(2):
                for gi, nm in enumerate(("lru_wa", "lru_wx")):
                    for ct in range(4):
                        idx = (d * 2 + gi) * 4 + ct
                        for hh in range(2):
                            dma("sp", lambda e, idx=idx, hh=hh, nm=nm, d=d, ct=ct: e.dma_start(
                                out=wst[hh * 64:(hh + 1) * 64, idx, hh * 64:(hh + 1) * 64], in_=Wd[nm][L, d, 2 * ct + hh]), R=["b_wst"], W=["b_wst"])
            op("dve", lambda e: e.tensor_copy(out=wbd[:], in_=wst[:]), R=["b_wst"], W=["b_wbd"])
            op("act", lambda e: e.activation(out=cn[:, 0, :, :], in_=gb[:, 2, :, :], func=AF.Exp, scale=-1.0), R=["b_gb"], W=["b_cn"])
            op("act", lambda e: e.activation(out=cn[:, 0, :, :], in_=cn[:, 0, :, :], func=AF.Ln, bias=one_t[:, 0:1], scale=1.0), R=["b_cn", "b_one"], W=["b_cn"])
            op("dve", lambda e: e.tensor_scalar(out=cn[:, 1, :, :], in0=cn[:, 0, :, :], scalar1=-16.0, scalar2=None, op0=ALU.mult), R=["b_cn"], W=["b_cn"])
            op("dve", lambda e: e.tensor_scalar(out=cn[:, 0, :, :], in0=cn[:, 0, :, :], scalar1=-8.0, scalar2=None, op0=ALU.mult), R=["b_cn"], W=["b_cn"])
            chunks = [(i * 512, 512) for i in range(8)] + [(4096, 256)]
            pcn = [0]
            for ct in range(4):
                dma("sp", lambda e, ct=ct: e.dma_start(out=xp[:, 2:2 + NCTX], in_=xT_d[ct * 128:(ct + 1) * 128, 0:NCTX]), R=["xT_d"], W=["b_xp"])
                dma("sp", lambda e, ct=ct: e.dma_start(out=xp[:, 6 + NCTX:6 + T], in_=xT_d[ct * 128:(ct + 1) * 128, NCTX:T]), R=["xT_d", "b_xp"], W=["b_xp"])
                dma("sp", lambda e, ct=ct: e.dma_start(out=yb[:], in_=yT_d[ct * 128:(ct + 1) * 128, :]), R=["yT_d"], W=["b_yb"])
                for (base, n, o0) in ((0, NCTX, 0), (4 + NCTX, NLAT, NCTX)):
                    op("dve", lambda e, base=base, n=n, o0=o0, ct=ct: e.tensor_scalar(out=xs[:, o0:o0 + n], in0=xp[:, base:base + n], scalar1=cw[:, ct, 0:1], scalar2=cb[:, ct:ct + 1],
                                                                                  op0=ALU.mult, op1=ALU.add), R=["b_xp", "b_cw", "b_cb"], W=["b_xs"])
                    for j in range(1, 4):
                        op("dve", lambda e, base=base, n=n, o0=o0, ct=ct, j=j: e.scalar_tensor_tensor(out=xs[:, o0:o0 + n], in0=xp[:, base + j:base + j + n], scalar=cw[:, ct, j:j + 1],
                                                                                                   in1=xs[:, o0:o0 + n], op0=ALU.mult, op1=ALU.add), R=["b_xp", "b_cw", "b_xs"], W=["b_xs"])
                op("act", lambda e: e.copy(out=xsb[:], in_=xs[:]), R=["b_xs"], W=["b_xsb"])
                for d in range(2):
                    for gi, (gt, gk) in enumerate(((r_, "b_r"), (i_, "b_i"))):
                        idx = (d * 2 + gi) * 4 + ct
                        for (c0, cnb) in chunks:
                            pi = pcn[0] % 4
                            pcn[0] += 1
                            op("pe", lambda e, pi=pi, idx=idx, c0=c0, cnb=cnb: e.matmul(psA[pi][:, 0:cnb], lhsT=wbd[:, idx, :], rhs=xsb[:, c0:c0 + cnb], start=True, stop=True),
                               R=["b_wbd", "b_xsb"], W=[f"psA{pi}"])
                            op("act", lambda e, pi=pi, gt=gt, gi=gi, d=d, ct=ct, c0=c0, cnb=cnb: e.activation(out=gt[:, c0:c0 + cnb], in_=psA[pi][:, 0:cnb], func=AF.Sigmoid,
                                                                                                       bias=gb[:, gi, d, ct:ct + 1], scale=1.0), R=[f"psA{pi}", "b_gb"], W=[gk])
                    op("act", lambda e, d=d, ct=ct: e.activation(out=a2[:], in_=r_[:], func=AF.Exp, scale=cn[:, 1, d, ct:ct + 1]), R=["b_r", "b_cn"], W=["b_a2"])
                    op("act", lambda e, d=d, ct=ct: e.activation(out=r_[:], in_=r_[:], func=AF.Exp, scale=cn[:, 0, d, ct:ct + 1]), R=["b_r", "b_cn"], W=["b_r"])
                    op("act", lambda e: e.activation(out=a2[:], in_=a2[:], func=AF.Sqrt, bias=one_t[:, 0:1], scale=-1.0), R=["b_a2", "b_one"], W=["b_a2"])
                    op("dve", lambda e: e.tensor_tensor(out=i_[:], in0=i_[:], in1=xs[:], op=ALU.mult), R=["b_i", "b_xs"], W=["b_i"])
                    op("dve", lambda e: e.tensor_tensor(out=i_[:], in0=i_[:], in1=a2[:], op=ALU.mult), R=["b_i", "b_a2"], W=["b_i"])
                    if d == 0:
                        op("dve", lambda e: e.tensor_tensor_scan(out=hf[:], data0=r_[:], data1=i_[:], initial=0.0, op0=ALU.mult, op1=ALU.add), R=["b_r", "b_i"], W=["b_hf"])
                    else:
                        op("dve", lambda e: e.tensor_tensor_scan(out=hb_[:, 0:NCTX][:, ::-1], data0=r_[:, 0:NCTX][:, ::-1], data1=i_[:, 0:NCTX][:, ::-1], initial=0.0,
                                                                 op0=ALU.mult, op1=ALU.add), R=["b_r", "b_i"], W=["b_hb"])
                        op("dve", lambda e: e.tensor_tensor_scan(out=hb_[:, NCTX:T][:, ::-1], data0=r_[:, NCTX:T][:, ::-1], data1=i_[:, NCTX:T][:, ::-1], initial=hb_[:, 0:1],
                                                                 op0=ALU.mult, op1=ALU.add), R=["b_r", "b_i", "b_hb"], W=["b_hb"])
                op("dve", lambda e: e.tensor_tensor(out=hf[:], in0=hf[:], in1=hb_[:], op=ALU.add), R=["b_hf", "b_hb"], W=["b_hf"])
                op("dve", lambda e: e.tensor_tensor(out=oc[:], in0=hf[:], in1=yb[:], op=ALU.mult), R=["b_hf", "b_yb"], W=["b_oc"])
                dma("sp", lambda e, ct=ct: e.dma_start(out=ocT_d[ct * 128:(ct + 1) * 128, :], in_=oc[:]), R=["b_oc"], W=["ocT_d"])
            Bd.barrier()

        if "stopB" in debug:
            break

        with ExitStack() as ph:
            KT = sb(ph, "c_KT", [128, 4, T], BF16)
            VA = sb(ph, "c_VA", [128, NT, 4, 132], BF16)
            QT = [sb(ph, f"c_QT{i}", [128, 512], BF16) for i in range(2)]
            Pb = [sb(ph, f"c_P{i}", [128, 512], BF16) for i in range(3)]
            lq = sb(ph, "c_lq", [128, 4, 64], F32)
            lamt = sb(ph, "c_lamt", [128, 4], F32)
            gain = sb(ph, "c_gain", [128, 512], F32)
            sm = sb(ph, "c_sm", [128, 8], F32)
            o1 = sb(ph, "c_o1", [128, 128], F32)
            o2 = sb(ph, "c_o2", [128, 128], F32)
            oab = sb(ph, "c_oab", [128, 128], BF16)
            oaT = sb(ph, "c_oaT", [128, 4, 512], BF16)
            op("dve", lambda e: e.memset(VA[:], 1.0), W=["c_VA"])
            for h in range(4):
                dma("sp", lambda e, h=h: e.dma_start(out=KT[:, h, :], in_=kT_d[h]), R=["kT_d"], W=["c_KT"])
            for kt in range(NT):
                dma("sp", lambda e, kt=kt: e.dma_start(out=VA[:, kt, :, 0:128], in_=v_d[kt * 128:(kt + 1) * 128, :].rearrange("p (h e) -> p h e", h=4)), R=["v_d", "c_VA"], W=["c_VA"])
            for i, nm in enumerate(("lam_q1", "lam_k1", "lam_q2", "lam_k2")):
                dma("sp", bcast_load("sp", lq[:, i, :], Wd[nm][L, :], 64), R=["c_lq"], W=["c_lq"])
            dma("sp", bcast_load("sp", gain[:], Wd["attn_norm_g"][L].rearrange("h e -> (h e)"), 512), W=["c_gain"])
            op("dve", lambda e: e.tensor_scalar(out=gain[:], in0=gain[:], scalar1=(1.0 - lam_init), scalar2=None, op0=ALU.mult), R=["c_gain"], W=["c_gain"])
            for i in range(2):
                op("dve", lambda e, i=i: e.tensor_tensor(out=lq[:, 2 * i, :], in0=lq[:, 2 * i, :], in1=lq[:, 2 * i + 1, :], op=ALU.mult), R=["c_lq"], W=["c_lq"])
                op("dve", lambda e, i=i: e.reduce_sum(out=lamt[:, i:i + 1], in_=lq[:, 2 * i, :], axis=AX.X), R=["c_lq", "c_lamt"], W=["c_lamt"])
            op("act", lambda e: e.activation(out=lamt[:, 0:2], in_=lamt[:, 0:2], func=AF.Exp), R=["c_lamt"], W=["c_lamt"])
            op("dve", lambda e: e.tensor_tensor(out=lamt[:, 2:3], in0=lamt[:, 0:1], in1=lamt[:, 1:2], op=ALU.subtract), R=["c_lamt"], W=["c_lamt"])
            op("dve", lambda e: e.tensor_scalar(out=lamt[:, 2:3], in0=lamt[:, 2:3], scalar1=lam_init, scalar2=None, op0=ALU.add), R=["c_lamt"], W=["c_lamt"])
            if "lamt" in debug:
                dma("sp", lambda e: e.dma_start(out=dbg_d[0:128, 0:4], in_=lamt[:]), R=["c_lamt"], W=["dbg_d"])

            def acc(c, qs):
                return psA[c * 2 + qs // 2][:, (qs % 2) * 256:(qs % 2) * 256 + 129], f"psA{c * 2 + qs // 2}"

            sc_ = [0]
            qblocks = blocks if not last else blocks[1:]
            zf = [0]
            ZROWS = 2048
            if L == 0:
                ztile = sb(ph, "c_zero", [128, (ZROWS // 128) * D], BF16)
                op("pool", lambda e: e.memset(ztile[:], 0.0), W=["c_zero"])
            for (t0, nb) in qblocks:
                nsub = nb // 128
                key_tiles = list(range(2)) if t0 < NCTX else list(range(NT))
                nkt = len(key_tiles)
                for h in range(4):
                    qi = h % 2
                    if L == 0:
                        for _ in range(2):
                            if zf[0] * ZROWS < NSLOT:
                                r0 = zf[0] * ZROWS
                                zf[0] += 1
                                dma("sp", lambda e, r0=r0: e.dma_start(out=Xg[r0:r0 + ZROWS, :].rearrange("(p r) d -> p (r d)", p=128), in_=ztile[:]), R=["c_zero"], W=["Xg"])
                    dma("sp", lambda e, qi=qi, h=h: e.dma_start(out=QT[qi][:, 0:nb], in_=qT_d[h, :, t0:t0 + nb]), R=["qT_d"], W=[f"c_QT{qi}"])
                    seq = [(c, ki, kt) for c in range(2) for ki, kt in enumerate(key_tiles)]
                    base = sc_[0]
                    sc_[0] += len(seq)

                    def emit_qk(i, qi=qi, h=h, seq=seq, base=base):
                        c, ki, kt = seq[i]
                        si = (base + i) % 2
                        op("pe", lambda e: e.matmul(psS[si][:, 0:nb], lhsT=KT[c * 64:(c + 1) * 64, h, kt * 128:(kt + 1) * 128],
                                                    rhs=QT[qi][c * 64:(c + 1) * 64, 0:nb], start=True, stop=True),
                           R=["c_KT", f"c_QT{qi}"], W=[f"psS{si}"])

                    emit_qk(0)
                    for i in range(len(seq)):
                        c, ki, kt = seq[i]
                        si = (base + i) % 2
                        pj = (base + i) % 3
                        if i + 1 < len(seq):
                            emit_qk(i + 1)
                        op("act", lambda e, si=si, pj=pj: e.activation(out=Pb[pj][:, 0:nb], in_=psS[si][:, 0:nb], func=AF.Exp, scale=0.125), R=[f"psS{si}"], W=[f"c_P{pj}"])
                        for qs in range(nsub):
                            a_ap, a_key = acc(c, qs)
                            op("pe", lambda e, a_ap=a_ap, pj=pj, qs=qs, kt=kt, h=h, ki=ki: e.matmul(a_ap, lhsT=Pb[pj][:, qs * 128:(qs + 1) * 128], rhs=VA[:, kt, h, 0:129],
                                                                                             start=(ki == 0 and qs % 2 == 0), stop=(ki == nkt - 1)),
                               R=[f"c_P{pj}", "c_VA"], W=[a_key])
                    for qs in range(nsub):
                        a0, k0 = acc(0, qs)
                        a1, k1 = acc(1, qs)
                        op("dve", lambda e, a0=a0: e.reciprocal(out=sm[:, 0:1], in_=a0[:, 128:129]), R=[k0], W=["c_sm"])
                        op("dve", lambda e, a1=a1: e.reciprocal(out=sm[:, 1:2], in_=a1[:, 128:129]), R=[k1, "c_sm"], W=["c_sm"])
                        op("dve", lambda e: e.tensor_tensor(out=sm[:, 1:2], in0=sm[:, 1:2], in1=lamt[:, 2:3], op=ALU.mult), R=["c_sm", "c_lamt"], W=["c_sm"])
                        op("dve", lambda e, a1=a1: e.tensor_scalar(out=o2[:], in0=a1[:, 0:128], scalar1=sm[:, 1:2], scalar2=None, op0=ALU.mult), R=[k1, "c_sm"], W=["c_o2"])
                        op("dve", lambda e, a0=a0: e.scalar_tensor_tensor(out=o1[:], in0=a0[:, 0:128], scalar=sm[:, 0:1], in1=o2[:], op0=ALU.mult, op1=ALU.subtract),
                           R=[k0, "c_sm", "c_o2"], W=["c_o1"])
                        op("act", lambda e: e.activation(out=o2[:], in_=o1[:], func=AF.Square), R=["c_o1"], W=["c_o2"])
                        op("dve", lambda e: e.reduce_sum(out=sm[:, 2:3], in_=o2[:], axis=AX.X), R=["c_o2", "c_sm"], W=["c_sm"])
                        op("act", lambda e: e.activation(out=sm[:, 3:4], in_=sm[:, 2:3], func=AF.Sqrt, bias=eps_t[:, 1:2], scale=1.0 / 128), R=["c_sm", "eps_t"], W=["c_sm"])
                        op("dve", lambda e: e.reciprocal(out=sm[:, 3:4], in_=sm[:, 3:4]), R=["c_sm"], W=["c_sm"])
                        op("dve", lambda e, h=h: e.scalar_tensor_tensor(out=oab[:], in0=o1[:], scalar=sm[:, 3:4], in1=gain[:, h * 128:(h + 1) * 128], op0=ALU.mult, op1=ALU.mult),
                           R=["c_o1", "c_sm", "c_gain"], W=["c_oab"])
                        op("pe", lambda e: e.transpose(out=psT[0][:, 0:128], in_=oab[:], identity=ident[:]), R=["c_oab", "ident"], W=["psT0"])
                        op("act", lambda e, h=h, qs=qs: e.copy(out=oaT[:, h, qs * 128:(qs + 1) * 128], in_=psT[0][:, 0:128]), R=["psT0"], W=["c_oaT"])
                for h in range(4):
                    dma("sp", lambda e, h=h: e.dma_start(out=oaT_d[h * 128:(h + 1) * 128, t0:t0 + nb], in_=oaT[:, h, 0:nb]), R=["c_oaT"], W=["oaT_d"])
            Bd.barrier()

        if "stopC" in debug:
            break

        with ExitStack() as ph:
            wb = sb(ph, "d_wb", [128, 12, D], BF16)
            wo = sb(ph, "d_wo", [128, 8, D], BF16)
            wr = sb(ph, "d_wr", [128, 8, 64], BF16)
            wsg = sb(ph, "d_wsg", [128, 8, 256], BF16)
            wsu = sb(ph, "d_wsu", [128, 8, 256], BF16)
            wsd = sb(ph, "d_wsd", [128, 2, D], BF16)
            modb2 = sb(ph, "d_modb", [128, 6, D], F32)
            lnb = sb(ph, "d_lnb", [128, 2, D], F32)
            rbias = sb(ph, "d_rbias", [128, 64], F32)
            oT = sb(ph, "d_oT", [128, 3, 4, 512], BF16)
            gT = sb(ph, "d_gT", [128, 24, 512], BF16)
            mT = sb(ph, "d_mT", [128, 8, 512], BF16)
            tm = [sb(ph, f"d_tm{i}", [128, 512], F32) for i in range(2)]
            xt = [sb(ph, f"d_xt{i}", [128, D], F32) for i in range(2)]
            r1 = sb(ph, "d_r1", [128, D], F32)
            scr = sb(ph, "d_scr", [128, D], F32)
            st = sb(ph, "d_st", [128, 8], F32)
            x1t = sb(ph, "d_x1t", [128, D], F32)
            h2b = [sb(ph, f"d_h2b{i}", [128, D], BF16) for i in range(2)]
            h2T = sb(ph, "d_h2T", [128, 8, 512], BF16)
            sgt = sb(ph, "d_sgt", [128, 512], F32)
            actT = sb(ph, "d_actT", [128, 2, 512], BF16)
            ysh = sb(ph, "d_ysh", [128, D], F32)
            rs_sc = sb(ph, "d_rsc", [128, 64], F32)
            rs_bs = sb(ph, "d_rbs", [128, 64], F32)
            rs_t = sb(ph, "d_rt", [128, 64], F32)
            rs_sel = sb(ph, "d_rsel", [128, 64], F32)
            rs_selb = sb(ph, "d_rselb", [128, 64], BF16)
            rs_wd = sb(ph, "d_rwd", [128, 64], F32)
            rs_da = sb(ph, "d_rda", [128, 64], F32)
            rs_g = sb(ph, "d_rg", [128, 6, 8], F32)
            rs_m8 = sb(ph, "d_rm8", [128, 8], F32)
            rs_d8 = sb(ph, "d_rd8", [128, 8], F32)
            rs_s = sb(ph, "d_rs", [128, 4], F32)
            rs_m3 = sb(ph, "d_rm3", [128, 8, 64], F32)
            Rrun = sb(ph, "d_Rrun", [128, 64], F32)
            Rrunb = sb(ph, "d_Rrunb", [128, 64], BF16)
            op("dve", lambda e: e.memset(Rrun[:], 0.0), W=["d_Rrun"])
            for r in range(3):
                dma("pool", lambda e, r=r: e.dma_start(out=wb[:, r * 4:(r + 1) * 4, :], in_=Wd["w_branch"][L, r].rearrange("(cc p) n -> p cc n", p=128)), R=["d_wb"], W=["d_wb"])
            dma("pool", lambda e: e.dma_start(out=wo[:], in_=Wd["w_out"][L].rearrange("(k p) n -> p k n", p=128)), W=["d_wo"])
            dma("pool", lambda e: e.dma_start(out=wr[:], in_=Wd["w_router"][L].rearrange("(k p) n -> p k n", p=128)), W=["d_wr"])
            dma("pool", lambda e: e.dma_start(out=wsg[:], in_=Wd["sh_w_gate"][L].rearrange("(k p) n -> p k n", p=128)), W=["d_wsg"])
            dma("pool", lambda e: e.dma_start(out=wsu[:], in_=Wd["sh_w_up"][L].rearrange("(k p) n -> p k n", p=128)), W=["d_wsu"])
            dma("pool", lambda e: e.dma_start(out=wsd[:], in_=Wd["sh_w_down"][L].rearrange("(k p) n -> p k n", p=128)), W=["d_wsd"])
            for r in range(2):
                for j, seg in enumerate((2, 3, 4)):
                    dma("sp", bcast_load("sp", modb2[:, r * 3 + j, :], mod_b(r, seg), D), R=["modv", "d_modb"], W=["d_modb"])
            dma("sp", bcast_load("sp", lnb[:, 0, :], Wd["ln1_g"][L, :], D), R=["d_lnb"], W=["d_lnb"])
            dma("sp", bcast_load("sp", lnb[:, 1, :], Wd["ln1_b"][L, :], D), R=["d_lnb"], W=["d_lnb"])
            dma("sp", bcast_load("sp", rbias[:], Wd["router_bias"][L, :], 64), W=["d_rbias"])

            pc = [0]
            dblocks = blocks if not last else blocks[1:]
            for (t0, nb) in dblocks:
                mr = 1 if t0 < NCTX else 0
                nsub = nb // 128
                for r, srcT in enumerate((oaT_d, obT_d, ocT_d)):
                    dma("sp", lambda e, r=r, srcT=srcT: e.dma_start(out=oT[:, r, :, 0:nb], in_=srcT[:, t0:t0 + nb].rearrange("(cc p) t -> p cc t", p=128)),
                        R=[("oaT_d", "obT_d", "ocT_d")[r], "d_oT"], W=["d_oT"])
                dma("sp", lambda e: e.dma_start(out=gT[:, :, 0:nb], in_=gT_d[:, t0:t0 + nb].rearrange("(j p) t -> p j t", p=128)), R=["gT_d"], W=["d_gT"])
                for dt in range(8):
                    pss = []
                    for r in range(3):
                        pi = pc[0] % 4
                        pc[0] += 1
                        pss.append(pi)
                        for cc in range(4):
                            op("pe", lambda e, pi=pi, r=r, cc=cc, dt=dt: e.matmul(psA[pi][:, 0:nb], lhsT=wb[:, r * 4 + cc, dt * 128:(dt + 1) * 128], rhs=oT[:, r, cc, 0:nb],
                                                                                  start=(cc == 0), stop=(cc == 3)), R=["d_wb", "d_oT"], W=[f"psA{pi}"])
                    op("dve", lambda e, dt=dt, p0=pss[0]: e.tensor_tensor(out=tm[0][:, 0:nb], in0=psA[p0][:, 0:nb], in1=gT[:, dt, 0:nb], op=ALU.mult), R=[f"psA{pss[0]}", "d_gT"], W=["d_tm0"])
                    op("dve", lambda e, dt=dt, p1=pss[1]: e.tensor_tensor(out=tm[1][:, 0:nb], in0=psA[p1][:, 0:nb], in1=gT[:, 8 + dt, 0:nb], op=ALU.mult), R=[f"psA{pss[1]}", "d_gT"], W=["d_tm1"])
                    op("dve", lambda e: e.tensor_tensor(out=tm[0][:, 0:nb], in0=tm[0][:, 0:nb], in1=tm[1][:, 0:nb], op=ALU.add), R=["d_tm0", "d_tm1"], W=["d_tm0"])
                    op("dve", lambda e, dt=dt, p2=pss[2]: e.tensor_tensor(out=tm[1][:, 0:nb], in0=psA[p2][:, 0:nb], in1=gT[:, 16 + dt, 0:nb], op=ALU.mult), R=[f"psA{pss[2]}", "d_gT", "d_tm1"], W=["d_tm1"])
                    op("dve", lambda e, dt=dt: e.tensor_tensor(out=mT[:, dt, 0:nb], in0=tm[0][:, 0:nb], in1=tm[1][:, 0:nb], op=ALU.add), R=["d_tm0", "d_tm1"], W=["d_mT"])
                for s_ in range(nsub):
                    tt = t0 + s_ * 128
                    ti = tt // 128
                    xx = xt[s_ % 2]
                    kx = f"d_xt{s_ % 2}"
                    hh = h2b[s_ % 2]
                    kh = f"d_h2b{s_ % 2}"
                    dma("sp", lambda e, xx=xx, tt=tt: e.dma_start(out=xx[:], in_=src_rows(tt, 128)), W=[kx])
                    for half in range(2):
                        for k in range(8):
                            op("pe", lambda e, half=half, k=k, s_=s_: e.matmul(psS[half][:], lhsT=mT[:, k, s_ * 128:(s_ + 1) * 128], rhs=wo[:, k, half * 512:(half + 1) * 512],
                                                                             start=(k == 0), stop=(k == 7)), R=["d_mT", "d_wo"], W=[f"psS{half}"])
                        op("dve", lambda e, half=half: e.tensor_tensor(out=r1[:, half * 512:(half + 1) * 512], in0=psS[half][:], in1=modb2[:, mr * 3, half * 512:(half + 1) * 512], op=ALU.mult),
                           R=[f"psS{half}", "d_modb", "d_r1"], W=["d_r1"])
                    op("dve", lambda e, xx=xx: e.scalar_tensor_tensor(out=r1[:], in0=xx[:], scalar=DN_ALPHA, in1=r1[:], op0=ALU.mult, op1=ALU.add), R=[kx, "d_r1"], W=["d_r1"])
                    layer_norm_tile(r1[:], "d_r1", x1t[:], "d_x1t", scr[:], "d_scr", st, "d_st", mul_b=lnb[:, 0, :], add_b=lnb[:, 1, :], keys_b=["d_lnb"])
                    dma("sp", lambda e, tt=tt: e.dma_start(out=x1s[tt:tt + 128, :], in_=x1t[:]), R=["d_x1t"], W=["x1s"])
                    layer_norm_tile(x1t[:], "d_x1t", hh[:], kh, scr[:], "d_scr", st, "d_st", mul_b=modb2[:, mr * 3 + 2, :], add_b=modb2[:, mr * 3 + 1, :], keys_b=["d_modb"])
                    transpose_to(hh, kh, 8, h2T, "d_h2T", s_ * 128, s_ % 2)
                    pi = pc[0] % 4
                    pc[0] += 1
                    for k in range(8):
                        op("pe", lambda e, pi=pi, k=k, s_=s_: e.matmul(psA[pi][:, 0:64], lhsT=h2T[:, k, s_ * 128:(s_ + 1) * 128], rhs=wr[:, k, :], start=(k == 0), stop=(k == 7)),
                           R=["d_h2T", "d_wr"], W=[f"psA{pi}"])
                    op("act", lambda e, pi=pi: e.activation(out=rs_sc[:], in_=psA[pi][:, 0:64], func=AF.Sigmoid), R=[f"psA{pi}"], W=["d_rsc"])
                    op("dve", lambda e: e.tensor_tensor(out=rs_bs[:], in0=rs_sc[:], in1=rbias[:], op=ALU.add), R=["d_rsc", "d_rbias"], W=["d_rbs"])
                    bs3 = rs_bs[:].rearrange("p (g e) -> p g e", g=8)
                    t3 = rs_t[:].rearrange("p (g e) -> p g e", g=8)
                    op("dve", lambda e: e.tensor_reduce(out=rs_g[:, 0, :], in_=bs3, axis=AX.X, op=ALU.max), R=["d_rbs"], W=["d_rg"])
                    op("dve", lambda e: e.tensor_tensor(out=t3, in0=bs3, in1=rs_g[:, 0, :].unsqueeze(2).to_broadcast([128, 8, 8]), op=ALU.is_equal), R=["d_rbs", "d_rg"], W=["d_rt"])
                    op("dve", lambda e: e.scalar_tensor_tensor(out=rs_t[:], in0=rs_t[:], scalar=-BIG, in1=rs_bs[:], op0=ALU.mult, op1=ALU.add), R=["d_rt", "d_rbs"], W=["d_rt"])
                    op("dve", lambda e: e.tensor_reduce(out=rs_g[:, 1, :], in_=t3, axis=AX.X, op=ALU.max), R=["d_rt", "d_rg"], W=["d_rg"])
                    op("dve", lambda e: e.tensor_tensor(out=rs_g[:, 2, :], in0=rs_g[:, 0, :], in1=rs_g[:, 1, :], op=ALU.add), R=["d_rg"], W=["d_rg"])
                    op("dve", lambda e: e.max(out=rs_m8[:], in_=rs_g[:, 2, :]), R=["d_rg"], W=["d_rm8"])
                    op("dve", lambda e: e.tensor_scalar(out=rs_g[:, 3, :], in0=rs_g[:, 2, :], scalar1=rs_m8[:, 3:4], scalar2=None, op0=ALU.is_ge), R=["d_rg", "d_rm8"], W=["d_rg"])
                    op("dve", lambda e: e.tensor_scalar(out=rs_g[:, 3, :], in0=rs_g[:, 3, :], scalar1=BIG, scalar2=-BIG, op0=ALU.mult, op1=ALU.add), R=["d_rg"], W=["d_rg"])
                    op("dve", lambda e: e.tensor_tensor(out=t3, in0=bs3, in1=rs_g[:, 3, :].unsqueeze(2).to_broadcast([128, 8, 8]), op=ALU.add), R=["d_rbs", "d_rg"], W=["d_rt"])
                    op("dve", lambda e: e.max(out=rs_m8[:], in_=rs_t[:]), R=["d_rt", "d_rm8"], W=["d_rm8"])
                    op("dve", lambda e: e.tensor_scalar(out=rs_sel[:], in0=rs_t[:], scalar1=rs_m8[:, 7:8], scalar2=None, op0=ALU.is_ge), R=["d_rt", "d_rm8"], W=["d_rsel"])
                    op("dve", lambda e: e.tensor_tensor(out=rs_wd[:], in0=rs_sel[:], in1=rs_sc[:], op=ALU.mult), R=["d_rsel", "d_rsc"], W=["d_rwd"])
                    op("dve", lambda e: e.reduce_sum(out=rs_s[:, 0:1], in_=rs_wd[:], axis=AX.X), R=["d_rwd"], W=["d_rs"])
                    op("dve", lambda e: e.reciprocal(out=rs_s[:, 1:2], in_=rs_s[:, 0:1]), R=["d_rs"], W=["d_rs"])
                    op("dve", lambda e: e.tensor_scalar(out=rs_wd[:], in0=rs_wd[:], scalar1=rs_s[:, 1:2], scalar2=2.5, op0=ALU.mult, op1=ALU.mult), R=["d_rwd", "d_rs"], W=["d_rwd"])
                    op("act", lambda e: e.copy(out=rs_selb[:], in_=rs_sel[:]), R=["d_rsel"], W=["d_rselb"])
                    op("act", lambda e: e.copy(out=Rrunb[:], in_=Rrun[:]), R=["d_Rrun"], W=["d_Rrunb"])
                    pi = pc[0] % 4
                    pc[0] += 1
                    op("pe", lambda e, pi=pi: e.matmul(psA[pi][:, 0:64], lhsT=Ustr[:], rhs=rs_selb[:], start=True, stop=False), R=["Ustr", "d_rselb"], W=[f"psA{pi}"])
                    op("pe", lambda e, pi=pi: e.matmul(psA[pi][:, 0:64], lhsT=ones_bf[:], rhs=Rrunb[:], start=False, stop=True), R=["ones_bf", "d_Rrunb"], W=[f"psA{pi}"])
                    op("dve", lambda e: e.tensor_tensor(out=Rrun[:], in0=Rrun[:], in1=rs_sel[:], op=ALU.add), R=["d_Rrun", "d_rsel"], W=["d_Rrun"])
                    op("dve", lambda e, pi=pi: e.tensor_scalar(out=rs_t[:], in0=psA[pi][:, 0:64], scalar1=float(CAP), scalar2=None, op0=ALU.is_lt), R=[f"psA{pi}"], W=["d_rt"])
                    op("dve", lambda e: e.tensor_tensor(out=rs_t[:], in0=rs_t[:], in1=rs_sel[:], op=ALU.mult), R=["d_rt", "d_rsel"], W=["d_rt"])
                    op("dve", lambda e, pi=pi: e.tensor_tensor(out=rs_da[:], in0=psA[pi][:, 0:64], in1=eidx[:], op=ALU.add), R=[f"psA{pi}", "eidx"], W=["d_rda"])
                    op("dve", lambda e: e.tensor_scalar(out=rs_bs[:], in0=rs_da[:], scalar1=-1.0, scalar2=BIG, op0=ALU.mult, op1=ALU.add), R=["d_rda"], W=["d_rbs"])
                    op("dve", lambda e: e.tensor_tensor(out=rs_bs[:], in0=rs_bs[:], in1=rs_t[:], op=ALU.mult), R=["d_rbs", "d_rt"], W=["d_rbs"])
                    op("dve", lambda e: e.max(out=rs_m8[:], in_=rs_bs[:]), R=["d_rbs", "d_rm8"], W=["d_rm8"])
                    op("dve", lambda e: e.tensor_scalar(out=rs_d8[:], in0=rs_m8[:], scalar1=-1.0, scalar2=BIG, op0=ALU.mult, op1=ALU.add), R=["d_rm8"], W=["d_rd8"])
                    op("dve", lambda e, ti=ti: e.tensor_copy(out=destI[:, ti, :], in_=rs_d8[:]), R=["d_rd8", "destI"], W=["destI"])
                    op("dve", lambda e: e.tensor_tensor(out=rs_m3[:], in0=rs_da[:].unsqueeze(1).to_broadcast([128, 8, 64]), in1=rs_d8[:].unsqueeze(2).to_broadcast([128, 8, 64]), op=ALU.is_equal),
                       R=["d_rda", "d_rd8"], W=["d_rm3"])
                    op("dve", lambda e: e.tensor_tensor(out=rs_m3[:], in0=rs_m3[:], in1=rs_wd[:].unsqueeze(1).to_broadcast([128, 8, 64]), op=ALU.mult), R=["d_rm3", "d_rwd"], W=["d_rm3"])
                    op("dve", lambda e, ti=ti: e.reduce_sum(out=wk[:, ti, :], in_=rs_m3[:], axis=AX.X), R=["d_rm3", "wk"], W=["wk"])
                    if "route" in debug:
                        dma("sp", lambda e, tt=tt: e.dma_start(out=dbg_d[tt:tt + 128, 0:8], in_=rs_d8[:]), R=["d_rd8"], W=["dbg_d"])
                        dma("sp", lambda e, tt=tt, ti=ti: e.dma_start(out=dbg_d[tt:tt + 128, 8:16], in_=wk[:, ti, :]), R=["wk"], W=["dbg_d"])
                    for k in range(8):
                        dma("pool", lambda e, hh=hh, ti=ti, k=k: e.indirect_dma_start(out=Xg[:, :], out_offset=bass.IndirectOffsetOnAxis(ap=destI[:, ti, k:k + 1], axis=0),
                                                                                 in_=hh[:, :], in_offset=None, bounds_check=bc_reg, oob_is_err=False),
                            R=[kh, "destI"], W=["Xg"])
                for ft in range(2):
                    pg = pc[0] % 4
                    pu = (pc[0] + 1) % 4
                    pc[0] += 2
                    for k in range(8):
                        op("pe", lambda e, pg=pg, k=k, ft=ft: e.matmul(psA[pg][:, 0:nb], lhsT=wsg[:, k, ft * 128:(ft + 1) * 128], rhs=h2T[:, k, 0:nb], start=(k == 0), stop=(k == 7)),
                           R=["d_wsg", "d_h2T"], W=[f"psA{pg}"])
                    for k in range(8):
                        op("pe", lambda e, pu=pu, k=k, ft=ft: e.matmul(psA[pu][:, 0:nb], lhsT=wsu[:, k, ft * 128:(ft + 1) * 128], rhs=h2T[:, k, 0:nb], start=(k == 0), stop=(k == 7)),
                           R=["d_wsu", "d_h2T"], W=[f"psA{pu}"])
                    op("act", lambda e, pg=pg: e.activation(out=sgt[:, 0:nb], in_=psA[pg][:, 0:nb], func=AF.Silu), R=[f"psA{pg}"], W=["d_sgt"])
                    op("dve", lambda e, pu=pu, ft=ft: e.tensor_tensor(out=actT[:, ft, 0:nb], in0=sgt[:, 0:nb], in1=psA[pu][:, 0:nb], op=ALU.mult), R=["d_sgt", f"psA{pu}"], W=["d_actT"])
                for s_ in range(nsub):
                    tt = t0 + s_ * 128
                    for half in range(2):
                        for fc in range(2):
                            op("pe", lambda e, half=half, fc=fc, s_=s_: e.matmul(psS[half][:], lhsT=actT[:, fc, s_ * 128:(s_ + 1) * 128], rhs=wsd[:, fc, half * 512:(half + 1) * 512],
                                                                               start=(fc == 0), stop=(fc == 1)), R=["d_actT", "d_wsd"], W=[f"psS{half}"])
                        op("act", lambda e, half=half: e.copy(out=ysh[:, half * 512:(half + 1) * 512], in_=psS[half][:]), R=[f"psS{half}", "d_ysh"], W=["d_ysh"])
                    dma("sp", lambda e, tt=tt: e.dma_start(out=ysh_d[tt:tt + 128, :], in_=ysh[:]), R=["d_ysh"], W=["ysh_d"])
            Bd.barrier()

        if "stopD" in debug:
            break

        with ExitStack() as ph:
            wg = [sb(ph, f"f_wg{i}", [128, 8, 256], BF16) for i in range(2)]
            wu = [sb(ph, f"f_wu{i}", [128, 8, 256], BF16) for i in range(2)]
            wdn = [sb(ph, f"f_wd{i}", [128, 2, D], BF16) for i in range(2)]
            xg = [sb(ph, f"f_xg{i}", [128, 8, 512], BF16) for i in range(2)]
            sgt = sb(ph, "f_sgt", [128, 512], F32)
            aT = [sb(ph, f"f_aT{i}", [128, 2, 512], BF16) for i in range(2)]
            yo = [sb(ph, f"f_yo{i}", [128, D], BF16) for i in range(2)]
            pc = [0]
            yi = [0]
            NXG = 3
            xg3 = xg + [sb(ph, "f_xg2", [128, 8, 512], BF16)]
            aT3 = aT + [sb(ph, "f_aT2", [128, 2, 512], BF16)]
            chunks_f = [(ex, ch) for ex in range(NE) for ch in range(CAP // 512)]

            def load_w(ex):
                b = ex % 2
                dma("pool", lambda e: e.dma_start(out=wg[b][:], in_=Wd["moe_w_gate"][L, ex].rearrange("(k p) n -> p k n", p=128)), W=[f"f_wg{b}"])
                dma("pool", lambda e: e.dma_start(out=wu[b][:], in_=Wd["moe_w_up"][L, ex].rearrange("(k p) n -> p k n", p=128)), W=[f"f_wu{b}"])
                dma("pool", lambda e: e.dma_start(out=wdn[b][:], in_=Wd["moe_w_down"][L, ex].rearrange("(k p) n -> p k n", p=128)), W=[f"f_wd{b}"])

            def load_x(i):
                ex, ch = chunks_f[i]
                slot0 = ex * CAP + ch * 512
                xb = i % NXG
                for k in range(8):
                    dma("sp", lambda e, k=k: e.dma_start_transpose(out=xg3[xb][:, k, :], in_=Xg[slot0:slot0 + 512, k * 128:(k + 1) * 128]), R=["Xg", f"f_xg{xb}"], W=[f"f_xg{xb}"])

            load_w(0)
            load_w(1)
            xrow = [[sb(ph, f"f_xr{p}{st}", [128, D], BF16) for st in range(4)] for p in range(2)]

            def load_rows(i):
                ex, ch = chunks_f[i]
                slot0 = ex * CAP + ch * 512
                p = i % 2
                for st in range(4):
                    dma("sp", lambda e, st=st: e.dma_start(out=xrow[p][st][:], in_=Xg[slot0 + st * 128:slot0 + (st + 1) * 128, :]), R=["Xg"], W=[f"f_xr{p}{st}"])

            def emit_T(i):
                p = i % 2
                xb = i % NXG
                for st in range(4):
                    pt = psT[st % 2]
                    for k in range(8):
                        op("pe", lambda e, k=k: e.transpose(out=pt[:, k * 128:(k + 1) * 128], in_=xrow[p][st][:, k * 128:(k + 1) * 128], identity=ident[:]),
                           R=[f"f_xr{p}{st}", "ident"], W=[f"psT{st % 2}"])
                    if st % 2 == 0:
                        op("act", lambda e: e.copy(out=xg3[xb][:, :, st * 128:(st + 1) * 128], in_=pt[:, 0:1024].rearrange("p (k t) -> p k t", k=8)),
                           R=[f"psT{st % 2}"], W=[f"f_xg{xb}"])
                    else:
                        op("dve", lambda e: e.tensor_copy(out=xg3[xb][:, :, st * 128:(st + 1) * 128], in_=pt[:, 0:1024].rearrange("p (k t) -> p k t", k=8)),
                           R=[f"psT{st % 2}"], W=[f"f_xg{xb}"])

            load_rows(0)
            load_rows(1)
            bank6 = [(psA[0], "psA0"), (psA[1], "psA1"), (psA[2], "psA2"), (psA[3], "psA3"), (psS[0], "psS0"), (psS[1], "psS1")]

            def nxt():
                bk = bank6[pc[0] % 6]
                pc[0] += 1
                return bk

            def emit_gu(i):
                ex, ch = chunks_f[i]
                b = ex % 2
                xb = i % NXG
                for ft in range(2):
                    (pg, kg), (pu, ku) = nxt(), nxt()
                    for k in range(8):
                        op("pe", lambda e, k=k: e.matmul(pg[:], lhsT=wg[b][:, k, ft * 128:(ft + 1) * 128], rhs=xg3[xb][:, k, :], start=(k == 0), stop=(k == 7)),
                           R=[f"f_wg{b}", f"f_xg{xb}"], W=[kg])
                    for k in range(8):
                        op("pe", lambda e, k=k: e.matmul(pu[:], lhsT=wu[b][:, k, ft * 128:(ft + 1) * 128], rhs=xg3[xb][:, k, :], start=(k == 0), stop=(k == 7)),
                           R=[f"f_wu{b}", f"f_xg{xb}"], W=[ku])
                    op("act", lambda e: e.activation(out=sgt2[ft][:], in_=pg[:], func=AF.Silu), R=[kg], W=[f"f_sgt{ft}"])
                    op("dve", lambda e: e.tensor_tensor(out=aT3[xb][:, ft, :], in0=sgt2[ft][:], in1=pu[:], op=ALU.mult), R=[f"f_sgt{ft}", ku], W=[f"f_aT{xb}"])

            def emit_down(i):
                ex, ch = chunks_f[i]
                b = ex % 2
                xb = i % NXG
                slot0 = ex * CAP + ch * 512
                for st_ in range(4):
                    yb_ = yi[0] % 3
                    yi[0] += 1
                    for half in range(2):
                        pd, kd = nxt()
                        for fc in range(2):
                            op("pe", lambda e, fc=fc: e.matmul(pd[:], lhsT=aT3[xb][:, fc, st_ * 128:(st_ + 1) * 128], rhs=wdn[b][:, fc, half * 512:(half + 1) * 512],
                                                               start=(fc == 0), stop=(fc == 1)), R=[f"f_aT{xb}", f"f_wd{b}"], W=[kd])
                        if half == 0:
                            op("act", lambda e: e.copy(out=yo3[yb_][:, 0:512], in_=pd[:]), R=[kd, f"f_yo{yb_}"], W=[f"f_yo{yb_}"])
                        else:
                            op("dve", lambda e: e.tensor_copy(out=yo3[yb_][:, 512:1024], in_=pd[:]), R=[kd, f"f_yo{yb_}"], W=[f"f_yo{yb_}"])
                    dma("sp", lambda e: e.dma_start(out=Yg[slot0 + st_ * 128:slot0 + (st_ + 1) * 128, :], in_=yo3[yb_][:]), R=[f"f_yo{yb_}"], W=["Yg"])

            sgt2 = [sgt, sb(ph, "f_sgtb", [128, 512], F32)]
            yo3 = yo + [sb(ph, "f_yo2", [128, D], BF16)]
            emit_T(0)
            emit_gu(0)
            for i in range(len(chunks_f)):
                if i + 1 < len(chunks_f):
                    emit_T(i + 1)
                    if i + 2 < len(chunks_f):
                        load_rows(i + 2)
                    emit_gu(i + 1)
                emit_down(i)
                ex_i, ch_i = chunks_f[i]
                if ch_i == CAP // 512 - 1 and ex_i + 2 < NE:
                    load_w(ex_i + 2)
            Bd.barrier()

        with ExitStack() as ph:
            g2b = sb(ph, "g_g2b", [128, 2, D], F32)
            lnb2 = sb(ph, "g_lnb", [128, 2, D], F32)
            x1t = [sb(ph, f"g_x1t{i}", [128, D], F32) for i in range(2)]
            accb = [sb(ph, f"g_acc{i}", [128, D], F32) for i in range(2)]
            gath = [sb(ph, f"g_gath{i}", [128, D], BF16) for i in range(4)]
            scr = sb(ph, "g_scr", [128, D], F32)
            st = sb(ph, "g_st", [128, 8], F32)
            xo = [sb(ph, f"g_xo{i}", [128, D], F32) for i in range(2)]
            for r in range(2):
                dma("sp", bcast_load("sp", g2b[:, r, :], mod_b(r, 5), D), R=["modv", "g_g2b"], W=["g_g2b"])
            dma("sp", bcast_load("sp", lnb2[:, 0, :], Wd["ln2_g"][L, :], D), R=["g_lnb"], W=["g_lnb"])
            dma("sp", bcast_load("sp", lnb2[:, 1, :], Wd["ln2_b"][L, :], D), R=["g_lnb"], W=["g_lnb"])
            for i in range(4):
                op("dve", lambda e, i=i: e.memset(gath[i][:], 0.0), W=[f"g_gath{i}"])
            gi = [0]
            tiles = list(range(NT)) if not last else list(range(2, NT))
            for n_, ti in enumerate(tiles):
                tt = ti * 128
                mr = 1 if tt < NCTX else 0
                bi = n_ % 2
                dma("sp", lambda e, bi=bi, tt=tt: e.dma_start(out=x1t[bi][:], in_=x1s[tt:tt + 128, :]), R=["x1s"], W=[f"g_x1t{bi}"])
                dma("sp", lambda e, bi=bi, tt=tt: e.dma_start(out=accb[bi][:], in_=ysh_d[tt:tt + 128, :]), R=["ysh_d"], W=[f"g_acc{bi}"])
                for k in range(8):
                    gj = gi[0] % 4
                    gi[0] += 1
                    dma("pool", lambda e, gj=gj, ti=ti, k=k: e.indirect_dma_start(out=gath[gj][:, :], out_offset=None, in_=Yg[:, :],
                                                                              in_offset=bass.IndirectOffsetOnAxis(ap=destI[:, ti, k:k + 1], axis=0),
                                                                              bounds_check=bc_reg, oob_is_err=False), R=["Yg", "destI"], W=[f"g_gath{gj}"])
                    op("dve", lambda e, gj=gj, bi=bi, ti=ti, k=k: e.scalar_tensor_tensor(out=accb[bi][:], in0=gath[gj][:], scalar=wk[:, ti, k:k + 1], in1=accb[bi][:], op0=ALU.mult, op1=ALU.add),
                       R=[f"g_gath{gj}", "wk", f"g_acc{bi}"], W=[f"g_acc{bi}"])
                op("dve", lambda e, bi=bi, mr=mr: e.tensor_tensor(out=accb[bi][:], in0=accb[bi][:], in1=g2b[:, mr, :], op=ALU.mult), R=[f"g_acc{bi}", "g_g2b"], W=[f"g_acc{bi}"])
                op("dve", lambda e, bi=bi: e.scalar_tensor_tensor(out=accb[bi][:], in0=x1t[bi][:], scalar=DN_ALPHA, in1=accb[bi][:], op0=ALU.mult, op1=ALU.add), R=[f"g_x1t{bi}", f"g_acc{bi}"], W=[f"g_acc{bi}"])
                layer_norm_tile(accb[bi][:], f"g_acc{bi}", xo[bi][:], f"g_xo{bi}", scr[:], "g_scr", st, "g_st", mul_b=lnb2[:, 0, :], add_b=lnb2[:, 1, :], keys_b=["g_lnb"])
                if last:
                    dma("sp", lambda e, bi=bi, tt=tt: e.dma_start(out=out_d[tt - NCTX:tt - NCTX + 128, :], in_=xo[bi][:]), R=[f"g_xo{bi}"], W=["out"])
                else:
                    dma("sp", lambda e, bi=bi, tt=tt: e.dma_start(out=xA[tt:tt + 128, :], in_=xo[bi][:]), R=[f"g_xo{bi}"], W=["xA"])
            Bd.barrier()

    Bd.barrier()
    print("instructions:", Bd.ninst, "semaphores:", Bd.nsem)
    return nc


_NC_CACHE = {}


def kernel(**inputs):
    x = np.ascontiguousarray(inputs["x"], dtype=np.float32)
    ctx = np.ascontiguousarray(inputs["ctx"], dtype=np.float32)
    c = np.asarray(inputs["c"], dtype=np.float32)
    c_ctx = np.asarray(inputs["c_ctx"], dtype=np.float32)
    nb = x.shape[0]
    if "nc" not in _NC_CACHE:
        _NC_CACHE["nc"] = build()
    nc = _NC_CACHE["nc"]
    shared = {n: np.ascontiguousarray(inputs[n], dtype=np.float32) for n in W_NAMES}
    in_maps = []
    for b in range(nb):
        m = dict(shared)
        m["x"] = x[b]
        m["ctx"] = ctx[b]
        m["c"] = np.stack([c[b], c_ctx], axis=0)
        in_maps.append(m)
    res = run_bass_kernel_spmd(nc, in_maps, core_ids=list(range(nb)))
    return np.stack([np.asarray(r["out"], dtype=np.float32) for r in res.results], axis=0)
```

```python
import math
import numpy as np
import concourse.bass as bass
import concourse.mybir as mybir
from concourse.bass_utils import run_bass_kernel_spmd
from contextlib import ExitStack

F32 = mybir.dt.float32
BF16 = mybir.dt.bfloat16
I32 = mybir.dt.int32
AF = mybir.ActivationFunctionType
ALU = mybir.AluOpType
AX = mybir.AxisListType

D = 1024
NCTX = 256
NLAT = 4096
T = NCTX + NLAT
NT = T // 128
DEPTH = 2
INC = 6656
NE = 64
CAP = 2048
NSLOT = NE * CAP
DN_ALPHA = (2 * DEPTH) ** 0.25
LN_EPS = 1e-6
RMS_EPS = 1e-5
BIG = 1.0e6
OQ, OK_, OV, OU, OS, OX, OY, OG = 0, 512, 1024, 1536, 2048, 2560, 3072, 3584

W_NAMES = ["w_mod", "b_mod", "w_in", "b_in", "lam_q1", "lam_k1", "lam_q2", "lam_k2", "attn_norm_g",
           "sg_ln_g", "sg_ln_b", "sg_w", "sg_b", "conv_w", "conv_b", "lru_wa", "lru_ba", "lru_wx", "lru_bx",
           "lru_lam", "w_branch", "w_out", "ln1_g", "ln1_b", "w_router", "router_bias", "moe_w_gate",
           "moe_w_up", "moe_w_down", "sh_w_gate", "sh_w_up", "sh_w_down", "ln2_g", "ln2_b"]
W_SHAPES = {
    "w_mod": [2, 1024, 6144], "b_mod": [2, 6144], "w_in": [2, 1024, 6656], "b_in": [2, 6656],
    "lam_q1": [2, 64], "lam_k1": [2, 64], "lam_q2": [2, 64], "lam_k2": [2, 64], "attn_norm_g": [2, 4, 128],
    "sg_ln_g": [2, 512], "sg_ln_b": [2, 512], "sg_w": [2, 4, 128, 128], "sg_b": [2, 4, 128],
    "conv_w": [2, 4, 512], "conv_b": [2, 512], "lru_wa": [2, 2, 8, 64, 64], "lru_ba": [2, 2, 512],
    "lru_wx": [2, 2, 8, 64, 64], "lru_bx": [2, 2, 512], "lru_lam": [2, 2, 512],
    "w_branch": [2, 3, 512, 1024], "w_out": [2, 1024, 1024], "ln1_g": [2, 1024], "ln1_b": [2, 1024],
    "w_router": [2, 1024, 64], "router_bias": [2, 64], "moe_w_gate": [2, 64, 1024, 256],
    "moe_w_up": [2, 64, 1024, 256], "moe_w_down": [2, 64, 256, 1024], "sh_w_gate": [2, 1024, 256],
    "sh_w_up": [2, 1024, 256], "sh_w_down": [2, 256, 1024], "ln2_g": [2, 1024], "ln2_b": [2, 1024],
}

SEM_EPOCH = 30000
NS_DMA = 8


class Builder:
    def __init__(self, nc):
        self.nc = nc
        self.E = {"pe": nc.tensor, "act": nc.scalar, "dve": nc.vector, "pool": nc.gpsimd, "sp": nc.sync}
        self.cur = {}
        self.known = {e: {} for e in self.E}
        self.lastw = {}
        self.rd = {}
        self.nsem = 0
        self.own = {e: set() for e in self.E}
        self.dq = {q: {"sems": [None] * NS_DMA, "val": [0] * NS_DMA, "i": 0} for q in ("sp", "pool", "act")}
        self.ninst = 0

    def newsem(self):
        self.nsem += 1
        return self.nc.semaphore(f"sm{self.nsem}").__enter__()

    def _wait(self, e, sem, val):
        if self.known[e].get(sem, 0) >= val:
            return
        self.E[e].wait_ge(sem, val)
        self.known[e][sem] = val

    def _deps(self, e, R, W):
        toks = {}
        for r in R:
            t = self.lastw.get(r)
            if t is not None and toks.get(t[0], 0) < t[1]:
                toks[t[0]] = t[1]
        for w in W:
            t = self.lastw.get(w)
            if t is not None and toks.get(t[0], 0) < t[1]:
                toks[t[0]] = t[1]
            for sm, v in self.rd.get(w, {}).items():
                if toks.get(sm, 0) < v:
                    toks[sm] = v
        for sm, v in toks.items():
            if e == "pe" and sm in self.own["pe"]:
                continue
            self._wait(e, sm, v)

    def _commit(self, tok, R, W):
        for w in W:
            self.lastw[w] = tok
            self.rd[w] = {}
        for r in R:
            d = self.rd.setdefault(r, {})
            if d.get(tok[0], 0) < tok[1]:
                d[tok[0]] = tok[1]

    def op(self, e, fn, R=(), W=()):
        self._deps(e, R, W)
        st = self.cur.get(e)
        if st is None or st[1] >= SEM_EPOCH:
            st = self.cur[e] = [self.newsem(), 0]
            self.own[e].add(st[0])
        ins = fn(self.E[e])
        st[1] += 1
        ins.then_inc(st[0], 1)
        self.ninst += 1
        self._commit((st[0], st[1]), R, W)

    def dma(self, q, fn, R=(), W=()):
        self._deps(q, R, W)
        d = self.dq[q]
        slot = d["i"] % NS_DMA
        d["i"] += 1
        if d["sems"][slot] is None or d["val"][slot] + 16 > SEM_EPOCH:
            if d["sems"][slot] is not None:
                self._wait(q, d["sems"][slot], d["val"][slot])
            d["sems"][slot] = self.newsem()
            d["val"][slot] = 0
        sem, prev = d["sems"][slot], d["val"][slot]
        if prev > 0:
            self._wait(q, sem, prev)
        ins = fn(self.E[q])
        ins.then_inc(sem, 16)
        d["val"][slot] = prev + 16
        self.ninst += 1
        self._commit((sem, prev + 16), R, W)

    def barrier(self, engines=None):
        toks = []
        for e, st in self.cur.items():
            if st[1] > 0:
                toks.append((st[0], st[1]))
        for q, d in self.dq.items():
            for sm, v in zip(d["sems"], d["val"]):
                if sm is not None and v > 0:
                    toks.append((sm, v))
        for e in (engines or list(self.E)):
            for sm, v in toks:
                self._wait(e, sm, v)
        if engines is None:
            self.lastw.clear()
            self.rd.clear()


def build(n_layers=DEPTH, debug=()):
    nc = bass.Bass("TRN2", target_bir_lowering=False)
    Bd = Builder(nc)
    op, dma = Bd.op, Bd.dma
    bc_reg = nc.gpsimd.to_reg(NSLOT - 1)

    def din(name, shape, dt=F32):
        return nc.dram_tensor(name, list(shape), dt, kind="ExternalInput").ap()

    def dscr(name, shape, dt=F32):
        kind = "ExternalOutput" if name in debug else "Internal"
        return nc.dram_tensor(name, list(shape), dt, kind=kind).ap()

    x_in = din("x", [NLAT, D])
    ctx_in = din("ctx", [NCTX, D])
    c_in = din("c", [2, D])
    Wd = {n: din(n, W_SHAPES[n]) for n in W_NAMES}
    out_d = nc.dram_tensor("out", [NLAT, D], F32, kind="ExternalOutput").ap()

    xA = dscr("xA", [T, D])
    x1s = dscr("x1s", [T, D])
    modv = dscr("modv", [2, 6144])
    qT_d = dscr("qT_d", [4, 128, T], BF16)
    kT_d = dscr("kT_d", [4, 128, T], BF16)
    v_d = dscr("v_d", [T, 512], BF16)
    xT_d = dscr("xT_d", [512, T], F32)
    yT_d = dscr("yT_d", [512, T], BF16)
    gT_d = dscr("gT_d", [3072, T], BF16)
    oaT_d = dscr("oaT_d", [512, T], BF16)
    obT_d = dscr("obT_d", [512, T], BF16)
    ocT_d = dscr("ocT_d", [512, T], BF16)
    cos_d = dscr("cos_d", [128, NLAT], F32)
    sin_d = dscr("sin_d", [128, NLAT], F32)
    Xg = dscr("Xg", [NSLOT, D], BF16)
    Yg = dscr("Yg", [NSLOT, D], BF16)
    ysh_d = dscr("ysh_d", [T, D], F32)
    dbg_d = dscr("dbg_d", [T, 64], F32)

    uid = [0]

    def sb(stack, name, shape, dt=F32):
        uid[0] += 1
        return stack.enter_context(nc.sbuf_tensor(f"{name}_u{uid[0]}", list(shape), dt))

    top = ExitStack()
    psA = [top.enter_context(nc.psum_tensor(f"psA{i}", [128, 512], F32)) for i in range(4)]
    psS = [top.enter_context(nc.psum_tensor(f"psS{i}", [128, 512], F32)) for i in range(2)]
    psT = [top.enter_context(nc.psum_tensor(f"psT{i}", [128, 1024], BF16)) for i in range(2)]

    ident = sb(top, "ident", [128, 128], BF16)
    ones_bf = sb(top, "ones_bf", [128, 128], BF16)
    Ustr = sb(top, "Ustr", [128, 128], BF16)
    Pm = sb(top, "Pm", [128, 128], BF16)
    rowi = sb(top, "rowi", [128, 1], F32)
    coli = sb(top, "coli", [128, 128], F32)
    ctmp = sb(top, "ctmp", [128, 128], F32)
    ctmp2 = sb(top, "ctmp2", [128, 128], F32)
    eidx = sb(top, "eidx", [128, 64], F32)
    op("pool", lambda e: e.iota(rowi[:], pattern=[[0, 1]], base=0, channel_multiplier=1,
                                allow_small_or_imprecise_dtypes=True), W=["rowi"])
    op("pool", lambda e: e.iota(coli[:], pattern=[[1, 128]], base=0, channel_multiplier=0,
                                allow_small_or_imprecise_dtypes=True), W=["coli"])
    op("pool", lambda e: e.iota(eidx[:], pattern=[[CAP, 64]], base=0, channel_multiplier=0,
                                allow_small_or_imprecise_dtypes=True), W=["eidx"])
    op("dve", lambda e: e.tensor_scalar(out=ident[:], in0=coli[:], scalar1=rowi[:, 0:1], scalar2=None,
                                        op0=ALU.is_equal), R=["coli", "rowi"], W=["ident"])
    op("dve", lambda e: e.tensor_scalar(out=Ustr[:], in0=coli[:], scalar1=rowi[:, 0:1], scalar2=None,
                                        op0=ALU.is_gt), R=["coli", "rowi"], W=["Ustr"])
    op("dve", lambda e: e.memset(ones_bf[:], 1.0), W=["ones_bf"])
    identF = sb(top, "identF", [128, 128], F32)
    destI = sb(top, "destI", [128, NT, 8], I32)
    wk = sb(top, "wk", [128, NT, 8], F32)
    eps_t = sb(top, "eps_t", [128, 2], F32)
    op("dve", lambda e: e.memset(eps_t[:, 0:1], LN_EPS), W=["eps_t"])
    op("dve", lambda e: e.memset(eps_t[:, 1:2], RMS_EPS), R=["eps_t"], W=["eps_t"])
    op("dve", lambda e: e.tensor_scalar(out=identF[:], in0=coli[:], scalar1=rowi[:, 0:1], scalar2=None,
                                        op0=ALU.is_equal), R=["coli", "rowi"], W=["identF"])
    op("pool", lambda e: e.iota(ctmp[:], pattern=[[32, 4], [-16, 2], [1, 16]], base=16, channel_multiplier=0,
                                allow_small_or_imprecise_dtypes=True), W=["ctmp"])
    op("dve", lambda e: e.tensor_scalar(out=Pm[:], in0=ctmp[:], scalar1=rowi[:, 0:1], scalar2=None,
                                        op0=ALU.is_equal), R=["ctmp", "rowi"], W=["Pm"])

    op("dve", lambda e: e.memset(destI[:], 0), W=["destI"])
    dzero = sb(top, "dzero", [128, 64], BF16)
    op("dve", lambda e: e.memset(dzero[:], 0.0), W=["dzero"])
    dma("pool", lambda e: e.indirect_dma_start(out=Xg[:, 0:64], out_offset=bass.IndirectOffsetOnAxis(ap=destI[:, 0, 0:1], axis=0),
                                               in_=dzero[:, :], in_offset=None, bounds_check=bc_reg, oob_is_err=False), R=["destI", "dzero"], W=["Xg"])

    with ExitStack() as ph:
        tok = sb(ph, "r_tok", [128, NLAT], F32)
        colp = sb(ph, "r_col", [128, NLAT], F32)
        ang = sb(ph, "r_ang", [128, NLAT], F32)
        tb = sb(ph, "r_tb", [128, NLAT], F32)
        ti = sb(ph, "r_ti", [128, NLAT], I32)
        pv = sb(ph, "r_pv", [128, 8], F32)
        pat = sb(ph, "r_pat", [128, 128], F32)

        def per_part(pattern, base, col):
            op("pool", lambda e: e.iota(pat[:], pattern=pattern, base=base, channel_multiplier=0,
                                        allow_small_or_imprecise_dtypes=True), W=["r_pat"])
            op("dve", lambda e: e.tensor_tensor(out=pat[:], in0=pat[:], in1=identF[:], op=ALU.mult), R=["r_pat", "identF"], W=["r_pat"])
            op("dve", lambda e: e.reduce_sum(out=pv[:, col:col + 1], in_=pat[:], axis=AX.X), R=["r_pat", "r_pv"], W=["r_pv"])

        per_part([[0, 8], [1, 16]], 0, 0)
        per_part([[0, 2], [1, 2], [0, 32]], 0, 2)
        per_part([[0, 4], [2, 2], [0, 16]], -1, 3)
        op("act", lambda e: e.activation(out=pv[:, 1:2], in_=pv[:, 0:1], func=AF.Exp, scale=-math.log(10000.0) / 16.0), R=["r_pv"], W=["r_pv"])
        op("pool", lambda e: e.iota(tok[:], pattern=[[1, 64], [0, 64]], base=0, channel_multiplier=0,
                                    allow_small_or_imprecise_dtypes=True), W=["r_tok"])
        op("pool", lambda e: e.iota(colp[:], pattern=[[0, 64], [1, 64]], base=0, channel_multiplier=0,
                                    allow_small_or_imprecise_dtypes=True), W=["r_col"])
        op("dve", lambda e: e.tensor_tensor(out=colp[:], in0=colp[:], in1=tok[:], op=ALU.subtract), R=["r_tok", "r_col"], W=["r_col"])
        op("dve", lambda e: e.scalar_tensor_tensor(out=ang[:], in0=colp[:], scalar=pv[:, 2:3], in1=tok[:], op0=ALU.mult, op1=ALU.add),
           R=["r_col", "r_tok", "r_pv"], W=["r_ang"])
        op("dve", lambda e: e.tensor_scalar(out=ang[:], in0=ang[:], scalar1=pv[:, 1:2], scalar2=None, op0=ALU.mult), R=["r_ang", "r_pv"], W=["r_ang"])

        def sin_of(src, key_src, dst, key_dst, shift):
            if shift != 0.0:
                op("dve", lambda e: e.tensor_scalar(out=dst, in0=src, scalar1=shift, scalar2=None, op0=ALU.add), R=[key_src], W=[key_dst])
                src, key_src = dst, key_dst
            op("dve", lambda e: e.tensor_scalar(out=tb[:], in0=src, scalar1=1.0 / (2 * math.pi), scalar2=None, op0=ALU.mult), R=[key_src], W=["r_tb"])
            op("dve", lambda e: e.tensor_copy(out=ti[:], in_=tb[:]), R=["r_tb"], W=["r_ti"])
            op("dve", lambda e: e.tensor_copy(out=tb[:], in_=ti[:]), R=["r_ti"], W=["r_tb"])
            op("dve", lambda e: e.scalar_tensor_tensor(out=dst, in0=tb[:], scalar=-2 * math.pi, in1=src, op0=ALU.mult, op1=ALU.add), R=["r_tb", key_src], W=[key_dst])
            op("act", lambda e: e.activation(out=dst, in_=dst, func=AF.Sin), R=[key_dst], W=[key_dst])

        sin_of(ang[:], "r_ang", colp[:], "r_col", 0.0)
        op("dve", lambda e: e.tensor_scalar(out=colp[:], in0=colp[:], scalar1=pv[:, 3:4], scalar2=None, op0=ALU.mult), R=["r_col", "r_pv"], W=["r_col"])
        dma("sp", lambda e: e.dma_start(out=sin_d, in_=colp[:]), R=["r_col"], W=["sin_d"])
        sin_of(ang[:], "r_ang", tok[:], "r_tok", 0.5 * math.pi)
        dma("sp", lambda e: e.dma_start(out=cos_d, in_=tok[:]), R=["r_tok"], W=["cos_d"])
        Bd.barrier()

    def bcast_load(q, dst, src_row, n):
        return lambda e: e.dma_start(out=dst, in_=src_row.partition_broadcast(128))

    def layer_norm_tile(xt, key_x, out_ap, key_out, scr, key_scr, st, key_st, n=D, mul_b=None, add_b=None, keys_b=()):
        op("dve", lambda e: e.reduce_sum(out=st[:, 0:1], in_=xt, axis=AX.X), R=[key_x], W=[key_st])
        op("act", lambda e: e.activation(out=scr, in_=xt, func=AF.Square), R=[key_x], W=[key_scr])
        op("dve", lambda e: e.reduce_sum(out=st[:, 1:2], in_=scr, axis=AX.X), R=[key_scr, key_st], W=[key_st])
        op("dve", lambda e: e.tensor_scalar(out=st[:, 2:3], in0=st[:, 0:1], scalar1=1.0 / n, scalar2=None, op0=ALU.mult), R=[key_st], W=[key_st])
        op("dve", lambda e: e.tensor_tensor(out=st[:, 3:4], in0=st[:, 2:3], in1=st[:, 2:3], op=ALU.mult), R=[key_st], W=[key_st])
        op("dve", lambda e: e.scalar_tensor_tensor(out=st[:, 4:5], in0=st[:, 1:2], scalar=1.0 / n, in1=st[:, 3:4], op0=ALU.mult, op1=ALU.subtract), R=[key_st], W=[key_st])
        op("act", lambda e: e.activation(out=st[:, 5:6], in_=st[:, 4:5], func=AF.Sqrt, bias=eps_t[:, 0:1], scale=1.0), R=[key_st, "eps_t"], W=[key_st])
        op("dve", lambda e: e.reciprocal(out=st[:, 5:6], in_=st[:, 5:6]), R=[key_st], W=[key_st])
        if mul_b is None:
            op("dve", lambda e: e.tensor_scalar(out=out_ap, in0=xt, scalar1=st[:, 2:3], scalar2=st[:, 5:6], op0=ALU.subtract, op1=ALU.mult),
               R=[key_x, key_st], W=[key_out])
        else:
            op("dve", lambda e: e.tensor_scalar(out=scr, in0=xt, scalar1=st[:, 2:3], scalar2=st[:, 5:6], op0=ALU.subtract, op1=ALU.mult),
               R=[key_x, key_st], W=[key_scr])
            op("dve", lambda e: e.tensor_tensor(out=scr, in0=scr, in1=mul_b, op=ALU.mult), R=[key_scr] + list(keys_b), W=[key_scr])
            op("dve", lambda e: e.tensor_tensor(out=out_ap, in0=scr, in1=add_b, op=ALU.add), R=[key_scr] + list(keys_b), W=[key_out])

    def transpose_to(src_bf, key_src, nchunk, dstT, key_dst, tcol, pbank):
        pt = psT[pbank]
        for k in range(nchunk):
            op("pe", lambda e, k=k: e.transpose(out=pt[:, k * 128:(k + 1) * 128], in_=src_bf[:, k * 128:(k + 1) * 128], identity=ident[:]),
               R=[key_src, "ident"], W=[f"psT{pbank}"])
        op("act", lambda e: e.copy(out=dstT[:, 0:nchunk, tcol:tcol + 128],
                                   in_=pt[:, 0:nchunk * 128].rearrange("p (k t) -> p k t", k=nchunk)),
           R=[f"psT{pbank}"], W=[key_dst])

    blocks = [(0, 256)] + [(256 + 512 * i, 512) for i in range(8)]

    for L in range(n_layers):
        last = L == DEPTH - 1
        lam_init = 0.8 - 0.6 * math.exp(-0.3 * L)
        src_lat = x_in if L == 0 else xA[NCTX:T, :]
        src_ctx = ctx_in if L == 0 else xA[0:NCTX, :]

        def src_rows(t0, n):
            return src_ctx[t0:t0 + n, :] if t0 < NCTX else src_lat[t0 - NCTX:t0 - NCTX + n, :]

        with ExitStack() as ph:
            cT = sb(ph, "m_cT", [128, 2, 8], F32)
            crep = sb(ph, "m_crep", [128, 2, 8, 128], BF16)
            wm = [sb(ph, f"m_wm{i}", [128, 8, 512], BF16) for i in range(2)]
            bm = sb(ph, "m_bm", [1, 6144], F32)
            row = sb(ph, "m_row", [1, 2, 6144], F32)
            with nc.allow_non_contiguous_dma(reason="tiny transposed load of c"):
                dma("sp", lambda e: e.dma_start(out=cT[:], in_=c_in.rearrange("r (k p) -> p r k", p=128)), W=["m_cT"])
            dma("sp", lambda e: e.dma_start(out=bm[:], in_=Wd["b_mod"][L:L + 1, :]), W=["m_bm"])
            op("act", lambda e: e.activation(out=cT[:], in_=cT[:], func=AF.Silu), R=["m_cT"], W=["m_cT"])
            for r in range(2):
                op("dve", lambda e, r=r: e.tensor_copy(out=crep[:, r, :, :], in_=cT[:, r, :].unsqueeze(2).to_broadcast([128, 8, 128])),
                   R=["m_cT"], W=["m_crep"])
            for ch in range(12):
                w = wm[ch % 2]
                dma("pool", lambda e, w=w, ch=ch: e.dma_start(out=w[:], in_=Wd["w_mod"][L, :, ch * 512:(ch + 1) * 512].rearrange("(k p) n -> p k n", p=128)),
                    W=[f"m_wm{ch % 2}"])
                for r in range(2):
                    ps = psA[(ch * 2 + r) % 4]
                    for k in range(8):
                        op("pe", lambda e, ps=ps, k=k, r=r, w=w: e.matmul(ps[:], lhsT=crep[:, r, k, :], rhs=w[:, k, :], start=(k == 0), stop=(k == 7)),
                           R=["m_crep", f"m_wm{ch % 2}"], W=[f"psA{(ch * 2 + r) % 4}"])
                    op("dve", lambda e, ps=ps, r=r, ch=ch: e.tensor_tensor(out=row[0:1, r, ch * 512:(ch + 1) * 512], in0=ps[0:1, :], in1=bm[0:1, ch * 512:(ch + 1) * 512], op=ALU.add),
                       R=[f"psA{(ch * 2 + r) % 4}", "m_bm"], W=["m_row"])
            for seg in (1, 4):
                op("dve", lambda e, seg=seg: e.tensor_scalar(out=row[0:1, :, seg * 1024:(seg + 1) * 1024], in0=row[0:1, :, seg * 1024:(seg + 1) * 1024],
                                                             scalar1=1.0, scalar2=None, op0=ALU.add), R=["m_row"], W=["m_row"])
            dma("sp", lambda e: e.dma_start(out=modv.rearrange("(o r) n -> o r n", o=1), in_=row[:]), R=["m_row"], W=["modv"])
            Bd.barrier()

        def mod_b(r, seg):
            return modv[r, seg * 1024:(seg + 1) * 1024]

        with ExitStack() as ph:
            win = sb(ph, "a_win", [128, 8, INC], BF16)
            for k in range(8):
                dma("pool", lambda e, k=k: e.dma_start(out=win[:, k, :], in_=Wd["w_in"][L, k * 128:(k + 1) * 128, :]), W=["a_win"])
            binT = sb(ph, "a_binT", [128, 52], F32)
            with nc.allow_non_contiguous_dma(reason="tiny transposed bias load"):
                dma("sp", lambda e: e.dma_start(out=binT[:], in_=Wd["b_in"][L, :].rearrange("(j p) -> p j", p=128)), W=["a_binT"])
            bvus = sb(ph, "a_bvus", [128, 1536], F32)
            dma("sp", bcast_load("sp", bvus[:], Wd["b_in"][L, OV:OV + 1536], 1536), W=["a_bvus"])
            modb = sb(ph, "a_modb", [128, 4, D], F32)
            for r in range(2):
                for j, seg in enumerate((0, 1)):
                    dma("sp", bcast_load("sp", modb[:, r * 2 + j, :], mod_b(r, seg), D), R=["modv"], W=["a_modb"])
            lng = sb(ph, "a_lng", [128, 2, 512], F32)
            dma("sp", bcast_load("sp", lng[:, 0, :], Wd["sg_ln_g"][L, :], 512), W=["a_lng"])
            dma("sp", bcast_load("sp", lng[:, 1, :], Wd["sg_ln_b"][L, :], 512), W=["a_lng"])
            wsf = sb(ph, "a_wsf", [128, 4, 128], F32)
            wsb = sb(ph, "a_wsb", [128, 4, 128], BF16)
            wsT = sb(ph, "a_wsT", [128, 4, 128], BF16)
            bsT = sb(ph, "a_bsT", [128, 4], F32)
            dma("sp", lambda e: e.dma_start(out=wsf[:], in_=Wd["sg_w"][L].rearrange("g p q -> p g q")), W=["a_wsf"])
            with nc.allow_non_contiguous_dma(reason="tiny transposed bias load"):
                dma("sp", lambda e: e.dma_start(out=bsT[:], in_=Wd["sg_b"][L].rearrange("g p -> p g")), W=["a_bsT"])
            op("dve", lambda e: e.tensor_copy(out=wsb[:], in_=wsf[:]), R=["a_wsf"], W=["a_wsb"])
            for g in range(4):
                op("pe", lambda e, g=g: e.transpose(out=psT[0][:, g * 128:(g + 1) * 128], in_=wsb[:, g, :], identity=ident[:]), R=["a_wsb", "ident"], W=["psT0"])
            op("act", lambda e: e.copy(out=wsT[:], in_=psT[0][:, 0:512].rearrange("p (g t) -> p g t", g=4)), R=["psT0"], W=["a_wsT"])

            xt = [sb(ph, f"a_xt{i}", [128, D], F32) for i in range(2)]
            scr = sb(ph, "a_scr", [128, D], F32)
            st = sb(ph, "a_st", [128, 8], F32)
            hb4 = [sb(ph, f"a_hb{i}", [128, D], BF16) for i in range(4)]
            hTs = [sb(ph, f"a_hT{i}", [128, 8, 512], BF16) for i in range(2)]
            fo = [sb(ph, f"a_fo{i}", [128, 512], F32) for i in range(2)]
            fob = [sb(ph, f"a_fob{i}", [128, 512], BF16) for i in range(2)]
            rp = sb(ph, "a_rp", [128, 512], F32)
            cs = sb(ph, "a_cs", [128, 2, 512], F32)
            tmv = sb(ph, "a_tmv", [128, 512], F32)
            tmb = sb(ph, "a_tmb", [128, 512], BF16)
            gu = sb(ph, "a_gu", [128, 512], F32)
            gs_ = sb(ph, "a_gs", [128, 512], F32)
            gsq = sb(ph, "a_gsq", [128, 512], F32)
            gst = sb(ph, "a_gst", [128, 4, 4], F32)
            vgb = sb(ph, "a_vgb", [128, 512], BF16)
            ob = sb(ph, "a_ob", [128, 512], BF16)
            obT = sb(ph, "a_obT", [128, 4, 512], BF16)

            fcnt = [0]

            def stage_ln(bi_):
                t0_, nb_ = blocks[bi_]
                mr_ = 1 if t0_ < NCTX else 0
                for s_ in range(nb_ // 128):
                    xx = xt[s_ % 2]
                    kx = f"a_xt{s_ % 2}"
                    dma("sp", lambda e: e.dma_start(out=xx[:], in_=src_rows(t0_ + s_ * 128, 128)), W=[kx])
                    layer_norm_tile(xx[:], kx, hb4[s_][:], f"a_hb{s_}", scr[:], "a_scr", st, "a_st",
                                    mul_b=modb[:, mr_ * 2 + 1, :], add_b=modb[:, mr_ * 2, :], keys_b=["a_modb"])

            def stage_tr(bi_):
                t0_, nb_ = blocks[bi_]
                for s_ in range(nb_ // 128):
                    transpose_to(hb4[s_], f"a_hb{s_}", 8, hTs[bi_ % 2], f"a_hT{bi_ % 2}", s_ * 128, s_ % 2)

            stage_ln(0)
            stage_tr(0)
            for bi, (t0, nb) in enumerate(blocks):
                is_ctx = t0 < NCTX
                mr = 1 if is_ctx else 0
                nsub = nb // 128
                hT = hTs[bi % 2]
                kT_ = f"a_hT{bi % 2}"
                if not is_ctx:
                    l0 = t0 - NCTX
                    dma("sp", lambda e: e.dma_start(out=cs[:, 0, :], in_=cos_d[:, l0:l0 + 512]), R=["cos_d"], W=["a_cs"])
                    dma("sp", lambda e: e.dma_start(out=cs[:, 1, :], in_=sin_d[:, l0:l0 + 512]), R=["sin_d"], W=["a_cs"])
                fm_tiles = [("q", j) for j in range(4)] + [("k", j) for j in range(4)] + [("x", j) for j in range(4)] + \
                           [("y", j) for j in range(4)] + [("g", j) for j in range(24)]
                for fi_, (kind, j) in enumerate(fm_tiles):
                    if fi_ == 8 and bi + 1 < len(blocks):
                        stage_ln(bi + 1)
                    col0 = {"q": OQ, "k": OK_, "x": OX, "y": OY, "g": OG}[kind] + j * 128
                    jt = col0 // 128
                    pi = fcnt[0] % 4
                    fi = fcnt[0] % 2
                    fcnt[0] += 1
                    ps = psA[pi]
                    for k in range(8):
                        op("pe", lambda e, ps=ps, k=k, col0=col0: e.matmul(ps[:, 0:nb], lhsT=win[:, k, col0:col0 + 128], rhs=hT[:, k, 0:nb], start=(k == 0), stop=(k == 7)),
                           R=["a_win", kT_], W=[f"psA{pi}"])
                    if kind in ("q", "k"):
                        dst = qT_d if kind == "q" else kT_d
                        if is_ctx:
                            op("act", lambda e, ps=ps, fi=fi, jt=jt: e.activation(out=fob[fi][:, 0:nb], in_=ps[:, 0:nb], func=AF.Identity, bias=binT[:, jt:jt + 1], scale=1.0),
                               R=[f"psA{pi}", "a_binT"], W=[f"a_fob{fi}"])
                            dma("sp", lambda e, fi=fi, dst=dst, j=j: e.dma_start(out=dst[j, :, t0:t0 + nb], in_=fob[fi][:, 0:nb]), R=[f"a_fob{fi}"], W=[kind + "T_d"])
                        else:
                            op("act", lambda e, ps=ps, fi=fi, jt=jt: e.activation(out=fo[fi][:, 0:nb], in_=ps[:, 0:nb], func=AF.Identity, bias=binT[:, jt:jt + 1], scale=1.0),
                               R=[f"psA{pi}", "a_binT"], W=[f"a_fo{fi}"])
                            op("dve", lambda e, fi=fi: e.tensor_copy(out=fob[fi][:, 0:nb], in_=fo[fi][:, 0:nb]), R=[f"a_fo{fi}"], W=[f"a_fob{fi}"])
                            op("pe", lambda e, fi=fi: e.matmul(psS[0][:, 0:nb], lhsT=Pm[:], rhs=fob[fi][:, 0:nb], start=True, stop=True),
                               R=["Pm", f"a_fob{fi}"], W=["psS0"])
                            op("dve", lambda e: e.tensor_tensor(out=rp[:, 0:nb], in0=psS[0][:, 0:nb], in1=cs[:, 1, 0:nb], op=ALU.mult), R=["psS0", "a_cs"], W=["a_rp"])
                            op("dve", lambda e, fi=fi: e.tensor_tensor(out=fo[fi][:, 0:nb], in0=fo[fi][:, 0:nb], in1=cs[:, 0, 0:nb], op=ALU.mult), R=[f"a_fo{fi}", "a_cs"], W=[f"a_fo{fi}"])
                            op("dve", lambda e, fi=fi: e.tensor_tensor(out=fob[fi][:, 0:nb], in0=fo[fi][:, 0:nb], in1=rp[:, 0:nb], op=ALU.add), R=[f"a_fo{fi}", "a_rp"], W=[f"a_fob{fi}"])
                            dma("sp", lambda e, fi=fi, dst=dst, j=j: e.dma_start(out=dst[j, :, t0:t0 + nb], in_=fob[fi][:, 0:nb]), R=[f"a_fob{fi}"], W=[kind + "T_d"])
                    elif kind == "x":
                        op("act", lambda e, ps=ps, fi=fi, jt=jt: e.activation(out=fo[fi][:, 0:nb], in_=ps[:, 0:nb], func=AF.Identity, bias=binT[:, jt:jt + 1], scale=1.0),
                           R=[f"psA{pi}", "a_binT"], W=[f"a_fo{fi}"])
                        dma("sp", lambda e, fi=fi, j=j: e.dma_start(out=xT_d[j * 128:(j + 1) * 128, t0:t0 + nb], in_=fo[fi][:, 0:nb]), R=[f"a_fo{fi}"], W=["xT_d"])
                    elif kind == "y":
                        op("act", lambda e, ps=ps, fi=fi, jt=jt: e.activation(out=fob[fi][:, 0:nb], in_=ps[:, 0:nb], func=AF.Gelu_apprx_tanh, bias=binT[:, jt:jt + 1], scale=1.0),
                           R=[f"psA{pi}", "a_binT"], W=[f"a_fob{fi}"])
                        dma("sp", lambda e, fi=fi, j=j: e.dma_start(out=yT_d[j * 128:(j + 1) * 128, t0:t0 + nb], in_=fob[fi][:, 0:nb]), R=[f"a_fob{fi}"], W=["yT_d"])
                    else:
                        op("act", lambda e, ps=ps, fi=fi, jt=jt: e.activation(out=fob[fi][:, 0:nb], in_=ps[:, 0:nb], func=AF.Sigmoid, bias=binT[:, jt:jt + 1], scale=1.0),
                           R=[f"psA{pi}", "a_binT"], W=[f"a_fob{fi}"])
                        dma("sp", lambda e, fi=fi, j=j: e.dma_start(out=gT_d[j * 128:(j + 1) * 128, t0:t0 + nb], in_=fob[fi][:, 0:nb]), R=[f"a_fob{fi}"], W=["gT_d"])
                if bi + 1 < len(blocks):
                    stage_tr(bi + 1)
                for s_ in range(nsub):
                    tt = t0 + s_ * 128
                    for wi, (kind, col0) in enumerate((("v", OV), ("u", OU), ("s", OS))):
                        pi = fcnt[0] % 4
                        fcnt[0] += 1
                        ps = psA[pi]
                        for k in range(8):
                            op("pe", lambda e, ps=ps, k=k, col0=col0, s_=s_: e.matmul(ps[:], lhsT=hT[:, k, s_ * 128:(s_ + 1) * 128], rhs=win[:, k, col0:col0 + 512], start=(k == 0), stop=(k == 7)),
                               R=["a_win", kT_], W=[f"psA{pi}"])
                        if kind == "v":
                            op("dve", lambda e, ps=ps: e.tensor_tensor(out=tmb[:], in0=ps[:], in1=bvus[:, 0:512], op=ALU.add), R=[f"psA{pi}", "a_bvus"], W=["a_tmb"])
                            dma("sp", lambda e, tt=tt: e.dma_start(out=v_d[tt:tt + 128, :], in_=tmb[:]), R=["a_tmb"], W=["v_d"])
                        elif kind == "u":
                            op("dve", lambda e, ps=ps: e.tensor_tensor(out=tmv[:], in0=ps[:], in1=bvus[:, 512:1024], op=ALU.add), R=[f"psA{pi}", "a_bvus"], W=["a_tmv"])
                            op("act", lambda e: e.activation(out=gu[:], in_=tmv[:], func=AF.Gelu_apprx_tanh), R=["a_tmv"], W=["a_gu"])
                        else:
                            op("dve", lambda e, ps=ps: e.tensor_tensor(out=tmv[:], in0=ps[:], in1=bvus[:, 1024:1536], op=ALU.add), R=[f"psA{pi}", "a_bvus"], W=["a_tmv"])
                            op("act", lambda e: e.activation(out=gs_[:], in_=tmv[:], func=AF.Gelu_apprx_tanh), R=["a_tmv"], W=["a_gs"])
                    g3 = gs_[:].rearrange("p (g c) -> p g c", g=4)
                    op("dve", lambda e: e.reduce_sum(out=gst[:, 0, :], in_=g3, axis=AX.X), R=["a_gs"], W=["a_gst"])
                    op("act", lambda e: e.activation(out=gsq[:], in_=gs_[:], func=AF.Square), R=["a_gs"], W=["a_gsq"])
                    op("dve", lambda e: e.reduce_sum(out=gst[:, 1, :], in_=gsq[:].rearrange("p (g c) -> p g c", g=4), axis=AX.X), R=["a_gsq", "a_gst"], W=["a_gst"])
                    op("dve", lambda e: e.tensor_scalar(out=gst[:, 0, :], in0=gst[:, 0, :], scalar1=1.0 / 128, scalar2=None, op0=ALU.mult), R=["a_gst"], W=["a_gst"])
                    op("dve", lambda e: e.tensor_tensor(out=gst[:, 2, :], in0=gst[:, 0, :], in1=gst[:, 0, :], op=ALU.mult), R=["a_gst"], W=["a_gst"])
                    op("dve", lambda e: e.scalar_tensor_tensor(out=gst[:, 1, :], in0=gst[:, 1, :], scalar=1.0 / 128, in1=gst[:, 2, :], op0=ALU.mult, op1=ALU.subtract), R=["a_gst"], W=["a_gst"])
                    op("act", lambda e: e.activation(out=gst[:, 3, :], in_=gst[:, 1, :], func=AF.Sqrt, bias=eps_t[:, 0:1], scale=1.0), R=["a_gst", "eps_t"], W=["a_gst"])
                    op("dve", lambda e: e.reciprocal(out=gst[:, 3, :], in_=gst[:, 3, :]), R=["a_gst"], W=["a_gst"])
                    op("dve", lambda e: e.tensor_tensor(out=gsq[:].rearrange("p (g c) -> p g c", g=4), in0=g3, in1=gst[:, 0, :].unsqueeze(2).to_broadcast([128, 4, 128]), op=ALU.subtract),
                       R=["a_gs", "a_gst"], W=["a_gsq"])
                    op("dve", lambda e: e.tensor_tensor(out=gsq[:].rearrange("p (g c) -> p g c", g=4), in0=gsq[:].rearrange("p (g c) -> p g c", g=4),
                                                        in1=gst[:, 3, :].unsqueeze(2).to_broadcast([128, 4, 128]), op=ALU.mult), R=["a_gsq", "a_gst"], W=["a_gsq"])
                    op("dve", lambda e: e.tensor_tensor(out=gsq[:], in0=gsq[:], in1=lng[:, 0, :], op=ALU.mult), R=["a_gsq", "a_lng"], W=["a_gsq"])
                    op("dve", lambda e: e.tensor_tensor(out=vgb[:], in0=gsq[:], in1=lng[:, 1, :], op=ALU.add), R=["a_gsq", "a_lng"], W=["a_vgb"])
                    pi = fcnt[0] % 4
                    fcnt[0] += 1
                    ps = psA[pi]
                    for g in range(4):
                        op("pe", lambda e, ps=ps, g=g: e.matmul(ps[:, g * 128:(g + 1) * 128], lhsT=wsT[:, g, :], rhs=vgb[:, g * 128:(g + 1) * 128], start=True, stop=True),
                           R=["a_wsT", "a_vgb"], W=[f"psA{pi}"])
                    for g in range(4):
                        op("dve", lambda e, ps=ps, g=g: e.scalar_tensor_tensor(out=ob[:, g * 128:(g + 1) * 128], in0=ps[:, g * 128:(g + 1) * 128], scalar=bsT[:, g:g + 1],
                                                                               in1=gu[:, g * 128:(g + 1) * 128], op0=ALU.add, op1=ALU.mult),
                           R=[f"psA{pi}", "a_bsT", "a_gu"], W=["a_ob"])
                    transpose_to(ob, "a_ob", 4, obT, "a_obT", s_ * 128, s_ % 2)
                for cc in range(4):
                    dma("sp", lambda e, cc=cc: e.dma_start(out=obT_d[cc * 128:(cc + 1) * 128, t0:t0 + nb], in_=obT[:, cc, 0:nb]), R=["a_obT"], W=["obT_d"])
            Bd.barrier()

        if "stopA" in debug:
            break

        with ExitStack() as ph:
            xp = sb(ph, "b_xp", [128, T + 8], F32)
            xs = sb(ph, "b_xs", [128, T], F32)
            xsb = sb(ph, "b_xsb", [128, T], BF16)
            r_ = sb(ph, "b_r", [128, T], F32)
            i_ = sb(ph, "b_i", [128, T], F32)
            a2 = sb(ph, "b_a2", [128, T], F32)
            hf = sb(ph, "b_hf", [128, T], F32)
            hb_ = sb(ph, "b_hb", [128, T], F32)
            yb = sb(ph, "b_yb", [128, T], BF16)
            oc = sb(ph, "b_oc", [128, T], BF16)
            cw = sb(ph, "b_cw", [128, 4, 4], F32)
            cb = sb(ph, "b_cb", [128, 4], F32)
            gb = sb(ph, "b_gb", [128, 3, 2, 4], F32)
            cn = sb(ph, "b_cn", [128, 2, 2, 4], F32)
            one_t = sb(ph, "b_one", [128, 1], F32)
            wst = sb(ph, "b_wst", [128, 16, 128], F32)
            wbd = sb(ph, "b_wbd", [128, 16, 128], BF16)
            op("dve", lambda e: e.memset(xp[:], 0.0), W=["b_xp"])
            op("dve", lambda e: e.memset(one_t[:], 1.0), W=["b_one"])
            op("pool", lambda e: e.memset(wst[:], 0.0), W=["b_wst"])
            with nc.allow_non_contiguous_dma(reason="tiny per-channel parameter loads"):
                for j in range(4):
                    dma("sp", lambda e, j=j: e.dma_start(out=cw[:, :, j], in_=Wd["conv_w"][L, j].rearrange("(ct p) -> p ct", p=128)), R=["b_cw"], W=["b_cw"])
                dma("sp", lambda e: e.dma_start(out=cb[:], in_=Wd["conv_b"][L].rearrange("(ct p) -> p ct", p=128)), W=["b_cb"])
                for wi, nm in enumerate(("lru_ba", "lru_bx", "lru_lam")):
                    for d in range(2):
                        dma("sp", lambda e, wi=wi, nm=nm, d=d: e.dma_start(out=gb[:, wi, d, :], in_=Wd[nm][L, d].rearrange("(ct p) -> p ct", p=128)), R=["b_gb"], W=["b_gb"])
            for d in range(2):
                for gi, nm in enumerate(("lru_wa", "lru_wx")):
                    for ct in range(4):
                        idx = (d * 2 + gi) * 4 + ct
                        for hh in range(2):
                            dma("sp", lambda e, idx=idx, hh=hh, nm=nm, d=d, ct=ct: e.dma_start(
                                out=wst[hh * 64:(hh + 1) * 64, idx, hh * 64:(hh + 1) * 64], in_=Wd[nm][L, d, 2 * ct + hh]), R=["b_wst"], W=["b_wst"])
            op("dve", lambda e: e.tensor_copy(out=wbd[:], in_=wst[:]), R=["b_wst"], W=["b_wbd"])
            op("act", lambda e: e.activation(out=cn[:, 0, :, :], in_=gb[:, 2, :, :], func=AF.Exp, scale=-1.0), R=["b_gb"], W=["b_cn"])
            op("act", lambda e: e.activation(out=cn[:, 0, :, :], in_=cn[:, 0, :, :], func=AF.Ln, bias=one_t[:, 0:1], scale=1.0), R=["b_cn", "b_one"], W=["b_cn"])
            op("dve", lambda e: e.tensor_scalar(out=cn[:, 1, :, :], in0=cn[:, 0, :, :], scalar1=-16.0, scalar2=None, op0=ALU.mult), R=["b_cn"], W=["b_cn"])
            op("dve", lambda e: e.tensor_scalar(out=cn[:, 0, :, :], in0=cn[:, 0, :, :], scalar1=-8.0, scalar2=None, op0=ALU.mult), R=["b_cn"], W=["b_cn"])
            chunks = [(i * 512, 512) for i in range(8)] + [(4096, 256)]
            pcn = [0]
            for ct in range(4):
                dma("sp", lambda e, ct=ct: e.dma_start(out=xp[:, 2:2 + NCTX], in_=xT_d[ct * 128:(ct + 1) * 128, 0:NCTX]), R=["xT_d"], W=["b_xp"])
                dma("sp", lambda e, ct=ct: e.dma_start(out=xp[:, 6 + NCTX:6 + T], in_=xT_d[ct * 128:(ct + 1) * 128, NCTX:T]), R=["xT_d", "b_xp"], W=["b_xp"])
                dma("sp", lambda e, ct=ct: e.dma_start(out=yb[:], in_=yT_d[ct * 128:(ct + 1) * 128, :]), R=["yT_d"], W=["b_yb"])
                for (base, n, o0) in ((0, NCTX, 0), (4 + NCTX, NLAT, NCTX)):
                    op("dve", lambda e, base=base, n=n, o0=o0, ct=ct: e.tensor_scalar(out=xs[:, o0:o0 + n], in0=xp[:, base:base + n], scalar1=cw[:, ct, 0:1], scalar2=cb[:, ct:ct + 1],
                                                                                  op0=ALU.mult, op1=ALU.add), R=["b_xp", "b_cw", "b_cb"], W=["b_xs"])
                    for j in range(1, 4):
                        op("dve", lambda e, base=base, n=n, o0=o0, ct=ct, j=j: e.scalar_tensor_tensor(out=xs[:, o0:o0 + n], in0=xp[:, base + j:base + j + n], scalar=cw[:, ct, j:j + 1],
                                                                                                   in1=xs[:, o0:o0 + n], op0=ALU.mult, op1=ALU.add), R=["b_xp", "b_cw", "b_xs"], W=["b_xs"])
                op("act", lambda e: e.copy(out=xsb[:], in_=xs[:]), R=["b_xs"], W=["b_xsb"])
                for d in range(2):
                    for gi, (gt, gk) in enumerate(((r_, "b_r"), (i_, "b_i"))):
                        idx = (d * 2 + gi) * 4 + ct
                        for (c0, cnb) in chunks:
                            pi = pcn[0] % 4
                            pcn[0] += 1
                            op("pe", lambda e, pi=pi, idx=idx, c0=c0, cnb=cnb: e.matmul(psA[pi][:, 0:cnb], lhsT=wbd[:, idx, :], rhs=xsb[:, c0:c0 + cnb], start=True, stop=True),
                               R=["b_wbd", "b_xsb"], W=[f"psA{pi}"])
                            op("act", lambda e, pi=pi, gt=gt, gi=gi, d=d, ct=ct, c0=c0, cnb=cnb: e.activation(out=gt[:, c0:c0 + cnb], in_=psA[pi][:, 0:cnb], func=AF.Sigmoid,
                                                                                                       bias=gb[:, gi, d, ct:ct + 1], scale=1.0), R=[f"psA{pi}", "b_gb"], W=[gk])
                    op("act", lambda e, d=d, ct=ct: e.activation(out=a2[:], in_=r_[:], func=AF.Exp, scale=cn[:, 1, d, ct:ct + 1]), R=["b_r", "b_cn"], W=["b_a2"])
                    op("act", lambda e, d=d, ct=ct: e.activation(out=r_[:], in_=r_[:], func=AF.Exp, scale=cn[:, 0, d, ct:ct + 1]), R=["b_r", "b_cn"], W=["b_r"])
                    op("act", lambda e: e.activation(out=a2[:], in_=a2[:], func=AF.Sqrt, bias=one_t[:, 0:1], scale=-1.0), R=["b_a2", "b_one"], W=["b_a2"])
                    op("dve", lambda e: e.tensor_tensor(out=i_[:], in0=i_[:], in1=xs[:], op=ALU.mult), R=["b_i", "b_xs"], W=["b_i"])
                    op("dve", lambda e: e.tensor_tensor(out=i_[:], in0=i_[:], in1=a2[:], op=ALU.mult), R=["b_i", "b_a2"], W=["b_i"])
                    if d == 0:
                        op("dve", lambda e: e.tensor_tensor_scan(out=hf[:], data0=r_[:], data1=i_[:], initial=0.0, op0=ALU.mult, op1=ALU.add), R=["b_r", "b_i"], W=["b_hf"])
                    else:
                        op("dve", lambda e: e.tensor_tensor_scan(out=hb_[:, 0:NCTX][:, ::-1], data0=r_[:, 0:NCTX][:, ::-1], data1=i_[:, 0:NCTX][:, ::-1], initial=0.0,
                                                                 op0=ALU.mult, op1=ALU.add), R=["b_r", "b_i"], W=["b_hb"])
                        op("dve", lambda e: e.tensor_tensor_scan(out=hb_[:, NCTX:T][:, ::-1], data0=r_[:, NCTX:T][:, ::-1], data1=i_[:, NCTX:T][:, ::-1], initial=hb_[:, 0:1],
                                                                 op0=ALU.mult, op1=ALU.add), R=["b_r", "b_i", "b_hb"], W=["b_hb"])
                op("dve", lambda e: e.tensor_tensor(out=hf[:], in0=hf[:], in1=hb_[:], op=ALU.add), R=["b_hf", "b_hb"], W=["b_hf"])
                op("dve", lambda e: e.tensor_tensor(out=oc[:], in0=hf[:], in1=yb[:], op=ALU.mult), R=["b_hf", "b_yb"], W=["b_oc"])
                dma("sp", lambda e, ct=ct: e.dma_start(out=ocT_d[ct * 128:(ct + 1) * 128, :], in_=oc[:]), R=["b_oc"], W=["ocT_d"])
            Bd.barrier()

        if "stopB" in debug:
            break

        with ExitStack() as ph:
            KT = sb(ph, "c_KT", [128, 4, T], BF16)
            VA = sb(ph, "c_VA", [128, NT, 4, 132], BF16)
            QT = [sb(ph, f"c_QT{i}", [128, 512], BF16) for i in range(2)]
            Pb = [sb(ph, f"c_P{i}", [128, 512], BF16) for i in range(3)]
            lq = sb(ph, "c_lq", [128, 4, 64], F32)
            lamt = sb(ph, "c_lamt", [128, 4], F32)
            gain = sb(ph, "c_gain", [128, 512], F32)
            sm = sb(ph, "c_sm", [128, 8], F32)
            o1 = sb(ph, "c_o1", [128, 128], F32)
            o2 = sb(ph, "c_o2", [128, 128], F32)
            oab = sb(ph, "c_oab", [128, 128], BF16)
            oaT = sb(ph, "c_oaT", [128, 4, 512], BF16)
            op("dve", lambda e: e.memset(VA[:], 1.0), W=["c_VA"])
            for h in range(4):
                dma("sp", lambda e, h=h: e.dma_start(out=KT[:, h, :], in_=kT_d[h]), R=["kT_d"], W=["c_KT"])
            for kt in range(NT):
                dma("sp", lambda e, kt=kt: e.dma_start(out=VA[:, kt, :, 0:128], in_=v_d[kt * 128:(kt + 1) * 128, :].rearrange("p (h e) -> p h e", h=4)), R=["v_d", "c_VA"], W=["c_VA"])
            for i, nm in enumerate(("lam_q1", "lam_k1", "lam_q2", "lam_k2")):
                dma("sp", bcast_load("sp", lq[:, i, :], Wd[nm][L, :], 64), R=["c_lq"], W=["c_lq"])
            dma("sp", bcast_load("sp", gain[:], Wd["attn_norm_g"][L].rearrange("h e -> (h e)"), 512), W=["c_gain"])
            op("dve", lambda e: e.tensor_scalar(out=gain[:], in0=gain[:], scalar1=(1.0 - lam_init), scalar2=None, op0=ALU.mult), R=["c_gain"], W=["c_gain"])
            for i in range(2):
                op("dve", lambda e, i=i: e.tensor_tensor(out=lq[:, 2 * i, :], in0=lq[:, 2 * i, :], in1=lq[:, 2 * i + 1, :], op=ALU.mult), R=["c_lq"], W=["c_lq"])
                op("dve", lambda e, i=i: e.reduce_sum(out=lamt[:, i:i + 1], in_=lq[:, 2 * i, :], axis=AX.X), R=["c_lq", "c_lamt"], W=["c_lamt"])
            op("act", lambda e: e.activation(out=lamt[:, 0:2], in_=lamt[:, 0:2], func=AF.Exp), R=["c_lamt"], W=["c_lamt"])
            op("dve", lambda e: e.tensor_tensor(out=lamt[:, 2:3], in0=lamt[:, 0:1], in1=lamt[:, 1:2], op=ALU.subtract), R=["c_lamt"], W=["c_lamt"])
            op("dve", lambda e: e.tensor_scalar(out=lamt[:, 2:3], in0=lamt[:, 2:3], scalar1=lam_init, scalar2=None, op0=ALU.add), R=["c_lamt"], W=["c_lamt"])
            if "lamt" in debug:
                dma("sp", lambda e: e.dma_start(out=dbg_d[0:128, 0:4], in_=lamt[:]), R=["c_lamt"], W=["dbg_d"])

            def acc(c, qs):
                return psA[c * 2 + qs // 2][:, (qs % 2) * 256:(qs % 2) * 256 + 129], f"psA{c * 2 + qs // 2}"

            sc_ = [0]
            qblocks = blocks if not last else blocks[1:]
            zf = [0]
            ZROWS = 2048
            if L == 0:
                ztile = sb(ph, "c_zero", [128, (ZROWS // 128) * D], BF16)
                op("pool", lambda e: e.memset(ztile[:], 0.0), W=["c_zero"])
            for (t0, nb) in qblocks:
                nsub = nb // 128
                key_tiles = list(range(2)) if t0 < NCTX else list(range(NT))
                nkt = len(key_tiles)
                for h in range(4):
                    qi = h % 2
                    if L == 0:
                        for _ in range(2):
                            if zf[0] * ZROWS < NSLOT:
                                r0 = zf[0] * ZROWS
                                zf[0] += 1
                                dma("sp", lambda e, r0=r0: e.dma_start(out=Xg[r0:r0 + ZROWS, :].rearrange("(p r) d -> p (r d)", p=128), in_=ztile[:]), R=["c_zero"], W=["Xg"])
                    dma("sp", lambda e, qi=qi, h=h: e.dma_start(out=QT[qi][:, 0:nb], in_=qT_d[h, :, t0:t0 + nb]), R=["qT_d"], W=[f"c_QT{qi}"])
                    seq = [(c, ki, kt) for c in range(2) for ki, kt in enumerate(key_tiles)]
                    base = sc_[0]
                    sc_[0] += len(seq)

                    def emit_qk(i, qi=qi, h=h, seq=seq, base=base):
                        c, ki, kt = seq[i]
                        si = (base + i) % 2
                        op("pe", lambda e: e.matmul(psS[si][:, 0:nb], lhsT=KT[c * 64:(c + 1) * 64, h, kt * 128:(kt + 1) * 128],
                                                    rhs=QT[qi][c * 64:(c + 1) * 64, 0:nb], start=True, stop=True),
                           R=["c_KT", f"c_QT{qi}"], W=[f"psS{si}"])

                    emit_qk(0)
                    for i in range(len(seq)):
                        c, ki, kt = seq[i]
                        si = (base + i) % 2
                        pj = (base + i) % 3
                        if i + 1 < len(seq):
                            emit_qk(i + 1)
                        op("act", lambda e, si=si, pj=pj: e.activation(out=Pb[pj][:, 0:nb], in_=psS[si][:, 0:nb], func=AF.Exp, scale=0.125), R=[f"psS{si}"], W=[f"c_P{pj}"])
                        for qs in range(nsub):
                            a_ap, a_key = acc(c, qs)
                            op("pe", lambda e, a_ap=a_ap, pj=pj, qs=qs, kt=kt, h=h, ki=ki: e.matmul(a_ap, lhsT=Pb[pj][:, qs * 128:(qs + 1) * 128], rhs=VA[:, kt, h, 0:129],
                                                                                             start=(ki == 0 and qs % 2 == 0), stop=(ki == nkt - 1)),
                               R=[f"c_P{pj}", "c_VA"], W=[a_key])
                    for qs in range(nsub):
                        a0, k0 = acc(0, qs)
                        a1, k1 = acc(1, qs)
                        op("dve", lambda e, a0=a0: e.reciprocal(out=sm[:, 0:1], in_=a0[:, 128:129]), R=[k0], W=["c_sm"])
                        op("dve", lambda e, a1=a1: e.reciprocal(out=sm[:, 1:2], in_=a1[:, 128:129]), R=[k1, "c_sm"], W=["c_sm"])
                        op("dve", lambda e: e.tensor_tensor(out=sm[:, 1:2], in0=sm[:, 1:2], in1=lamt[:, 2:3], op=ALU.mult), R=["c_sm", "c_lamt"], W=["c_sm"])
                        op("dve", lambda e, a1=a1: e.tensor_scalar(out=o2[:], in0=a1[:, 0:128], scalar1=sm[:, 1:2], scalar2=None, op0=ALU.mult), R=[k1, "c_sm"], W=["c_o2"])
                        op("dve", lambda e, a0=a0: e.scalar_tensor_tensor(out=o1[:], in0=a0[:, 0:128], scalar=sm[:, 0:1], in1=o2[:], op0=ALU.mult, op1=ALU.subtract),
                           R=[k0, "c_sm", "c_o2"], W=["c_o1"])
                        op("act", lambda e: e.activation(out=o2[:], in_=o1[:], func=AF.Square), R=["c_o1"], W=["c_o2"])
                        op("dve", lambda e: e.reduce_sum(out=sm[:, 2:3], in_=o2[:], axis=AX.X), R=["c_o2", "c_sm"], W=["c_sm"])
                        op("act", lambda e: e.activation(out=sm[:, 3:4], in_=sm[:, 2:3], func=AF.Sqrt, bias=eps_t[:, 1:2], scale=1.0 / 128), R=["c_sm", "eps_t"], W=["c_sm"])
                        op("dve", lambda e: e.reciprocal(out=sm[:, 3:4], in_=sm[:, 3:4]), R=["c_sm"], W=["c_sm"])
                        op("dve", lambda e, h=h: e.scalar_tensor_tensor(out=oab[:], in0=o1[:], scalar=sm[:, 3:4], in1=gain[:, h * 128:(h + 1) * 128], op0=ALU.mult, op1=ALU.mult),
                           R=["c_o1", "c_sm", "c_gain"], W=["c_oab"])
                        op("pe", lambda e: e.transpose(out=psT[0][:, 0:128], in_=oab[:], identity=ident[:]), R=["c_oab", "ident"], W=["psT0"])
                        op("act", lambda e, h=h, qs=qs: e.copy(out=oaT[:, h, qs * 128:(qs + 1) * 128], in_=psT[0][:, 0:128]), R=["psT0"], W=["c_oaT"])
                for h in range(4):
                    dma("sp", lambda e, h=h: e.dma_start(out=oaT_d[h * 128:(h + 1) * 128, t0:t0 + nb], in_=oaT[:, h, 0:nb]), R=["c_oaT"], W=["oaT_d"])
            Bd.barrier()

        if "stopC" in debug:
            break

        with ExitStack() as ph:
            wb = sb(ph, "d_wb", [128, 12, D], BF16)
            wo = sb(ph, "d_wo", [128, 8, D], BF16)
            wr = sb(ph, "d_wr", [128, 8, 64], BF16)
            wsg = sb(ph, "d_wsg", [128, 8, 256], BF16)
            wsu = sb(ph, "d_wsu", [128, 8, 256], BF16)
            wsd = sb(ph, "d_wsd", [128, 2, D], BF16)
            modb2 = sb(ph, "d_modb", [128, 6, D], F32)
            lnb = sb(ph, "d_lnb", [128, 2, D], F32)
            rbias = sb(ph, "d_rbias", [128, 64], F32)
            oT = sb(ph, "d_oT", [128, 3, 4, 512], BF16)
            gT = sb(ph, "d_gT", [128, 24, 512], BF16)
            mT = sb(ph, "d_mT", [128, 8, 512], BF16)
            tm = [sb(ph, f"d_tm{i}", [128, 512], F32) for i in range(2)]
            xt = [sb(ph, f"d_xt{i}", [128, D], F32) for i in range(2)]
            r1 = sb(ph, "d_r1", [128, D], F32)
            scr = sb(ph, "d_scr", [128, D], F32)
            st = sb(ph, "d_st", [128, 8], F32)
            x1t = sb(ph, "d_x1t", [128, D], F32)
            h2b = [sb(ph, f"d_h2b{i}", [128, D], BF16) for i in range(2)]
            h2T = sb(ph, "d_h2T", [128, 8, 512], BF16)
            sgt = sb(ph, "d_sgt", [128, 512], F32)
            actT = sb(ph, "d_actT", [128, 2, 512], BF16)
            ysh = sb(ph, "d_ysh", [128, D], F32)
            rs_sc = sb(ph, "d_rsc", [128, 64], F32)
            rs_bs = sb(ph, "d_rbs", [128, 64], F32)
            rs_t = sb(ph, "d_rt", [128, 64], F32)
            rs_sel = sb(ph, "d_rsel", [128, 64], F32)
            rs_selb = sb(ph, "d_rselb", [128, 64], BF16)
            rs_wd = sb(ph, "d_rwd", [128, 64], F32)
            rs_da = sb(ph, "d_rda", [128, 64], F32)
            rs_g = sb(ph, "d_rg", [128, 6, 8], F32)
            rs_m8 = sb(ph, "d_rm8", [128, 8], F32)
            rs_d8 = sb(ph, "d_rd8", [128, 8], F32)
            rs_s = sb(ph, "d_rs", [128, 4], F32)
            rs_m3 = sb(ph, "d_rm3", [128, 8, 64], F32)
            Rrun = sb(ph, "d_Rrun", [128, 64], F32)
            Rrunb = sb(ph, "d_Rrunb", [128, 64], BF16)
            op("dve", lambda e: e.memset(Rrun[:], 0.0), W=["d_Rrun"])
            for r in range(3):
                dma("pool", lambda e, r=r: e.dma_start(out=wb[:, r * 4:(r + 1) * 4, :], in_=Wd["w_branch"][L, r].rearrange("(cc p) n -> p cc n", p=128)), R=["d_wb"], W=["d_wb"])
            dma("pool", lambda e: e.dma_start(out=wo[:], in_=Wd["w_out"][L].rearrange("(k p) n -> p k n", p=128)), W=["d_wo"])
            dma("pool", lambda e: e.dma_start(out=wr[:], in_=Wd["w_router"][L].rearrange("(k p) n -> p k n", p=128)), W=["d_wr"])
            dma("pool", lambda e: e.dma_start(out=wsg[:], in_=Wd["sh_w_gate"][L].rearrange("(k p) n -> p k n", p=128)), W=["d_wsg"])
            dma("pool", lambda e: e.dma_start(out=wsu[:], in_=Wd["sh_w_up"][L].rearrange("(k p) n -> p k n", p=128)), W=["d_wsu"])
            dma("pool", lambda e: e.dma_start(out=wsd[:], in_=Wd["sh_w_down"][L].rearrange("(k p) n -> p k n", p=128)), W=["d_wsd"])
            for r in range(2):
                for j, seg in enumerate((2, 3, 4)):
                    dma("sp", bcast_load("sp", modb2[:, r * 3 + j, :], mod_b(r, seg), D), R=["modv", "d_modb"], W=["d_modb"])
            dma("sp", bcast_load("sp", lnb[:, 0, :], Wd["ln1_g"][L, :], D), R=["d_lnb"], W=["d_lnb"])
            dma("sp", bcast_load("sp", lnb[:, 1, :], Wd["ln1_b"][L, :], D), R=["d_lnb"], W=["d_lnb"])
            dma("sp", bcast_load("sp", rbias[:], Wd["router_bias"][L, :], 64), W=["d_rbias"])

            pc = [0]
            dblocks = blocks if not last else blocks[1:]
            for (t0, nb) in dblocks:
                mr = 1 if t0 < NCTX else 0
                nsub = nb // 128
                for r, srcT in enumerate((oaT_d, obT_d, ocT_d)):
                    dma("sp", lambda e, r=r, srcT=srcT: e.dma_start(out=oT[:, r, :, 0:nb], in_=srcT[:, t0:t0 + nb].rearrange("(cc p) t -> p cc t", p=128)),
                        R=[("oaT_d", "obT_d", "ocT_d")[r], "d_oT"], W=["d_oT"])
                dma("sp", lambda e: e.dma_start(out=gT[:, :, 0:nb], in_=gT_d[:, t0:t0 + nb].rearrange("(j p) t -> p j t", p=128)), R=["gT_d"], W=["d_gT"])
                for dt in range(8):
                    pss = []
                    for r in range(3):
                        pi = pc[0] % 4
                        pc[0] += 1
                        pss.append(pi)
                        for cc in range(4):
                            op("pe", lambda e, pi=pi, r=r, cc=cc, dt=dt: e.matmul(psA[pi][:, 0:nb], lhsT=wb[:, r * 4 + cc, dt * 128:(dt + 1) * 128], rhs=oT[:, r, cc, 0:nb],
                                                                                  start=(cc == 0), stop=(cc == 3)), R=["d_wb", "d_oT"], W=[f"psA{pi}"])
                    op("dve", lambda e, dt=dt, p0=pss[0]: e.tensor_tensor(out=tm[0][:, 0:nb], in0=psA[p0][:, 0:nb], in1=gT[:, dt, 0:nb], op=ALU.mult), R=[f"psA{pss[0]}", "d_gT"], W=["d_tm0"])
                    op("dve", lambda e, dt=dt, p1=pss[1]: e.tensor_tensor(out=tm[1][:, 0:nb], in0=psA[p1][:, 0:nb], in1=gT[:, 8 + dt, 0:nb], op=ALU.mult), R=[f"psA{pss[1]}", "d_gT"], W=["d_tm1"])
                    op("dve", lambda e: e.tensor_tensor(out=tm[0][:, 0:nb], in0=tm[0][:, 0:nb], in1=tm[1][:, 0:nb], op=ALU.add), R=["d_tm0", "d_tm1"], W=["d_tm0"])
                    op("dve", lambda e, dt=dt, p2=pss[2]: e.tensor_tensor(out=tm[1][:, 0:nb], in0=psA[p2][:, 0:nb], in1=gT[:, 16 + dt, 0:nb], op=ALU.mult), R=[f"psA{pss[2]}", "d_gT", "d_tm1"], W=["d_tm1"])
                    op("dve", lambda e, dt=dt: e.tensor_tensor(out=mT[:, dt, 0:nb], in0=tm[0][:, 0:nb], in1=tm[1][:, 0:nb], op=ALU.add), R=["d_tm0", "d_tm1"], W=["d_mT"])
                for s_ in range(nsub):
                    tt = t0 + s_ * 128
                    ti = tt // 128
                    xx = xt[s_ % 2]
                    kx = f"d_xt{s_ % 2}"
                    hh = h2b[s_ % 2]
                    kh = f"d_h2b{s_ % 2}"
                    dma("sp", lambda e, xx=xx, tt=tt: e.dma_start(out=xx[:], in_=src_rows(tt, 128)), W=[kx])
                    for half in range(2):
                        for k in range(8):
                            op("pe", lambda e, half=half, k=k, s_=s_: e.matmul(psS[half][:], lhsT=mT[:, k, s_ * 128:(s_ + 1) * 128], rhs=wo[:, k, half * 512:(half + 1) * 512],
                                                                             start=(k == 0), stop=(k == 7)), R=["d_mT", "d_wo"], W=[f"psS{half}"])
                        op("dve", lambda e, half=half: e.tensor_tensor(out=r1[:, half * 512:(half + 1) * 512], in0=psS[half][:], in1=modb2[:, mr * 3, half * 512:(half + 1) * 512], op=ALU.mult),
                           R=[f"psS{half}", "d_modb", "d_r1"], W=["d_r1"])
                    op("dve", lambda e, xx=xx: e.scalar_tensor_tensor(out=r1[:], in0=xx[:], scalar=DN_ALPHA, in1=r1[:], op0=ALU.mult, op1=ALU.add), R=[kx, "d_r1"], W=["d_r1"])
                    layer_norm_tile(r1[:], "d_r1", x1t[:], "d_x1t", scr[:], "d_scr", st, "d_st", mul_b=lnb[:, 0, :], add_b=lnb[:, 1, :], keys_b=["d_lnb"])
                    dma("sp", lambda e, tt=tt: e.dma_start(out=x1s[tt:tt + 128, :], in_=x1t[:]), R=["d_x1t"], W=["x1s"])
                    layer_norm_tile(x1t[:], "d_x1t", hh[:], kh, scr[:], "d_scr", st, "d_st", mul_b=modb2[:, mr * 3 + 2, :], add_b=modb2[:, mr * 3 + 1, :], keys_b=["d_modb"])
                    transpose_to(hh, kh, 8, h2T, "d_h2T", s_ * 128, s_ % 2)
                    pi = pc[0] % 4
                    pc[0] += 1
                    for k in range(8):
                        op("pe", lambda e, pi=pi, k=k, s_=s_: e.matmul(psA[pi][:, 0:64], lhsT=h2T[:, k, s_ * 128:(s_ + 1) * 128], rhs=wr[:, k, :], start=(k == 0), stop=(k == 7)),
                           R=["d_h2T", "d_wr"], W=[f"psA{pi}"])
                    op("act", lambda e, pi=pi: e.activation(out=rs_sc[:], in_=psA[pi][:, 0:64], func=AF.Sigmoid), R=[f"psA{pi}"], W=["d_rsc"])
                    op("dve", lambda e: e.tensor_tensor(out=rs_bs[:], in0=rs_sc[:], in1=rbias[:], op=ALU.add), R=["d_rsc", "d_rbias"], W=["d_rbs"])
                    bs3 = rs_bs[:].rearrange("p (g e) -> p g e", g=8)
                    t3 = rs_t[:].rearrange("p (g e) -> p g e", g=8)
                    op("dve", lambda e: e.tensor_reduce(out=rs_g[:, 0, :], in_=bs3, axis=AX.X, op=ALU.max), R=["d_rbs"], W=["d_rg"])
                    op("dve", lambda e: e.tensor_tensor(out=t3, in0=bs3, in1=rs_g[:, 0, :].unsqueeze(2).to_broadcast([128, 8, 8]), op=ALU.is_equal), R=["d_rbs", "d_rg"], W=["d_rt"])
                    op("dve", lambda e: e.scalar_tensor_tensor(out=rs_t[:], in0=rs_t[:], scalar=-BIG, in1=rs_bs[:], op0=ALU.mult, op1=ALU.add), R=["d_rt", "d_rbs"], W=["d_rt"])
                    op("dve", lambda e: e.tensor_reduce(out=rs_g[:, 1, :], in_=t3, axis=AX.X, op=ALU.max), R=["d_rt", "d_rg"], W=["d_rg"])
                    op("dve", lambda e: e.tensor_tensor(out=rs_g[:, 2, :], in0=rs_g[:, 0, :], in1=rs_g[:, 1, :], op=ALU.add), R=["d_rg"], W=["d_rg"])
                    op("dve", lambda e: e.max(out=rs_m8[:], in_=rs_g[:, 2, :]), R=["d_rg"], W=["d_rm8"])
                    op("dve", lambda e: e.tensor_scalar(out=rs_g[:, 3, :], in0=rs_g[:, 2, :], scalar1=rs_m8[:, 3:4], scalar2=None, op0=ALU.is_ge), R=["d_rg", "d_rm8"], W=["d_rg"])
                    op("dve", lambda e: e.tensor_scalar(out=rs_g[:, 3, :], in0=rs_g[:, 3, :], scalar1=BIG, scalar2=-BIG, op0=ALU.mult, op1=ALU.add), R=["d_rg"], W=["d_rg"])
                    op("dve", lambda e: e.tensor_tensor(out=t3, in0=bs3, in1=rs_g[:, 3, :].unsqueeze(2).to_broadcast([128, 8, 8]), op=ALU.add), R=["d_rbs", "d_rg"], W=["d_rt"])
                    op("dve", lambda e: e.max(out=rs_m8[:], in_=rs_t[:]), R=["d_rt", "d_rm8"], W=["d_rm8"])
                    op("dve", lambda e: e.tensor_scalar(out=rs_sel[:], in0=rs_t[:], scalar1=rs_m8[:, 7:8], scalar2=None, op0=ALU.is_ge), R=["d_rt", "d_rm8"], W=["d_rsel"])
                    op("dve", lambda e: e.tensor_tensor(out=rs_wd[:], in0=rs_sel[:], in1=rs_sc[:], op=ALU.mult), R=["d_rsel", "d_rsc"], W=["d_rwd"])
                    op("dve", lambda e: e.reduce_sum(out=rs_s[:, 0:1], in_=rs_wd[:], axis=AX.X), R=["d_rwd"], W=["d_rs"])
                    op("dve", lambda e: e.reciprocal(out=rs_s[:, 1:2], in_=rs_s[:, 0:1]), R=["d_rs"], W=["d_rs"])
                    op("dve", lambda e: e.tensor_scalar(out=rs_wd[:], in0=rs_wd[:], scalar1=rs_s[:, 1:2], scalar2=2.5, op0=ALU.mult, op1=ALU.mult), R=["d_rwd", "d_rs"], W=["d_rwd"])
                    op("act", lambda e: e.copy(out=rs_selb[:], in_=rs_sel[:]), R=["d_rsel"], W=["d_rselb"])
                    op("act", lambda e: e.copy(out=Rrunb[:], in_=Rrun[:]), R=["d_Rrun"], W=["d_Rrunb"])
                    pi = pc[0] % 4
                    pc[0] += 1
                    op("pe", lambda e, pi=pi: e.matmul(psA[pi][:, 0:64], lhsT=Ustr[:], rhs=rs_selb[:], start=True, stop=False), R=["Ustr", "d_rselb"], W=[f"psA{pi}"])
                    op("pe", lambda e, pi=pi: e.matmul(psA[pi][:, 0:64], lhsT=ones_bf[:], rhs=Rrunb[:], start=False, stop=True), R=["ones_bf", "d_Rrunb"], W=[f"psA{pi}"])
                    op("dve", lambda e: e.tensor_tensor(out=Rrun[:], in0=Rrun[:], in1=rs_sel[:], op=ALU.add), R=["d_Rrun", "d_rsel"], W=["d_Rrun"])
                    op("dve", lambda e, pi=pi: e.tensor_scalar(out=rs_t[:], in0=psA[pi][:, 0:64], scalar1=float(CAP), scalar2=None, op0=ALU.is_lt), R=[f"psA{pi}"], W=["d_rt"])
                    op("dve", lambda e: e.tensor_tensor(out=rs_t[:], in0=rs_t[:], in1=rs_sel[:], op=ALU.mult), R=["d_rt", "d_rsel"], W=["d_rt"])
                    op("dve", lambda e, pi=pi: e.tensor_tensor(out=rs_da[:], in0=psA[pi][:, 0:64], in1=eidx[:], op=ALU.add), R=[f"psA{pi}", "eidx"], W=["d_rda"])
                    op("dve", lambda e: e.tensor_scalar(out=rs_bs[:], in0=rs_da[:], scalar1=-1.0, scalar2=BIG, op0=ALU.mult, op1=ALU.add), R=["d_rda"], W=["d_rbs"])
                    op("dve", lambda e: e.tensor_tensor(out=rs_bs[:], in0=rs_bs[:], in1=rs_t[:], op=ALU.mult), R=["d_rbs", "d_rt"], W=["d_rbs"])
                    op("dve", lambda e: e.max(out=rs_m8[:], in_=rs_bs[:]), R=["d_rbs", "d_rm8"], W=["d_rm8"])
                    op("dve", lambda e: e.tensor_scalar(out=rs_d8[:], in0=rs_m8[:], scalar1=-1.0, scalar2=BIG, op0=ALU.mult, op1=ALU.add), R=["d_rm8"], W=["d_rd8"])
                    op("dve", lambda e, ti=ti: e.tensor_copy(out=destI[:, ti, :], in_=rs_d8[:]), R=["d_rd8", "destI"], W=["destI"])
                    op("dve", lambda e: e.tensor_tensor(out=rs_m3[:], in0=rs_da[:].unsqueeze(1).to_broadcast([128, 8, 64]), in1=rs_d8[:].unsqueeze(2).to_broadcast([128, 8, 64]), op=ALU.is_equal),
                       R=["d_rda", "d_rd8"], W=["d_rm3"])
                    op("dve", lambda e: e.tensor_tensor(out=rs_m3[:], in0=rs_m3[:], in1=rs_wd[:].unsqueeze(1).to_broadcast([128, 8, 64]), op=ALU.mult), R=["d_rm3", "d_rwd"], W=["d_rm3"])
                    op("dve", lambda e, ti=ti: e.reduce_sum(out=wk[:, ti, :], in_=rs_m3[:], axis=AX.X), R=["d_rm3", "wk"], W=["wk"])
                    if "route" in debug:
                        dma("sp", lambda e, tt=tt: e.dma_start(out=dbg_d[tt:tt + 128, 0:8], in_=rs_d8[:]), R=["d_rd8"], W=["dbg_d"])
                        dma("sp", lambda e, tt=tt, ti=ti: e.dma_start(out=dbg_d[tt:tt + 128, 8:16], in_=wk[:, ti, :]), R=["wk"], W=["dbg_d"])
                    for k in range(8):
                        dma("pool", lambda e, hh=hh, ti=ti, k=k: e.indirect_dma_start(out=Xg[:, :], out_offset=bass.IndirectOffsetOnAxis(ap=destI[:, ti, k:k + 1], axis=0),
                                                                                 in_=hh[:, :], in_offset=None, bounds_check=bc_reg, oob_is_err=False),
                            R=[kh, "destI"], W=["Xg"])
                for ft in range(2):
                    pg = pc[0] % 4
                    pu = (pc[0] + 1) % 4
                    pc[0] += 2
                    for k in range(8):
                        op("pe", lambda e, pg=pg, k=k, ft=ft: e.matmul(psA[pg][:, 0:nb], lhsT=wsg[:, k, ft * 128:(ft + 1) * 128], rhs=h2T[:, k, 0:nb], start=(k == 0), stop=(k == 7)),
                           R=["d_wsg", "d_h2T"], W=[f"psA{pg}"])
                    for k in range(8):
                        op("pe", lambda e, pu=pu, k=k, ft=ft: e.matmul(psA[pu][:, 0:nb], lhsT=wsu[:, k, ft * 128:(ft + 1) * 128], rhs=h2T[:, k, 0:nb], start=(k == 0), stop=(k == 7)),
                           R=["d_wsu", "d_h2T"], W=[f"psA{pu}"])
                    op("act", lambda e, pg=pg: e.activation(out=sgt[:, 0:nb], in_=psA[pg][:, 0:nb], func=AF.Silu), R=[f"psA{pg}"], W=["d_sgt"])
                    op("dve", lambda e, pu=pu, ft=ft: e.tensor_tensor(out=actT[:, ft, 0:nb], in0=sgt[:, 0:nb], in1=psA[pu][:, 0:nb], op=ALU.mult), R=["d_sgt", f"psA{pu}"], W=["d_actT"])
                for s_ in range(nsub):
                    tt = t0 + s_ * 128
                    for half in range(2):
                        for fc in range(2):
                            op("pe", lambda e, half=half, fc=fc, s_=s_: e.matmul(psS[half][:], lhsT=actT[:, fc, s_ * 128:(s_ + 1) * 128], rhs=wsd[:, fc, half * 512:(half + 1) * 512],
                                                                               start=(fc == 0), stop=(fc == 1)), R=["d_actT", "d_wsd"], W=[f"psS{half}"])
                        op("act", lambda e, half=half: e.copy(out=ysh[:, half * 512:(half + 1) * 512], in_=psS[half][:]), R=[f"psS{half}", "d_ysh"], W=["d_ysh"])
                    dma("sp", lambda e, tt=tt: e.dma_start(out=ysh_d[tt:tt + 128, :], in_=ysh[:]), R=["d_ysh"], W=["ysh_d"])
            Bd.barrier()

        if "stopD" in debug:
            break

        with ExitStack() as ph:
            wg = [sb(ph, f"f_wg{i}", [128, 8, 256], BF16) for i in range(2)]
            wu = [sb(ph, f"f_wu{i}", [128, 8, 256], BF16) for i in range(2)]
            wdn = [sb(ph, f"f_wd{i}", [128, 2, D], BF16) for i in range(2)]
            xg = [sb(ph, f"f_xg{i}", [128, 8, 512], BF16) for i in range(2)]
            sgt = sb(ph, "f_sgt", [128, 512], F32)
            aT = [sb(ph, f"f_aT{i}", [128, 2, 512], BF16) for i in range(2)]
            yo = [sb(ph, f"f_yo{i}", [128, D], BF16) for i in range(2)]
            pc = [0]
            yi = [0]
            NXG = 3
            xg3 = xg + [sb(ph, "f_xg2", [128, 8, 512], BF16)]
            aT3 = aT + [sb(ph, "f_aT2", [128, 2, 512], BF16)]
            chunks_f = [(ex, ch) for ex in range(NE) for ch in range(CAP // 512)]

            def load_w(ex):
                b = ex % 2
                dma("pool", lambda e: e.dma_start(out=wg[b][:], in_=Wd["moe_w_gate"][L, ex].rearrange("(k p) n -> p k n", p=128)), W=[f"f_wg{b}"])
                dma("pool", lambda e: e.dma_start(out=wu[b][:], in_=Wd["moe_w_up"][L, ex].rearrange("(k p) n -> p k n", p=128)), W=[f"f_wu{b}"])
                dma("pool", lambda e: e.dma_start(out=wdn[b][:], in_=Wd["moe_w_down"][L, ex].rearrange("(k p) n -> p k n", p=128)), W=[f"f_wd{b}"])

            def load_x(i):
                ex, ch = chunks_f[i]
                slot0 = ex * CAP + ch * 512
                xb = i % NXG
                for k in range(8):
                    dma("sp", lambda e, k=k: e.dma_start_transpose(out=xg3[xb][:, k, :], in_=Xg[slot0:slot0 + 512, k * 128:(k + 1) * 128]), R=["Xg", f"f_xg{xb}"], W=[f"f_xg{xb}"])

            load_w(0)
            load_w(1)
            xrow = [[sb(ph, f"f_xr{p}{st}", [128, D], BF16) for st in range(4)] for p in range(2)]

            def load_rows(i):
                ex, ch = chunks_f[i]
                slot0 = ex * CAP + ch * 512
                p = i % 2
                for st in range(4):
                    dma("sp", lambda e, st=st: e.dma_start(out=xrow[p][st][:], in_=Xg[slot0 + st * 128:slot0 + (st + 1) * 128, :]), R=["Xg"], W=[f"f_xr{p}{st}"])

            def emit_T(i):
                p = i % 2
                xb = i % NXG
                for st in range(4):
                    pt = psT[st % 2]
                    for k in range(8):
                        op("pe", lambda e, k=k: e.transpose(out=pt[:, k * 128:(k + 1) * 128], in_=xrow[p][st][:, k * 128:(k + 1) * 128], identity=ident[:]),
                           R=[f"f_xr{p}{st}", "ident"], W=[f"psT{st % 2}"])
                    if st % 2 == 0:
                        op("act", lambda e: e.copy(out=xg3[xb][:, :, st * 128:(st + 1) * 128], in_=pt[:, 0:1024].rearrange("p (k t) -> p k t", k=8)),
                           R=[f"psT{st % 2}"], W=[f"f_xg{xb}"])
                    else:
                        op("dve", lambda e: e.tensor_copy(out=xg3[xb][:, :, st * 128:(st + 1) * 128], in_=pt[:, 0:1024].rearrange("p (k t) -> p k t", k=8)),
                           R=[f"psT{st % 2}"], W=[f"f_xg{xb}"])

            load_rows(0)
            load_rows(1)
            bank6 = [(psA[0], "psA0"), (psA[1], "psA1"), (psA[2], "psA2"), (psA[3], "psA3"), (psS[0], "psS0"), (psS[1], "psS1")]

            def nxt():
                bk = bank6[pc[0] % 6]
                pc[0] += 1
                return bk

            def emit_gu(i):
                ex, ch = chunks_f[i]
                b = ex % 2
                xb = i % NXG
                for ft in range(2):
                    (pg, kg), (pu, ku) = nxt(), nxt()
                    for k in range(8):
                        op("pe", lambda e, k=k: e.matmul(pg[:], lhsT=wg[b][:, k, ft * 128:(ft + 1) * 128], rhs=xg3[xb][:, k, :], start=(k == 0), stop=(k == 7)),
                           R=[f"f_wg{b}", f"f_xg{xb}"], W=[kg])
                    for k in range(8):
                        op("pe", lambda e, k=k: e.matmul(pu[:], lhsT=wu[b][:, k, ft * 128:(ft + 1) * 128], rhs=xg3[xb][:, k, :], start=(k == 0), stop=(k == 7)),
                           R=[f"f_wu{b}", f"f_xg{xb}"], W=[ku])
                    op("act", lambda e: e.activation(out=sgt2[ft][:], in_=pg[:], func=AF.Silu), R=[kg], W=[f"f_sgt{ft}"])
                    op("dve", lambda e: e.tensor_tensor(out=aT3[xb][:, ft, :], in0=sgt2[ft][:], in1=pu[:], op=ALU.mult), R=[f"f_sgt{ft}", ku], W=[f"f_aT{xb}"])

            def emit_down(i):
                ex, ch = chunks_f[i]
                b = ex % 2
                xb = i % NXG
                slot0 = ex * CAP + ch * 512
                for st_ in range(4):
                    yb_ = yi[0] % 3
                    yi[0] += 1
                    for half in range(2):
                        pd, kd = nxt()
                        for fc in range(2):
                            op("pe", lambda e, fc=fc: e.matmul(pd[:], lhsT=aT3[xb][:, fc, st_ * 128:(st_ + 1) * 128], rhs=wdn[b][:, fc, half * 512:(half + 1) * 512],
                                                               start=(fc == 0), stop=(fc == 1)), R=[f"f_aT{xb}", f"f_wd{b}"], W=[kd])
                        if half == 0:
                            op("act", lambda e: e.copy(out=yo3[yb_][:, 0:512], in_=pd[:]), R=[kd, f"f_yo{yb_}"], W=[f"f_yo{yb_}"])
                        else:
                            op("dve", lambda e: e.tensor_copy(out=yo3[yb_][:, 512:1024], in_=pd[:]), R=[kd, f"f_yo{yb_}"], W=[f"f_yo{yb_}"])
                    dma("sp", lambda e: e.dma_start(out=Yg[slot0 + st_ * 128:slot0 + (st_ + 1) * 128, :], in_=yo3[yb_][:]), R=[f"f_yo{yb_}"], W=["Yg"])

            sgt2 = [sgt, sb(ph, "f_sgtb", [128, 512], F32)]
            yo3 = yo + [sb(ph, "f_yo2", [128, D], BF16)]
            emit_T(0)
            emit_gu(0)
            for i in range(len(chunks_f)):
                if i + 1 < len(chunks_f):
                    emit_T(i + 1)
                    if i + 2 < len(chunks_f):
                        load_rows(i + 2)
                    emit_gu(i + 1)
                emit_down(i)
                ex_i, ch_i = chunks_f[i]
                if ch_i == CAP // 512 - 1 and ex_i + 2 < NE:
                    load_w(ex_i + 2)
            Bd.barrier()

        with ExitStack() as ph:
            g2b = sb(ph, "g_g2b", [128, 2, D], F32)
            lnb2 = sb(ph, "g_lnb", [128, 2, D], F32)
            x1t = [sb(ph, f"g_x1t{i}", [128, D], F32) for i in range(2)]
            accb = [sb(ph, f"g_acc{i}", [128, D], F32) for i in range(2)]
            gath = [sb(ph, f"g_gath{i}", [128, D], BF16) for i in range(4)]
            scr = sb(ph, "g_scr", [128, D], F32)
            st = sb(ph, "g_st", [128, 8], F32)
            xo = [sb(ph, f"g_xo{i}", [128, D], F32) for i in range(2)]
            for r in range(2):
                dma("sp", bcast_load("sp", g2b[:, r, :], mod_b(r, 5), D), R=["modv", "g_g2b"], W=["g_g2b"])
            dma("sp", bcast_load("sp", lnb2[:, 0, :], Wd["ln2_g"][L, :], D), R=["g_lnb"], W=["g_lnb"])
            dma("sp", bcast_load("sp", lnb2[:, 1, :], Wd["ln2_b"][L, :], D), R=["g_lnb"], W=["g_lnb"])
            for i in range(4):
                op("dve", lambda e, i=i: e.memset(gath[i][:], 0.0), W=[f"g_gath{i}"])
            gi = [0]
            tiles = list(range(NT)) if not last else list(range(2, NT))
            for n_, ti in enumerate(tiles):
                tt = ti * 128
                mr = 1 if tt < NCTX else 0
                bi = n_ % 2
                dma("sp", lambda e, bi=bi, tt=tt: e.dma_start(out=x1t[bi][:], in_=x1s[tt:tt + 128, :]), R=["x1s"], W=[f"g_x1t{bi}"])
                dma("sp", lambda e, bi=bi, tt=tt: e.dma_start(out=accb[bi][:], in_=ysh_d[tt:tt + 128, :]), R=["ysh_d"], W=[f"g_acc{bi}"])
                for k in range(8):
                    gj = gi[0] % 4
                    gi[0] += 1
                    dma("pool", lambda e, gj=gj, ti=ti, k=k: e.indirect_dma_start(out=gath[gj][:, :], out_offset=None, in_=Yg[:, :],
                                                                              in_offset=bass.IndirectOffsetOnAxis(ap=destI[:, ti, k:k + 1], axis=0),
                                                                              bounds_check=bc_reg, oob_is_err=False), R=["Yg", "destI"], W=[f"g_gath{gj}"])
                    op("dve", lambda e, gj=gj, bi=bi, ti=ti, k=k: e.scalar_tensor_tensor(out=accb[bi][:], in0=gath[gj][:], scalar=wk[:, ti, k:k + 1], in1=accb[bi][:], op0=ALU.mult, op1=ALU.add),
                       R=[f"g_gath{gj}", "wk", f"g_acc{bi}"], W=[f"g_acc{bi}"])
                op("dve", lambda e, bi=bi, mr=mr: e.tensor_tensor(out=accb[bi][:], in0=accb[bi][:], in1=g2b[:, mr, :], op=ALU.mult), R=[f"g_acc{bi}", "g_g2b"], W=[f"g_acc{bi}"])
                op("dve", lambda e, bi=bi: e.scalar_tensor_tensor(out=accb[bi][:], in0=x1t[bi][:], scalar=DN_ALPHA, in1=accb[bi][:], op0=ALU.mult, op1=ALU.add), R=[f"g_x1t{bi}", f"g_acc{bi}"], W=[f"g_acc{bi}"])
                layer_norm_tile(accb[bi][:], f"g_acc{bi}", xo[bi][:], f"g_xo{bi}", scr[:], "g_scr", st, "g_st", mul_b=lnb2[:, 0, :], add_b=lnb2[:, 1, :], keys_b=["g_lnb"])
                if last:
                    dma("sp", lambda e, bi=bi, tt=tt: e.dma_start(out=out_d[tt - NCTX:tt - NCTX + 128, :], in_=xo[bi][:]), R=[f"g_xo{bi}"], W=["out"])
                else:
                    dma("sp", lambda e, bi=bi, tt=tt: e.dma_start(out=xA[tt:tt + 128, :], in_=xo[bi][:]), R=[f"g_xo{bi}"], W=["xA"])
            Bd.barrier()

    Bd.barrier()
    print("instructions:", Bd.ninst, "semaphores:", Bd.nsem)
    return nc


_NC_CACHE = {}


def kernel(**inputs):
    x = np.ascontiguousarray(inputs["x"], dtype=np.float32)
    ctx = np.ascontiguousarray(inputs["ctx"], dtype=np.float32)
    c = np.asarray(inputs["c"], dtype=np.float32)
    c_ctx = np.asarray(inputs["c_ctx"], dtype=np.float32)
    nb = x.shape[0]
    if "nc" not in _NC_CACHE:
        _NC_CACHE["nc"] = build()
    nc = _NC_CACHE["nc"]
    shared = {n: np.ascontiguousarray(inputs[n], dtype=np.float32) for n in W_NAMES}
    in_maps = []
    for b in range(nb):
        m = dict(shared)
        m["x"] = x[b]
        m["ctx"] = ctx[b]
        m["c"] = np.stack([c[b], c_ctx], axis=0)
        in_maps.append(m)
    res = run_bass_kernel_spmd(nc, in_maps, core_ids=list(range(nb)))
    return np.stack([np.asarray(r["out"], dtype=np.float32) for r in res.results], axis=0)
```
